# Optimizing a Trainium2 kernel written in Bass

```python
import math
import jax, jax.numpy as jnp
from jax import lax
import numpy as np

D_MODEL = 2048
BATCH = 4
SEQ = 2048
DEPTH = 2

N_MEM = 256
GRID_W = 64
MIX_W = D_MODEL
NA_HEAD_DIM = 64
NA_W = 3 * MIX_W // 8
NA_HEADS = NA_W // NA_HEAD_DIM
NA_KR = 8
NA_KC = 16
HY_W = MIX_W // 4
HY_ORDER = 2
HY_BANDS = 8
HY_POS_DIM = 1 + 2 * HY_BANDS
HY_FILT_FF = 64
HY_FAST_DECAY_PCT = 0.3
HY_SLOW_DECAY_PCT = 1.5
HY_DECAY_TARGET = 1e-2
RET_HEAD_DIM = 128
RET_W = MIX_W - NA_W - HY_W
RET_HEADS = RET_W // RET_HEAD_DIM
RET_CHUNK = 128
P_IN = 3 * NA_W + 3 * HY_W + 4 * RET_W
CROSS_HEADS = 4
CROSS_HEAD_DIM = D_MODEL // CROSS_HEADS
N_EXPERTS = 16
EXPERT_FF = 2048
EC_CAPACITY = 2
RMS_EPS = 1e-6
GN_EPS = 1e-5

kernel_name = "hymba_style_hybrid_encoder"

F32 = jnp.float32


def rms_norm(x, g):
    xf = x.astype(F32)
    y = xf * lax.rsqrt(jnp.mean(xf * xf, axis=-1, keepdims=True) + RMS_EPS)
    return (y * g.astype(F32)).astype(x.dtype)


def group_rms_norm(parts, gain):
    normed = [p.astype(F32) * lax.rsqrt(jnp.mean(jnp.square(p.astype(F32)), axis=-1, keepdims=True) + RMS_EPS) for p in parts]
    y = jnp.concatenate(normed, axis=-1) * gain.astype(F32)
    return y.astype(parts[0].dtype)


def neighbourhood_attention(q, k, v, rpb):
    B, L, H, dh = q.shape
    rows = L // GRID_W
    kr = min(NA_KR, rows)
    q = q.reshape(B, rows, GRID_W, H, dh)
    k = k.reshape(B, rows, GRID_W, H, dh)
    v = v.reshape(B, rows, GRID_W, H, dh)
    r = jnp.arange(rows)
    row_start = jnp.clip(r - kr // 2, 0, rows - kr)
    row_idx = row_start[:, None] + jnp.arange(kr)[None, :]
    k_band = k[:, row_idx]
    v_band = v[:, row_idx]
    s = jnp.einsum('brchd,brkwhd->bhrckw', q, k_band).astype(F32) * (dh ** -0.5)
    c = jnp.arange(GRID_W)
    col_start = jnp.clip(c - NA_KC // 2, 0, GRID_W - NA_KC)
    col_in = (c[None, :] >= col_start[:, None]) & (c[None, :] < col_start[:, None] + NA_KC)
    dr = row_idx - r[:, None] + (NA_KR - 1)
    dc = jnp.clip(c[None, :] - c[:, None] + (NA_KC - 1), 0, 2 * NA_KC - 2)
    bias = rpb[:, dr[:, None, :, None], dc[None, :, None, :]].astype(F32)
    s = jnp.where(col_in[None, None, None, :, None, :], s + bias[None], -jnp.inf)
    p = jax.nn.softmax(s, axis=(-2, -1)).astype(v.dtype)
    o = jnp.einsum('bhrckw,brkwhd->brchd', p, v_band)
    return o.reshape(B, L, H * dh)


def hyena_filters(L, w1, b1, w2, b2, w3, freq):
    t = jnp.arange(L, dtype=F32)
    t01 = t / (L - 1)
    bands = jnp.linspace(1e-4, HY_BANDS - 1, HY_BANDS, dtype=F32)
    ang = (2.0 * math.pi) * (t[:, None] / L) * bands[None, :]
    feats = jnp.concatenate([t01[:, None], jnp.cos(ang), -jnp.sin(ang)], axis=-1)
    f = freq.astype(F32)
    h = jnp.sin(f * (feats @ w1.astype(F32) + b1.astype(F32)))
    h = jnp.sin(f * (h @ w2.astype(F32) + b2.astype(F32)))
    h = (h @ w3.astype(F32)).reshape(L, HY_ORDER, 2, HY_W)
    min_decay = math.log(HY_DECAY_TARGET) / HY_SLOW_DECAY_PCT
    max_decay = math.log(HY_DECAY_TARGET) / HY_FAST_DECAY_PCT
    deltas = jnp.abs(jnp.linspace(min_decay, max_decay, HY_W, dtype=F32))
    window = jnp.exp(-t01[:, None] * deltas[None, :])
    return h * window[:, None, None, :]


def hyena_mixer(u, conv_w, conv_b, w1, b1, w2, b2, w3, freq, skip_d):
    B, L, _ = u.shape
    p = jnp.pad(u, ((0, 0), (1, 1), (0, 0)))
    s = p[:, :-2] * conv_w[0] + p[:, 1:-1] * conv_w[1] + p[:, 2:] * conv_w[2] + conv_b
    x1, x2, v = jnp.split(s, 3, axis=-1)
    filt = hyena_filters(L, w1, b1, w2, b2, w3, freq)
    kbuf = jnp.concatenate([filt[:, :, 0], jnp.zeros((1, HY_ORDER, HY_W), F32), filt[1:, :, 1][::-1]], axis=0)
    kfreq = jnp.fft.rfft(kbuf, axis=0)
    n = 2 * L
    z = v.astype(F32)
    for o, gate in enumerate((x1, x2)):
        conv = jnp.fft.irfft(jnp.fft.rfft(z, n=n, axis=1) * kfreq[None, :, o], n=n, axis=1)[:, :L]
        z = gate.astype(F32) * (conv + skip_d[o].astype(F32) * z)
    return z.astype(u.dtype)


def rotary(x, pos):
    half = x.shape[-1] // 2
    inv = 1.0 / (10000.0 ** jnp.linspace(0.0, 1.0, half, dtype=F32))
    ang = pos[:, None] * inv[None, :]
    cos = jnp.cos(ang)[None, :, None, :]
    sin = jnp.sin(ang)[None, :, None, :]
    x1, x2 = x[..., :half], x[..., half:]
    return jnp.concatenate([x1 * cos - x2 * sin, x1 * sin + x2 * cos], axis=-1)


def retention_chunkwise(q, k, v, log_gamma, include_diag):
    B, H, L, d = q.shape
    c = RET_CHUNK
    n = L // c
    qc = q.reshape(B, H, n, c, d)
    kc = k.reshape(B, H, n, c, d)
    vc = v.reshape(B, H, n, c, d)
    i = jnp.arange(c, dtype=F32)
    diff = i[:, None] - i[None, :]
    mask = (diff >= 0) if include_diag else (diff > 0)
    dec = jnp.where(mask[None], jnp.exp(log_gamma[:, None, None] * jnp.where(mask, diff, 0.0)[None]), 0.0)
    a = jnp.einsum('bhnid,bhnjd->bhnij', qc, kc) * dec[None, :, None]
    y = jnp.einsum('bhnij,bhnje->bhnie', a, vc)
    zeta = jnp.exp(log_gamma[:, None] * (c - 1 - i)[None, :])
    kv = jnp.einsum('bhnjd,hj,bhnje->nbhde', kc, zeta, vc)
    chunk_decay = jnp.exp(log_gamma * c)[None, :, None, None]

    def step(state, kv_n):
        return chunk_decay * state + kv_n, state

    _, prev = lax.scan(step, jnp.zeros((B, H, d, d), F32), kv)
    xi = jnp.exp(log_gamma[:, None] * (i + 1.0)[None, :])
    y = y + jnp.einsum('bhnid,hi,nbhde->bhnie', qc, xi, prev)
    return y.reshape(B, H, L, d)


def retention_mixer(r_in):
    B, L, _ = r_in.shape
    q, k, v, g = jnp.split(r_in, 4, axis=-1)
    shp = (B, L, RET_HEADS, RET_HEAD_DIM)
    pos = jnp.arange(L, dtype=F32)
    q = rotary(q.reshape(shp).astype(F32), pos) * (RET_HEAD_DIM ** -0.5)
    k = rotary(k.reshape(shp).astype(F32), pos)
    v = v.reshape(shp).astype(F32)
    q, k, v = (jnp.transpose(t, (0, 2, 1, 3)) for t in (q, k, v))
    hidx = jnp.arange(RET_HEADS, dtype=F32)
    lg_fwd = jnp.log1p(-jnp.exp2(-5.0 - hidx))
    lg_bwd = jnp.log1p(-jnp.exp2(-5.5 - hidx))
    y_f = retention_chunkwise(q, k, v, lg_fwd, True)
    y_b = jnp.flip(retention_chunkwise(jnp.flip(q, 2), jnp.flip(k, 2), jnp.flip(v, 2), lg_bwd, False), 2)
    y = jnp.transpose(y_f + y_b, (0, 2, 1, 3))
    mu = jnp.mean(y, axis=-1, keepdims=True)
    var = jnp.mean(jnp.square(y - mu), axis=-1, keepdims=True)
    y = ((y - mu) * lax.rsqrt(var + GN_EPS)).reshape(B, L, RET_W)
    return (y * jax.nn.silu(g.astype(F32))).astype(r_in.dtype)


def cross_attend(h, mem_n, w_cq, w_ckv, w_co):
    B, L, _ = h.shape
    M = mem_n.shape[1]
    q = (h @ w_cq).reshape(B, L, CROSS_HEADS, CROSS_HEAD_DIM)
    kv = (mem_n @ w_ckv).reshape(B, M, 2, CROSS_HEADS, CROSS_HEAD_DIM)
    k, v = kv[:, :, 0], kv[:, :, 1]
    s = jnp.einsum('blhd,bmhd->bhlm', q, k).astype(F32) * (CROSS_HEAD_DIM ** -0.5)
    p = jax.nn.softmax(s, axis=-1).astype(v.dtype)
    o = jnp.einsum('bhlm,bmhd->blhd', p, v).reshape(B, L, D_MODEL)
    return o @ w_co


def expert_choice_ffn(h, w_router, w_gate, w_up, w_down):
    B, T, D = h.shape
    cap = EC_CAPACITY * T // N_EXPERTS
    aff = jax.nn.softmax(jnp.einsum('btd,de->bte', h, w_router).astype(F32), axis=-1)
    g, idx = lax.top_k(jnp.transpose(aff, (0, 2, 1)), cap)
    xe = jax.vmap(lambda hb, ib: hb[ib])(h, idx)
    a = jnp.einsum('becd,edf->becf', xe, w_gate)
    u = jnp.einsum('becd,edf->becf', xe, w_up)
    y = jnp.einsum('becf,efd->becd', jax.nn.silu(a) * u, w_down)
    y = y * g[..., None].astype(y.dtype)
    return jax.vmap(lambda yb, ib: jnp.zeros((T, D), yb.dtype).at[ib.reshape(-1)].add(yb.reshape(-1, D)))(y, idx)


def setup_inputs(seed: int = 0) -> dict:
    key = jax.random.key(seed)
    ks = jax.random.split(key, 32)

    def nrm(k, shape, scale):
        return jax.random.normal(k, shape, F32) * scale

    def gain(k, shape):
        return 1.0 + 0.05 * jax.random.normal(k, shape, F32)

    L_ = DEPTH
    return {
        "x": nrm(ks[0], (BATCH, SEQ, D_MODEL), 1.0),
        "mem": nrm(ks[1], (BATCH, N_MEM, D_MODEL), 1.0),
        "norm_mix": gain(ks[2], (L_, D_MODEL)),
        "w_in": nrm(ks[3], (L_, D_MODEL, P_IN), D_MODEL ** -0.5),
        "na_rpb": nrm(ks[4], (L_, NA_HEADS, 2 * NA_KR - 1, 2 * NA_KC - 1), 0.02),
        "hy_conv_w": nrm(ks[5], (L_, 3, 3 * HY_W), 3 ** -0.5),
        "hy_conv_b": nrm(ks[6], (L_, 3 * HY_W), 0.02),
        "hy_filt_w1": nrm(ks[7], (L_, HY_POS_DIM, HY_FILT_FF), HY_POS_DIM ** -0.5),
        "hy_filt_b1": nrm(ks[8], (L_, HY_FILT_FF), 0.02),
        "hy_filt_w2": nrm(ks[9], (L_, HY_FILT_FF, HY_FILT_FF), HY_FILT_FF ** -0.5),
        "hy_filt_b2": nrm(ks[10], (L_, HY_FILT_FF), 0.02),
        "hy_filt_w3": nrm(ks[11], (L_, HY_FILT_FF, HY_ORDER * 2 * HY_W), HY_FILT_FF ** -0.5),
        "hy_sin_freq": gain(ks[12], (L_, HY_FILT_FF)),
        "hy_skip_d": nrm(ks[13], (L_, HY_ORDER, HY_W), 0.1),
        "branch_norm": gain(ks[14], (L_, MIX_W)),
        "w_out": nrm(ks[15], (L_, MIX_W, D_MODEL), MIX_W ** -0.5),
        "norm_cross": gain(ks[16], (L_, D_MODEL)),
        "mem_norm": gain(ks[17], (D_MODEL,)),
        "w_cq": nrm(ks[18], (L_, D_MODEL, D_MODEL), D_MODEL ** -0.5),
        "w_ckv": nrm(ks[19], (L_, D_MODEL, 2 * D_MODEL), D_MODEL ** -0.5),
        "w_co": nrm(ks[20], (L_, D_MODEL, D_MODEL), D_MODEL ** -0.5),
        "norm_moe": gain(ks[21], (L_, D_MODEL)),
        "w_router": nrm(ks[22], (L_, D_MODEL, N_EXPERTS), D_MODEL ** -0.5),
        "w_gate": nrm(ks[23], (L_, N_EXPERTS, D_MODEL, EXPERT_FF), D_MODEL ** -0.5),
        "w_up": nrm(ks[24], (L_, N_EXPERTS, D_MODEL, EXPERT_FF), D_MODEL ** -0.5),
        "w_down": nrm(ks[25], (L_, N_EXPERTS, EXPERT_FF, D_MODEL), EXPERT_FF ** -0.5),
        "final_norm": gain(ks[26], (D_MODEL,)),
    }


def reference(x, mem, norm_mix, w_in, na_rpb, hy_conv_w, hy_conv_b, hy_filt_w1, hy_filt_b1,
              hy_filt_w2, hy_filt_b2, hy_filt_w3, hy_sin_freq, hy_skip_d, branch_norm, w_out,
              norm_cross, mem_norm, w_cq, w_ckv, w_co, norm_moe, w_router, w_gate, w_up, w_down,
              final_norm):
    B, L, _ = x.shape
    mem_n = rms_norm(mem, mem_norm)
    for l in range(DEPTH):
        h = rms_norm(x, norm_mix[l])
        proj = h @ w_in[l]
        na_in, hy_in, ret_in = jnp.split(proj, [3 * NA_W, 3 * NA_W + 3 * HY_W], axis=-1)
        na_q, na_k, na_v = (t.reshape(B, L, NA_HEADS, NA_HEAD_DIM) for t in jnp.split(na_in, 3, axis=-1))
        y_na = neighbourhood_attention(na_q, na_k, na_v, na_rpb[l])
        y_hy = hyena_mixer(hy_in, hy_conv_w[l], hy_conv_b[l], hy_filt_w1[l], hy_filt_b1[l],
                           hy_filt_w2[l], hy_filt_b2[l], hy_filt_w3[l], hy_sin_freq[l], hy_skip_d[l])
        y_ret = retention_mixer(ret_in)
        y = group_rms_norm([y_na, y_hy, y_ret], branch_norm[l])
        x = x + y @ w_out[l]
        x = x + cross_attend(rms_norm(x, norm_cross[l]), mem_n, w_cq[l], w_ckv[l], w_co[l])
        x = x + expert_choice_ffn(rms_norm(x, norm_moe[l]), w_router[l], w_gate[l], w_up[l], w_down[l])
    return rms_norm(x, final_norm)
```

```python
import math
import numpy as np
import ml_dtypes
import concourse.bass as bass
import concourse.mybir as mybir
from concourse.bass_utils import run_bass_kernel_spmd
from contextlib import ExitStack

dt = mybir.dt
F32, BF16, I32 = dt.float32, dt.bfloat16, dt.int32
AF = mybir.ActivationFunctionType
ALU = mybir.AluOpType
AX = mybir.AxisListType

SAME_ENGINE_SYNC = True

D = 2048
L = 2048
DEPTH = 2
NMEM = 256
NA_H = 12
P_IN = 6912
NE = 16
CAP = 256
RMS_EPS = 1e-6
GN_EPS = 1e-5
PI = math.pi


class Sem:
    def __init__(self, h):
        self.h = h
        self.n = 0


class Buf:
    def __init__(self, t=None, name=""):
        self.t = t
        self.name = name
        self.w = None
        self.r = {}
        self.dsem = None

    def __getitem__(self, idx):
        return self.t[idx]


class Eng:
    def __init__(self, h, sem, name):
        self.h = h
        self.sem = sem
        self.name = name
        self.known = {}


class Ctx:
    def __init__(self, nc):
        self.nc = nc
        self.es = ExitStack()
        self.eng = {}
        for name, h in (("pe", nc.tensor), ("dve", nc.vector), ("act", nc.scalar),
                        ("pool", nc.gpsimd), ("sp", nc.sync)):
            s = Sem(self.es.enter_context(nc.semaphore("es_" + name)))
            self.eng[name] = Eng(h, s, name)
        self.free_sems = []
        self.all_dsems = []
        self.stage_bufs = []
        self.ninst = 0
        self.flip = 0

    def new_dsem(self):
        if self.free_sems:
            return self.free_sems.pop()
        s = Sem(self.es.enter_context(self.nc.semaphore("ds%d" % len(self.all_dsems))))
        self.all_dsems.append(s)
        return s

    def sb(self, st, name, shape, dtype):
        self.uid = getattr(self, "uid", 0) + 1
        name = "s%d_%s" % (self.uid, name)
        t = st.enter_context(self.nc.sbuf_tensor(name, list(shape), dtype))
        b = Buf(t, name)
        self.stage_bufs.append(b)
        return b

    def ps(self, st, name, shape, dtype=F32):
        self.uid = getattr(self, "uid", 0) + 1
        name = "p%d_%s" % (self.uid, name)
        t = st.enter_context(self.nc.psum_tensor(name, list(shape), dtype))
        b = Buf(t, name)
        self.stage_bufs.append(b)
        return b

    def _waits(self, E, r, w):
        waits = {}

        def need(s, v):
            if waits.get(s, 0) < v:
                waits[s] = v
        for b in r:
            if b.w is not None:
                need(*b.w)
        for b in w:
            if b.w is not None:
                need(*b.w)
            for s, v in b.r.items():
                need(s, v)
        for s, v in waits.items():
            if E.known.get(s, 0) >= v:
                continue
            if s is E.sem and (E.name == "pe" or not SAME_ENGINE_SYNC):
                continue
            E.h.wait_ge(s.h, v)
            self.ninst += 1
            E.known[s] = v

    def _record(self, dep, r, w):
        s, v = dep
        for b in r:
            if b.r.get(s, 0) < v:
                b.r[s] = v
        for b in w:
            b.w = dep
            b.r = {}

    def op(self, eng, fn, r=(), w=(), signal=True):
        E = self.eng[eng]
        self._waits(E, r, w)
        inst = fn(E.h)
        self.ninst += 1
        if signal:
            E.sem.n += 1
            inst.then_inc(E.sem.h, 1)
            dep = (E.sem, E.sem.n)
        else:
            dep = (E.sem, E.sem.n + 1)
        self._record(dep, r, w)
        return inst

    def dma(self, q, out, in_, r=(), w=(), sbuf=None, **kw):
        return self.custom_dma(q, lambda e: e.dma_start(out=out, in_=in_, **kw), r=r, w=w, sbuf=sbuf)

    def custom_dma(self, q, fn, r=(), w=(), sbuf=None):
        E = self.eng[q]
        self._waits(E, r, w)
        inst = fn(E.h)
        self.ninst += 1
        if sbuf.dsem is None:
            sbuf.dsem = self.new_dsem()
        s = sbuf.dsem
        s.n += 16
        inst.then_inc(s.h, 16)
        self._record((s, s.n), r, w)
        return inst

    def barrier(self):
        targets = [(e.sem, e.sem.n) for e in self.eng.values() if e.sem.n > 0]
        targets += [(s, s.n) for s in self.all_dsems if s.n > 0]
        for E in self.eng.values():
            for s, v in targets:
                if s is E.sem or E.known.get(s, 0) >= v:
                    continue
                E.h.wait_ge(s.h, v)
                self.ninst += 1
                E.known[s] = v

    def end_stage(self, dram_bufs=()):
        self.barrier()
        for b in self.stage_bufs:
            if b.dsem is not None:
                self.free_sems.append(b.dsem)
                b.dsem = None
        self.stage_bufs = []
        for b in dram_bufs:
            b.w = None
            b.r = {}

    def evac_eng(self):
        self.flip ^= 1
        return "act" if self.flip else "dve"


def copy_on(c, eng, out, in_, r, w, scale=None):
    if eng == "act":
        if scale is None:
            c.op("act", lambda e: e.copy(out=out, in_=in_), r=r, w=w)
        else:
            c.op("act", lambda e: e.mul(out, in_, float(scale)), r=r, w=w)
    else:
        if scale is None:
            c.op(eng, lambda e: e.tensor_copy(out=out, in_=in_), r=r, w=w)
        else:
            c.op(eng, lambda e: e.tensor_scalar(out=out, in0=in_, scalar1=float(scale), scalar2=None, op0=ALU.mult), r=r, w=w)


def bf(a):
    return np.ascontiguousarray(np.asarray(a, np.float32).astype(ml_dtypes.bfloat16))


def na_geometry():
    rows = 32

    def rs(r):
        return min(max(r - 4, 0), rows - 8)
    pairs = {}
    pats = {}
    c = np.arange(64)
    cs = np.clip(c - 8, 0, 48)
    colok = (c[:, None] >= cs[None, :]) & (c[:, None] < cs[None, :] + 16)
    tiles = []
    for i in range(16):
        lst = []
        for j in range(16):
            v = [[rs(2 * i + b) <= 2 * j + a < rs(2 * i + b) + 8 for b in range(2)] for a in range(2)]
            if not any(v[0] + v[1]):
                continue
            key = tuple(v[0] + v[1])
            if key not in pats:
                pats[key] = len(tiles)
                m = np.full((128, 128), -30000.0, np.float32)
                for a in range(2):
                    for b in range(2):
                        if v[a][b]:
                            blk = np.where(colok, 0.0, -30000.0)
                            m[a * 64:(a + 1) * 64, b * 64:(b + 1) * 64] = blk
                tiles.append(m)
            lst.append((j, 2 * j - 2 * i, pats[key]))
        pairs[i] = lst
    masks = np.stack(tiles, 0)
    return pairs, masks


def make_consts():
    cst = {}
    t = np.arange(L, dtype=np.float64)
    f = np.arange(L, dtype=np.float64)
    th = 2.0 * np.pi * (f[None, :] + 0.5) * t[:, None] / (2 * L)
    C = np.cos(th)
    S = np.sin(th)
    def fwd_layout(M):
        return M.reshape(16, 128, 16, 128).transpose(2, 1, 0, 3)
    cst["dftC"] = bf(fwd_layout(C))
    cst["dftS"] = bf(fwd_layout(S))
    def inv_layout(M):
        MT = M.T
        return MT.reshape(16, 128, 16, 128).transpose(2, 1, 0, 3)
    cst["dftCT"] = bf(inv_layout(C))
    cst["dftnST"] = bf(inv_layout(-S))
    tt = np.arange(L, dtype=np.float32)
    t01 = tt / (L - 1)
    bands = np.linspace(1e-4, 8 - 1, 8, dtype=np.float32)
    ang = (2.0 * np.float32(np.pi)) * (tt[:, None] / L) * bands[None, :]
    feats = np.concatenate([t01[:, None], np.cos(ang), -np.sin(ang)], axis=-1).astype(np.float32)
    cst["featsT"] = np.ascontiguousarray(feats.T)
    min_decay = math.log(1e-2) / 1.5
    max_decay = math.log(1e-2) / 0.3
    deltas = np.abs(np.linspace(min_decay, max_decay, 512, dtype=np.float32))
    cst["window"] = np.exp(-t01[:, None] * deltas[None, :]).astype(np.float32)
    inv = 1.0 / (10000.0 ** np.linspace(0.0, 1.0, 64, dtype=np.float32))
    angr = tt[:, None] * inv[None, :]
    cosT = np.cos(angr).T.astype(np.float32)
    sinT = np.sin(angr).T.astype(np.float32)
    cst["rotcos"] = np.ascontiguousarray(np.concatenate([cosT, cosT], 0))
    cst["rotsin"] = np.ascontiguousarray(np.concatenate([sinT, sinT], 0))
    R = np.zeros((128, 128), np.float32)
    for m in range(64):
        R[m + 64, m] = -1.0
    for m in range(64, 128):
        R[m - 64, m] = 1.0
    cst["rotR"] = bf(R)
    hidx = np.arange(6, dtype=np.float64)
    lgf = np.log1p(-np.exp2(-5.0 - hidx))
    lgb = np.log1p(-np.exp2(-5.5 - hidx))
    dd = np.zeros((6, 4, 128, 512), np.float32)
    jj = np.arange(128)[:, None]
    ii = np.arange(512)[None, :]
    for h in range(6):
        for pos in range(4):
            diff = ii - (jj + pos * 128)
            dd[h, pos] = np.where(diff >= 0, np.exp(lgf[h] * np.maximum(diff, 0)), np.exp(lgb[h] * np.maximum(-diff, 0)))
    cst["retD"] = bf(dd.transpose(0, 2, 1, 3))
    cst["retE"] = (ii - jj).astype(np.float32)
    cst["_lgf"] = lgf
    cst["_lgb"] = lgb
    pairs, masks = na_geometry()
    cst["namask"] = bf(masks.transpose(1, 0, 2))
    J = np.zeros((128, 128), np.float32)
    for a in range(2):
        for k in range(64):
            J[a * 64 + 63 - k, a * 64 + k] = 1.0
    cst["naJ"] = bf(J)
    cst["ident"] = bf(np.eye(128))
    cst["identf"] = np.eye(128, dtype=np.float32)
    cst["ones"] = bf(np.ones((128, 128)))
    cst["iota256"] = np.tile(np.arange(256, dtype=np.float32)[None, :], (128, 1))
    pj = np.zeros((128, 16, 3), np.float32)
    pj[:, :, 0] = np.arange(128)[:, None]
    pj[:, :, 1] = np.arange(16)[None, :]
    cst["pj"] = pj
    gw = np.zeros((128, 3), np.float32)
    gw[:, 0] = 1.0 / 768
    gw[:, 1] = 1.0 / 512
    gw[:, 2] = 1.0 / 768
    cst["ginvw"] = gw
    return cst, pairs


def rpb_layout(na_rpb):
    Y = np.zeros((DEPTH, NA_H, 15, 128), np.float32)
    for m in range(15):
        dri = 14 - m
        Y[:, :, m, 48:79] = na_rpb[:, :, dri, ::-1]
    return Y


CONST_DTYPES = {"dftC": BF16, "dftS": BF16, "dftCT": BF16, "dftnST": BF16, "featsT": F32, "window": F32,
                "rotcos": F32, "rotsin": F32, "rotR": BF16, "retD": BF16, "retE": F32, "namask": BF16,
                "naJ": BF16, "ident": BF16, "identf": F32, "ones": BF16, "iota256": F32, "pj": F32,
                "ginvw": F32}

WEIGHT_SHAPES = {
    "norm_mix": (DEPTH, D), "w_in": (DEPTH, D, P_IN), "rpbY": (DEPTH, NA_H, 15, 128),
    "hy_conv_w": (DEPTH, 3, 1536), "hy_conv_b": (DEPTH, 1536), "hy_filt_w1": (DEPTH, 17, 64),
    "hy_filt_b1": (DEPTH, 64), "hy_filt_w2": (DEPTH, 64, 64), "hy_filt_b2": (DEPTH, 64),
    "hy_filt_w3": (DEPTH, 64, 2048), "hy_sin_freq": (DEPTH, 64), "hy_skip_d": (DEPTH, 2, 512),
    "branch_norm": (DEPTH, D), "w_out": (DEPTH, D, D), "norm_cross": (DEPTH, D), "mem_norm": (D,),
    "w_cq": (DEPTH, D, D), "w_ckv": (DEPTH, D, 2 * D), "w_co": (DEPTH, D, D), "norm_moe": (DEPTH, D),
    "w_router": (DEPTH, D, NE), "w_gate": (DEPTH, NE, D, D), "w_up": (DEPTH, NE, D, D),
    "w_down": (DEPTH, NE, D, D), "final_norm": (D,),
}


def row_bc(t, off, n, parts=128):
    return bass.AP(t, off, [[0, parts], [1, n]])


class Prog:
    def __init__(self, debug=False, stop_after=None, nlayers=DEPTH):
        self.debug = debug
        self.stop_after = stop_after
        self.nlayers = nlayers
        self.consts, self.na_pairs = make_consts()
        self.npat = self.consts["namask"].shape[1]
        nc = bass.Bass("TRN2", target_bir_lowering=False)
        self.nc = nc
        self.c = Ctx(nc)
        T = {}
        T["x"] = nc.dram_tensor("x", [L, D], F32, kind="ExternalInput")
        T["mem"] = nc.dram_tensor("mem", [NMEM, D], F32, kind="ExternalInput")
        for k, shp in WEIGHT_SHAPES.items():
            T[k] = nc.dram_tensor(k, list(shp), F32, kind="ExternalInput")
        for k, dty in CONST_DTYPES.items():
            T[k] = nc.dram_tensor(k, list(self.consts[k].shape), dty, kind="ExternalInput")
        T["out"] = nc.dram_tensor("out", [L, D], F32, kind="ExternalOutput")
        sk = "ExternalOutput" if debug else "Internal"
        for name, shp, dty in [
            ("xres", [L, D], F32), ("naqT", [768, L], BF16), ("nakT", [768, L], BF16), ("nav", [L, 768], BF16),
            ("hyp", [L, 1536], F32), ("hyx", [L, 1024], F32), ("rqT", [768, L], BF16), ("rkT", [768, L], BF16),
            ("rv", [L, 768], BF16), ("rg", [L, 768], F32), ("ymix", [L, D], F32), ("cqT", [D, L], BF16),
            ("coT", [D, L], BF16), ("ckT", [D, NMEM], BF16), ("cv", [NMEM, D], BF16), ("hmoe", [L, D], BF16),
            ("affT", [NE, L], F32), ("affc", [L, NE], F32),
        ]:
            T[name] = nc.dram_tensor(name, shp, dty, kind=sk)
        self.T = T
        self.xres_buf = Buf(None, "xres")
        self.hmoe_buf = Buf(None, "hmoe")
        self.afft_buf = Buf(None, "affT")

    def build(self):
        c = self.c
        with ExitStack() as gst:
            self.ident = c.sb(gst, "ident", [128, 128], BF16)
            self.identf = c.sb(gst, "identf", [128, 128], F32)
            self.ones = c.sb(gst, "ones", [128, 128], BF16)
            c.dma("sp", self.ident[:], self.T["ident"].ap(), w=[self.ident], sbuf=self.ident)
            c.dma("sp", self.identf[:], self.T["identf"].ap(), w=[self.identf], sbuf=self.identf)
            c.dma("sp", self.ones[:], self.T["ones"].ap(), w=[self.ones], sbuf=self.ones)
            c.stage_bufs = []
            stages = []
            for l in range(self.nlayers):
                xsrc = self.T["x"] if l == 0 else self.T["xres"]
                stages += [
                    ("inproj%d" % l, lambda l=l, xsrc=xsrc: self.stage_inproj(l, xsrc)),
                    ("na%d" % l, lambda l=l: self.stage_na(l)),
                    ("hy%d" % l, lambda l=l: self.stage_hyena(l)),
                    ("ret%d" % l, lambda l=l: self.stage_ret(l)),
                    ("outproj%d" % l, lambda l=l, xsrc=xsrc: self.stage_outproj(l, xsrc)),
                    ("ckv%d" % l, lambda l=l: self.stage_ckv(l)),
                    ("cq%d" % l, lambda l=l: self.stage_cq(l)),
                    ("cattn%d" % l, lambda l=l: self.stage_cattn(l)),
                    ("co%d" % l, lambda l=l: self.stage_co(l)),
                    ("moeh%d" % l, lambda l=l: self.stage_moe_h(l)),
                    ("moex%d" % l, lambda l=l: self.stage_moe_x(l)),
                ]
            stages.append(("final", self.stage_final))
            for name, fn in stages:
                fn()
                c.end_stage([self.xres_buf, self.hmoe_buf, self.afft_buf])
                if self.stop_after == name:
                    break
            c.barrier()
        return self.nc

    def load_bc(self, st, name, t, off, n, dtype=F32):
        b = self.c.sb(st, name, [128, n], dtype)
        self.c.dma("sp", b[:], row_bc(t, off, n), w=[b], sbuf=b)
        return b

    def rms_to_T(self, xt, gain, hb, xT, col0, ptrs, small, eps=RMS_EPS, width=D, no_T=False):
        c = self.c
        ss, rstd = small
        c.op("act", lambda e: e.activation(out=hb[:], in_=xt[:], func=AF.Square, accum_out=ss[:, 0:1]), r=[xt], w=[hb, ss])
        c.op("act", lambda e: e.activation(out=ss[:, 1:2], in_=ss[:, 0:1], func=AF.Sqrt, scale=1.0 / width, bias=self.epsb[:, 0:1]), r=[ss, self.epsb], w=[ss])
        c.op("dve", lambda e: e.reciprocal(out=rstd[:, 0:1], in_=ss[:, 1:2]), r=[ss], w=[rstd])
        c.op("dve", lambda e: e.scalar_tensor_tensor(out=hb[:], in0=xt[:], scalar=rstd[:, 0:1], in1=gain[:], op0=ALU.mult, op1=ALU.mult), r=[xt, rstd, gain], w=[hb])
        if not no_T:
            self.transpose_into(hb, xT, col0, ptrs)

    def transpose_into(self, hb, xT, col0, ptrs, nk=16):
        c = self.c
        for k4 in range(nk // 4):
            pt = ptrs[k4 % len(ptrs)]
            for q in range(4):
                k = k4 * 4 + q
                c.op("pe", lambda e: e.transpose(pt[:, q, :], hb[:, k * 128:(k + 1) * 128], self.ident[:]),
                     r=[hb, self.ident], w=[pt], signal=(q == 3))
            eng = c.evac_eng()
            copy_on(c, eng, xT[:, k4 * 4:(k4 + 1) * 4, col0:col0 + 128], pt[:, :, :], r=[pt], w=[xT])

    def gemm(self, xT, KC, Tn, w_ap, N, bw, mode, evac, wbufs, pbanks, after_w=None):
        c = self.c
        for nb in range(N // bw):
            wb = wbufs[self.wctr % len(wbufs)]
            self.wctr += 1
            c.dma("pool", wb[:, 0:KC, 0:bw], w_ap[:, nb * bw:(nb + 1) * bw].rearrange("(k p) n -> p k n", p=128), w=[wb], sbuf=wb)
            if after_w is not None:
                after_w(nb)
            if mode == "TM":
                for tt in range(Tn // 128):
                    pb = pbanks[self.pctr % len(pbanks)]
                    self.pctr += 1
                    for k in range(KC):
                        c.op("pe", lambda e: e.matmul(pb[:, 0:bw], xT[:, k, tt * 128:(tt + 1) * 128], wb[:, k, 0:bw], start=(k == 0), stop=(k == KC - 1)),
                             r=[xT, wb], w=[pb], signal=(k == KC - 1))
                    evac(nb, tt, pb)
            else:
                tbs = min(512, Tn)
                for sub in range(bw // 128):
                    for tb in range(Tn // tbs):
                        pb = pbanks[self.pctr % len(pbanks)]
                        self.pctr += 1
                        for k in range(KC):
                            c.op("pe", lambda e: e.matmul(pb[:, 0:tbs], wb[:, k, sub * 128:(sub + 1) * 128], xT[:, k, tb * tbs:(tb + 1) * tbs], start=(k == 0), stop=(k == KC - 1)),
                                 r=[xT, wb], w=[pb], signal=(k == KC - 1))
                        evac(nb * (bw // 128) + sub, tb, pb)


    def run_halves(self, nhalf, A, B, gemm_half):
        for tt in range(8):
            A(0, tt)
            B(0, tt)
        for half in range(nhalf):
            sched = []
            if half + 1 < nhalf:
                hn = half + 1
                sched = [[("A", 0), ("A", 1)], [("B", 0), ("B", 1), ("A", 2), ("A", 3)], [("B", 2), ("B", 3), ("A", 4), ("A", 5)],
                         [("B", 4), ("B", 5), ("A", 6), ("A", 7)], [("B", 6), ("B", 7)]]
            state = [0]

            def hook(_nb, half=half):
                if state[0] < len(sched):
                    for kind, tt in sched[state[0]]:
                        (A if kind == "A" else B)(half + 1, tt)
                    state[0] += 1
            gemm_half(half, hook)
            while state[0] < len(sched):
                hook(0)

    def common_alloc(self, st, nw=3, npb=4, wk=16):
        c = self.c
        self.wctr = 0
        self.pctr = 0
        wbufs = [c.sb(st, "wb%d" % i, [128, wk, 512], BF16) for i in range(nw)]
        pbanks = [c.ps(st, "pb%d" % i, [128, 512], F32) for i in range(npb)]
        self.epsb = c.sb(st, "epsb", [128, 1], F32)
        c.op("dve", lambda e: e.memset(self.epsb[:], RMS_EPS), w=[self.epsb])
        return wbufs, pbanks

    def stage_inproj(self, l, xsrc):
        c, T = self.c, self.T
        with ExitStack() as st:
            wbufs, pbanks = self.common_alloc(st)
            gain = self.load_bc(st, "gain", T["norm_mix"], l * D, D)
            xTs = [c.sb(st, "xT%d" % i, [128, 16, 1024], BF16) for i in range(2)]
            xts = [c.sb(st, "xt%d" % i, [128, D], F32) for i in range(2)]
            hbs = [c.sb(st, "hb%d" % i, [128, D], BF16) for i in range(2)]
            ptrs = [c.ps(st, "ptr%d" % i, [128, 4, 128], BF16) for i in range(2)]
            smalls = [(c.sb(st, "ss%d" % i, [128, 2], F32), c.sb(st, "rs%d" % i, [128, 1], F32)) for i in range(2)]
            stg_bf = [c.sb(st, "sgb%d" % i, [128, 512], BF16) for i in range(4)]
            stg_f = [c.sb(st, "sgf%d" % i, [128, 512], F32) for i in range(3)]
            cnt = [0, 0]
            w_in = T["w_in"].ap()[l]

            def A(half, tt):
                xt = xts[tt % 2]
                c.dma("sp", xt[:], xsrc.ap()[half * 1024 + tt * 128:half * 1024 + (tt + 1) * 128, :], w=[xt], sbuf=xt)
                self.rms_to_T(xt, gain, hbs[tt % 2], None, 0, ptrs, smalls[tt % 2], no_T=True)

            def B(half, tt):
                self.transpose_into(hbs[tt % 2], xTs[half % 2], tt * 128, ptrs)

            def gemm_half(half, hook):
                t0 = half * 1024
                xT = xTs[half % 2]

                def mk_evac(dst, col_off, fm, dtype, scale, bw):
                    def evac(i0, i1, pb):
                        if dtype == BF16:
                            sg = stg_bf[cnt[0] % len(stg_bf)]
                            cnt[0] += 1
                        else:
                            sg = stg_f[cnt[1] % len(stg_f)]
                            cnt[1] += 1
                        eng = c.evac_eng()
                        if fm:
                            copy_on(c, eng, sg[:, 0:512], pb[:, 0:512], r=[pb], w=[sg], scale=scale)
                            c.dma("sp", dst.ap()[i0 * 128:(i0 + 1) * 128, t0 + i1 * 512:t0 + (i1 + 1) * 512], sg[:, 0:512], r=[sg], sbuf=sg)
                        else:
                            copy_on(c, eng, sg[:, 0:bw], pb[:, 0:bw], r=[pb], w=[sg], scale=scale)
                            c.dma("sp", dst.ap()[t0 + i1 * 128:t0 + (i1 + 1) * 128, col_off + i0 * bw:col_off + (i0 + 1) * bw], sg[:, 0:bw], r=[sg], sbuf=sg)
                    return evac
                groups = [
                    (0, 768, "FM", T["naqT"], BF16, 0.125, 384),
                    (768, 768, "FM", T["nakT"], BF16, None, 384),
                    (1536, 768, "TM", T["nav"], BF16, None, 384),
                    (2304, 1536, "TM", T["hyp"], F32, None, 512),
                    (3840, 768, "FM", T["rqT"], BF16, 128 ** -0.5, 384),
                    (4608, 768, "FM", T["rkT"], BF16, None, 384),
                    (5376, 768, "TM", T["rv"], BF16, None, 384),
                    (6144, 768, "TM", T["rg"], F32, None, 384),
                ]
                for (c0, n, mode, dst, dty, scale, bw) in groups:
                    self.gemm(xT, 16, 1024, w_in[:, c0:c0 + n], n, bw, mode, mk_evac(dst, 0, mode == "FM", dty, scale, bw), wbufs, pbanks, after_w=hook)
            self.run_halves(2, A, B, gemm_half)

    def stage_na(self, l):
        c, T = self.c, self.T
        DELTAS = [-6, -4, -2, 0, 2, 4, 6]
        with ExitStack() as st:
            qT = c.sb(st, "qT", [128, 6, L], BF16)
            kT = c.sb(st, "kT", [128, 6, L], BF16)
            vx = c.sb(st, "vx", [128, 16, 12, 65], BF16)
            COMBOS = sorted(set((dl, pat) for i in range(16) for (j, dl, pat) in self.na_pairs[i]))
            bt = c.sb(st, "bt", [128, NA_H * len(COMBOS), 128], BF16)
            bp = [c.sb(st, "bp%d" % i, [128, 2, 64], BF16) for i in range(4)]
            mt = c.sb(st, "mt", [128, self.npat, 128], BF16)
            jm = c.sb(st, "jm", [128, 128], BF16)
            psc = [c.ps(st, "psc%d" % i, [128, 1024], F32) for i in range(3)]
            pso = [c.ps(st, "pso%d" % i, [128, 2, 512], F32) for i in range(1)]
            osb = [c.sb(st, "osb%d" % i, [128, 2, 390], F32) for i in range(2)]
            pT = [c.sb(st, "pT%d" % i, [128, 640], BF16) for i in range(4)]
            rden = [c.sb(st, "rden%d" % i, [128, 12], F32) for i in range(2)]
            yt = [c.sb(st, "yt%d" % i, [128, 768], F32) for i in range(2)]
            c.dma("sp", qT[:], T["naqT"].ap().rearrange("(k p) t -> p k t", p=128), w=[qT], sbuf=qT)
            c.dma("sp", kT[:], T["nakT"].ap().rearrange("(k p) t -> p k t", p=128), w=[kT], sbuf=kT)
            c.op("pool", lambda e: e.memset(vx[:], 1.0), w=[vx])
            for j in range(16):
                c.dma("sp", vx[:, j, :, 0:64], T["nav"].ap()[j * 128:(j + 1) * 128, :].rearrange("p (h d) -> p h d", d=64), w=[vx], sbuf=vx)
            c.dma("sp", mt[:], T["namask"].ap(), w=[mt], sbuf=mt)
            c.dma("sp", jm[:], T["naJ"].ap(), w=[jm], sbuf=jm)
            n = 0
            for h in range(NA_H):
                for ci, (dl, pat) in enumerate(COMBOS):
                    slot = h * len(COMBOS) + ci
                    b = bp[n % 4]
                    n += 1
                    for a in range(2):
                        m0 = 7 - dl - a
                        off = ((l * NA_H + h) * 15 + m0) * 128
                        src = bass.AP(T["rpbY"], off, [[1, 64], [128, 2], [1, 64]])
                        c.dma("pool", b[a * 64:(a + 1) * 64, :, :], src, w=[b], sbuf=b)
                    pbb = psc[n % 2]
                    c.op("pe", lambda e: e.matmul(pbb[:, 0:128], jm[:], b[:].rearrange("p b q -> p (b q)"), start=True, stop=False), r=[jm, b], w=[pbb], signal=False)
                    c.op("pe", lambda e: e.matmul(pbb[:, 0:128], self.ident[:], mt[:, pat, :], start=False, stop=True), r=[self.ident, mt], w=[pbb])
                    copy_on(c, c.evac_eng(), bt[:, slot, :], pbb[:, 0:128], r=[pbb], w=[bt])
            n = 0
            pend = []

            def epilogue(i, po):
                rd = rden[i % 2]
                y = yt[i % 2]
                ob = osb[i % 2]
                c.op("act", lambda e: e.copy(out=ob[:, 0, :], in_=po[:, 0, 0:390]), r=[po], w=[ob])
                c.op("dve", lambda e: e.tensor_copy(out=ob[:, 1, :], in_=po[:, 1, 0:390]), r=[po], w=[ob])
                for g in range(2):
                    c.op("dve", lambda e: e.reciprocal(out=rd[:, g * 6:(g + 1) * 6], in_=ob[:, g, :].rearrange("p (h d) -> p h d", d=65)[:, :, 64]), r=[ob], w=[rd])
                for h in range(NA_H):
                    src = ob[:, h // 6, (h % 6) * 65:(h % 6) * 65 + 64]
                    if h % 2 == 0:
                        c.op("dve", lambda e: e.tensor_scalar(out=y[:, h * 64:(h + 1) * 64], in0=src, scalar1=rd[:, h:h + 1], scalar2=None, op0=ALU.mult), r=[ob, rd], w=[y])
                    else:
                        c.op("pool", lambda e: e.tensor_scalar(out=y[:, h * 64:(h + 1) * 64], in0=src, scalar1=rd[:, h:h + 1], scalar2=None, op0=ALU.mult), r=[ob, rd], w=[y])
                c.dma("sp", T["ymix"].ap()[i * 128:(i + 1) * 128, 0:768], y[:], r=[y], sbuf=y)

            for i in range(16):
                pairs = self.na_pairs[i]
                nk = len(pairs)
                po = pso[0]
                for h in range(NA_H):
                    hp, off = h // 2, (h % 2) * 64
                    ps = psc[n % 3]
                    pt_ = pT[n % 4]
                    n += 1
                    for jj, (j, dl, pat) in enumerate(pairs):
                        reg = ps[:, jj * 128:(jj + 1) * 128]
                        slot = h * len(COMBOS) + COMBOS.index((dl, pat))
                        c.op("pe", lambda e: e.matmul(reg, kT[off:off + 64, hp, j * 128:(j + 1) * 128], qT[off:off + 64, hp, i * 128:(i + 1) * 128], start=True, stop=False),
                             r=[kT, qT], w=[ps], signal=False)
                        c.op("pe", lambda e: e.matmul(reg, self.ident[:], bt[:, slot, :], start=False, stop=True), r=[self.ident, bt], w=[ps], signal=(jj == nk - 1))
                    c.op("act", lambda e: e.activation(out=pt_[:, 0:nk * 128], in_=ps[:, 0:nk * 128], func=AF.Exp), r=[ps], w=[pt_])

                    def pv(i=i, h=h, pairs=pairs, nk=nk, pt_=pt_, po=po):
                        oreg = po[:, h // 6, (h % 6) * 65:(h % 6) * 65 + 65]
                        for jj, (j, dl, pat) in enumerate(pairs):
                            c.op("pe", lambda e: e.matmul(oreg, pt_[:, jj * 128:(jj + 1) * 128], vx[:, j, h, :], start=(jj == 0), stop=(jj == nk - 1)),
                                 r=[pt_, vx], w=[po], signal=(jj == nk - 1))
                        if h == NA_H - 1:
                            epilogue(i, po)
                    if len(pend) >= 2:
                        pend.pop(0)()
                    pend.append(pv)
            while pend:
                pend.pop(0)()

    def stage_hyena(self, l):
        c, T = self.c, self.T
        with ExitStack() as st:
            z = [c.sb(st, "z%d" % i, [128, 16, 512], BF16) for i in range(2)]
            h2b = c.sb(st, "h2b", [64, L], BF16)
            w3b = c.sb(st, "w3b", [64, 2048], BF16)
            dsk = [self.load_bc(st, "dsk%d" % o, T["hy_skip_d"], (l * 2 + o) * 512, 512) for o in range(2)]
            c.dma("pool", w3b[:], T["hy_filt_w3"].ap()[l], w=[w3b], sbuf=w3b)
            with ExitStack() as s1:
                cw = [self.load_bc(s1, "cw%d" % k, T["hy_conv_w"], (l * 3 + k) * 1536, 1536) for k in range(3)]
                cb = self.load_bc(s1, "cb", T["hy_conv_b"], l * 1536, 1536)
                pm = [c.sb(s1, "pm%d" % i, [128, 1536], F32) for i in range(2)]
                p0 = [c.sb(s1, "p0%d" % i, [128, 1536], F32) for i in range(2)]
                pp = [c.sb(s1, "pp%d" % i, [128, 1536], F32) for i in range(2)]
                hyp = T["hyp"].ap()
                for tt in range(16):
                    a, b, d = pm[tt % 2], p0[tt % 2], pp[tt % 2]
                    r0 = tt * 128
                    if tt == 0:
                        c.op("dve", lambda e: e.memset(a[:], 0.0), w=[a])
                        c.dma("sp", a[1:128, :], hyp[0:127, :], w=[a], sbuf=a)
                    else:
                        c.dma("sp", a[:], hyp[r0 - 1:r0 + 127, :], w=[a], sbuf=a)
                    c.dma("sp", b[:], hyp[r0:r0 + 128, :], w=[b], sbuf=b)
                    if tt == 15:
                        c.op("dve", lambda e: e.memset(d[:], 0.0), w=[d])
                        c.dma("sp", d[0:127, :], hyp[r0 + 1:r0 + 128, :], w=[d], sbuf=d)
                    else:
                        c.dma("sp", d[:], hyp[r0 + 1:r0 + 129, :], w=[d], sbuf=d)
                    c.op("pool", lambda e: e.tensor_tensor(out=a[:], in0=a[:], in1=cw[0][:], op=ALU.mult), r=[a, cw[0]], w=[a])
                    c.op("dve", lambda e: e.tensor_tensor(out=b[:], in0=b[:], in1=cw[1][:], op=ALU.mult), r=[b, cw[1]], w=[b])
                    c.op("pool", lambda e: e.tensor_tensor(out=d[:], in0=d[:], in1=cw[2][:], op=ALU.mult), r=[d, cw[2]], w=[d])
                    c.op("dve", lambda e: e.tensor_tensor(out=b[:], in0=b[:], in1=cb[:], op=ALU.add), r=[b, cb], w=[b])
                    c.op("pool", lambda e: e.tensor_tensor(out=a[:], in0=a[:], in1=d[:], op=ALU.add), r=[a, d], w=[a])
                    c.op("dve", lambda e: e.tensor_tensor(out=b[:, 0:1024], in0=b[:, 0:1024], in1=a[:, 0:1024], op=ALU.add), r=[a, b], w=[b])
                    c.op("dve", lambda e: e.tensor_tensor(out=z[0][:, tt, :], in0=b[:, 1024:1536], in1=a[:, 1024:1536], op=ALU.add), r=[a, b], w=[z[0]])
                    c.dma("act", T["hyx"].ap()[r0:r0 + 128, :], b[:, 0:1024], r=[b], sbuf=b)
                fT = c.sb(s1, "fT", [17, L], F32)
                w1 = c.sb(s1, "w1", [17, 64], F32)
                w2 = c.sb(s1, "w2", [64, 64], F32)
                cols = c.sb(s1, "cols", [64, 6], F32)
                h1 = c.sb(s1, "h1", [64, L], F32)
                pre = [c.sb(s1, "pre%d" % i, [64, 512], F32) for i in range(2)]
                tmp = [c.sb(s1, "tmpm%d" % i, [64, 512], F32) for i in range(2)]
                pm_ = [c.ps(s1, "pmlp%d" % i, [64, 512], F32) for i in range(2)]
                c.dma("sp", fT[:], T["featsT"].ap(), w=[fT], sbuf=fT)
                c.dma("sp", w1[:], T["hy_filt_w1"].ap()[l], w=[w1], sbuf=w1)
                c.dma("sp", w2[:], T["hy_filt_w2"].ap()[l], w=[w2], sbuf=w2)
                c.dma("sp", cols[:, 0:1], T["hy_sin_freq"].ap()[l].rearrange("(p o) -> p o", o=1), w=[cols], sbuf=cols)
                c.dma("sp", cols[:, 1:2], T["hy_filt_b1"].ap()[l].rearrange("(p o) -> p o", o=1), w=[cols], sbuf=cols)
                c.dma("sp", cols[:, 2:3], T["hy_filt_b2"].ap()[l].rearrange("(p o) -> p o", o=1), w=[cols], sbuf=cols)
                c.op("dve", lambda e: e.tensor_tensor(out=cols[:, 3:4], in0=cols[:, 0:1], in1=cols[:, 1:2], op=ALU.mult), r=[cols], w=[cols])
                c.op("dve", lambda e: e.tensor_tensor(out=cols[:, 4:5], in0=cols[:, 0:1], in1=cols[:, 2:3], op=ALU.mult), r=[cols], w=[cols])

                def sin_layer(wt, kdim, src, dst, fbcol):
                    for tb in range(4):
                        pmm = pm_[tb % 2]
                        x_ = pre[tb % 2]
                        t_ = tmp[tb % 2]
                        c.op("pe", lambda e: e.matmul(pmm[:], wt[0:kdim, :], src[0:kdim, tb * 512:(tb + 1) * 512], start=True, stop=True), r=[wt, src], w=[pmm])
                        c.op("dve", lambda e: e.tensor_scalar(out=x_[:], in0=pmm[:], scalar1=cols[:, 0:1], scalar2=cols[:, fbcol:fbcol + 1], op0=ALU.mult, op1=ALU.add), r=[pmm, cols], w=[x_])
                        c.op("dve", lambda e: e.tensor_scalar(out=t_[:], in0=x_[:], scalar1=PI, scalar2=-2 * PI, op0=ALU.is_gt, op1=ALU.mult), r=[x_], w=[t_])
                        c.op("dve", lambda e: e.tensor_tensor(out=x_[:], in0=x_[:], in1=t_[:], op=ALU.add), r=[x_, t_], w=[x_])
                        c.op("dve", lambda e: e.tensor_scalar(out=t_[:], in0=x_[:], scalar1=-PI, scalar2=2 * PI, op0=ALU.is_lt, op1=ALU.mult), r=[x_], w=[t_])
                        c.op("dve", lambda e: e.tensor_tensor(out=x_[:], in0=x_[:], in1=t_[:], op=ALU.add), r=[x_, t_], w=[x_])
                        c.op("dve", lambda e: e.tensor_scalar(out=x_[:], in0=x_[:], scalar1=3.1415925, scalar2=-3.1415925, op0=ALU.min, op1=ALU.max), r=[x_], w=[x_])
                        c.op("act", lambda e: e.activation(out=dst[:, tb * 512:(tb + 1) * 512], in_=x_[:], func=AF.Sin), r=[x_], w=[dst])
                sin_layer(w1, 17, fT, h1, 3)
                sin_layer(w2, 64, h1, h2b, 4)
            c.barrier()
            with ExitStack() as s2:
                ksum = c.sb(s2, "ksum", [128, 16, 512], BF16)
                kdif = c.sb(s2, "kdif", [128, 16, 512], BF16)
                Pr = c.sb(s2, "Pr", [128, 16, 512], BF16)
                Pi = c.sb(s2, "Pi", [128, 16, 512], BF16)
                tabs = [(c.sb(s2, "tc%d" % i, [128, 16, 128], BF16), c.sb(s2, "ts%d" % i, [128, 16, 128], BF16)) for i in range(2)]
                pk = [c.ps(s2, "pk%d" % i, [128, 512], F32) for i in range(4)]
                pinv = [c.ps(s2, "pinv%d" % i, [128, 512], F32) for i in range(2)]
                pf = c.ps(s2, "pf", [128, 2, 512], F32)
                win = [c.sb(s2, "win%d" % i, [128, 512], F32) for i in range(2)]
                ff = [c.sb(s2, "ff%d" % i, [128, 512], F32) for i in range(2)]
                fb = [c.sb(s2, "fb%d" % i, [128, 512], F32) for i in range(2)]
                ksb = [c.sb(s2, "ksb%d" % i, [128, 2, 512], F32) for i in range(2)]
                ta = [c.sb(s2, "ta%d" % i, [128, 512], F32) for i in range(2)]
                tb_ = [c.sb(s2, "tbb%d" % i, [128, 512], F32) for i in range(2)]
                xg = [c.sb(s2, "xg%d" % i, [128, 512], F32) for i in range(2)]
                og = [c.sb(s2, "og%d" % i, [128, 512], F32) for i in range(2)]
                for o in range(2):
                    zin = z[o]
                    for tt in range(16):
                        w_ = win[tt % 2]
                        f_, b_ = ff[tt % 2], fb[tt % 2]
                        c.dma("sp", w_[:], T["window"].ap()[tt * 128:(tt + 1) * 128, :], w=[w_], sbuf=w_)
                        for dr in range(2):
                            c.op("pe", lambda e: e.matmul(pf[:, dr, :], h2b[:, tt * 128:(tt + 1) * 128], w3b[:, (o * 2 + dr) * 512:(o * 2 + dr + 1) * 512], start=True, stop=True),
                                 r=[h2b, w3b], w=[pf], signal=(dr == 1))
                        c.op("dve", lambda e: e.tensor_tensor(out=f_[:], in0=pf[:, 0, :], in1=w_[:], op=ALU.mult), r=[pf, w_], w=[f_])
                        c.op("dve", lambda e: e.tensor_tensor(out=b_[:], in0=pf[:, 1, :], in1=w_[:], op=ALU.mult), r=[pf, w_], w=[b_])
                        if tt == 0:
                            c.op("dve", lambda e: e.memset(b_[0:1, :], 0.0), w=[b_])
                        c.op("pool", lambda e: e.tensor_tensor(out=ksum[:, tt, :], in0=f_[:], in1=b_[:], op=ALU.add), r=[f_, b_], w=[ksum])
                        c.op("dve", lambda e: e.tensor_tensor(out=kdif[:, tt, :], in0=b_[:], in1=f_[:], op=ALU.subtract), r=[f_, b_], w=[kdif])
                    for fc in range(16):
                        tcb, tsb = tabs[fc % 2]
                        c.dma("sp", tcb[:], T["dftC"].ap()[fc], w=[tcb], sbuf=tcb)
                        c.dma("sp", tsb[:], T["dftS"].ap()[fc], w=[tsb], sbuf=tsb)
                        for gi, (tab, rhs) in enumerate([(tcb, ksum), (tsb, kdif), (tcb, zin), (tsb, zin)]):
                            for k in range(16):
                                c.op("pe", lambda e: e.matmul(pk[gi][:], tab[:, k, :], rhs[:, k, :], start=(k == 0), stop=(k == 15)), r=[tab, rhs], w=[pk[gi]], signal=(k == 15))
                        ks = ksb[fc % 2]
                        c.op("act", lambda e: e.copy(out=ks[:, 0, :], in_=pk[0][:]), r=[pk[0]], w=[ks])
                        c.op("act", lambda e: e.copy(out=ks[:, 1, :], in_=pk[1][:]), r=[pk[1]], w=[ks])
                        q0, q1, q2, q3 = ta[0], ta[1], tb_[0], tb_[1]
                        c.op("dve", lambda e: e.tensor_tensor(out=q0[:], in0=pk[2][:], in1=ks[:, 0, :], op=ALU.mult), r=[pk[2], ks], w=[q0])
                        c.op("dve", lambda e: e.tensor_tensor(out=q1[:], in0=pk[2][:], in1=ks[:, 1, :], op=ALU.mult), r=[pk[2], ks], w=[q1])
                        c.op("dve", lambda e: e.tensor_tensor(out=q2[:], in0=pk[3][:], in1=ks[:, 1, :], op=ALU.mult), r=[pk[3], ks], w=[q2])
                        c.op("dve", lambda e: e.tensor_tensor(out=q3[:], in0=pk[3][:], in1=ks[:, 0, :], op=ALU.mult), r=[pk[3], ks], w=[q3])
                        c.op("pool", lambda e: e.tensor_tensor(out=Pr[:, fc, :], in0=q0[:], in1=q2[:], op=ALU.add), r=[q0, q2], w=[Pr])
                        c.op("pool", lambda e: e.tensor_tensor(out=Pi[:, fc, :], in0=q1[:], in1=q3[:], op=ALU.subtract), r=[q1, q3], w=[Pi])
                    for tt in range(16):
                        tcb, tsb = tabs[tt % 2]
                        c.dma("sp", tcb[:], T["dftCT"].ap()[tt], w=[tcb], sbuf=tcb)
                        c.dma("sp", tsb[:], T["dftnST"].ap()[tt], w=[tsb], sbuf=tsb)
                        pv = pinv[tt % 2]
                        for k in range(16):
                            c.op("pe", lambda e: e.matmul(pv[:], tcb[:, k, :], Pr[:, k, :], start=(k == 0), stop=False), r=[tcb, Pr], w=[pv], signal=False)
                        for k in range(16):
                            c.op("pe", lambda e: e.matmul(pv[:], tsb[:, k, :], Pi[:, k, :], start=False, stop=(k == 15)), r=[tsb, Pi], w=[pv], signal=(k == 15))
                        x_ = xg[tt % 2]
                        a_ = ta[tt % 2]
                        c.dma("sp", x_[:], T["hyx"].ap()[tt * 128:(tt + 1) * 128, o * 512:(o + 1) * 512], w=[x_], sbuf=x_)
                        c.op("pool", lambda e: e.tensor_tensor(out=a_[:], in0=zin[:, tt, :], in1=dsk[o][:], op=ALU.mult), r=[zin, dsk[o]], w=[a_])
                        c.op("dve", lambda e: e.scalar_tensor_tensor(out=a_[:], in0=pv[:], scalar=1.0 / L, in1=a_[:], op0=ALU.mult, op1=ALU.add), r=[pv, a_], w=[a_])
                        if o == 0:
                            c.op("dve", lambda e: e.tensor_tensor(out=z[1][:, tt, :], in0=a_[:], in1=x_[:], op=ALU.mult), r=[a_, x_], w=[z[1]])
                        else:
                            o_ = og[tt % 2]
                            c.op("dve", lambda e: e.tensor_tensor(out=o_[:], in0=a_[:], in1=x_[:], op=ALU.mult), r=[a_, x_], w=[o_])
                            c.dma("act", T["ymix"].ap()[tt * 128:(tt + 1) * 128, 768:1280], o_[:], r=[o_], sbuf=o_)

    def stage_ret(self, l):
        c, T = self.c, self.T
        lgf, lgb = self.consts["_lgf"], self.consts["_lgb"]
        with ExitStack() as st:
            rc = c.sb(st, "rc", [128, L], F32)
            rs_ = c.sb(st, "rs", [128, L], F32)
            rR = c.sb(st, "rR", [128, 128], BF16)
            E = c.sb(st, "E", [128, 512], F32)
            gne = c.sb(st, "gne", [128, 1], F32)
            c.op("dve", lambda e: e.memset(gne[:], GN_EPS), w=[gne])
            c.dma("sp", rc[:], T["rotcos"].ap(), w=[rc], sbuf=rc)
            c.dma("sp", rs_[:], T["rotsin"].ap(), w=[rs_], sbuf=rs_)
            c.dma("sp", rR[:], T["rotR"].ap(), w=[rR], sbuf=rR)
            c.dma("sp", E[:], T["retE"].ap(), w=[E], sbuf=E)
            raw = [c.sb(st, "raw%d" % i, [128, L], BF16) for i in range(2)]
            qk = [[c.sb(st, "qk%d_%d" % (i, j), [128, L], BF16) for j in range(2)] for i in range(2)]
            vh = [c.sb(st, "vh%d" % i, [128, 16, 128], BF16) for i in range(2)]
            dg = [c.sb(st, "dg%d" % i, [128, 4, 512], BF16) for i in range(2)]
            prot = [c.ps(st, "prot%d" % i, [128, 512], F32) for i in range(2)]
            pss = [c.ps(st, "pss%d" % i, [128, 512], F32) for i in range(2)]
            psy = [c.ps(st, "psy%d" % i, [128, 512], F32) for i in range(2)]
            ptt = [c.ps(st, "ptt%d" % i, [128, 4, 128], F32) for i in range(2)]
            t1 = [c.sb(st, "t1_%d" % i, [128, 512], F32) for i in range(2)]
            t2 = [c.sb(st, "t2_%d" % i, [128, 512], F32) for i in range(2)]
            dec = [c.sb(st, "dec%d" % i, [128, 512], BF16) for i in range(3)]
            pT = [c.sb(st, "pT%d" % i, [128, 512], BF16) for i in range(3)]
            yT = [c.sb(st, "yT%d" % i, [128, 512], F32) for i in range(2)]
            gt = [c.sb(st, "gt%d" % i, [128, 4, 128], F32) for i in range(2)]
            sg = [c.sb(st, "sg%d" % i, [128, 4, 128], F32) for i in range(2)]
            yo = [c.sb(st, "yo%d" % i, [128, 4, 128], F32) for i in range(2)]
            stt = [c.sb(st, "stt%d" % i, [128, 4, 6], F32) for i in range(2)]
            mv = [c.sb(st, "mv%d" % i, [128, 4, 4], F32) for i in range(2)]
            n = 0
            pend = []
            ypend = []
            for h in range(6):
                hb = h % 2
                for wi, src in enumerate([T["rqT"], T["rkT"]]):
                    rw = raw[wi]
                    dstb = qk[hb][wi]
                    c.dma("sp", rw[:], src.ap()[h * 128:(h + 1) * 128, :], w=[rw], sbuf=rw)
                    for tb in range(4):
                        sl = slice(tb * 512, (tb + 1) * 512)
                        pr = prot[tb % 2]
                        a_, b_ = t1[tb % 2], t2[tb % 2]
                        c.op("pe", lambda e: e.matmul(pr[:], rR[:], rw[:, sl], start=True, stop=True), r=[rR, rw], w=[pr])
                        c.op("dve", lambda e: e.tensor_tensor(out=a_[:], in0=pr[:], in1=rs_[:, sl], op=ALU.mult), r=[pr, rs_], w=[a_])
                        c.op("pool", lambda e: e.tensor_tensor(out=b_[:], in0=rw[:, sl], in1=rc[:, sl], op=ALU.mult), r=[rw, rc], w=[b_])
                        c.op("pool", lambda e: e.tensor_tensor(out=dstb[:, sl], in0=a_[:], in1=b_[:], op=ALU.add), r=[a_, b_], w=[dstb])
                qr, kr = qk[hb]
                v_ = vh[hb]
                d_ = dg[hb]
                c.dma("sp", v_[:], T["rv"].ap()[:, h * 128:(h + 1) * 128].rearrange("(j p) d -> p j d", p=128), w=[v_], sbuf=v_)
                c.dma("sp", d_[:], T["retD"].ap()[h], w=[d_], sbuf=d_)
                decF, decB = dec[0], dec[1]
                c.op("act", lambda e: e.activation(out=decF[:], in_=E[:], func=AF.Exp, scale=float(lgf[h])), r=[E], w=[decF])
                c.op("act", lambda e: e.activation(out=decB[:], in_=E[:], func=AF.Exp, scale=float(-lgb[h])), r=[E], w=[decB])
                for ib in range(4):
                    py = psy[ib % 2]
                    for j in range(16):
                        ps = pss[n % 2]
                        p_ = pT[n % 3]
                        n += 1
                        c.op("pe", lambda e: e.matmul(ps[:], kr[:, j * 128:(j + 1) * 128], qr[:, ib * 512:(ib + 1) * 512], start=True, stop=True), r=[kr, qr], w=[ps])
                        offv = ib * 512 - j * 128
                        if 0 <= j - ib * 4 < 4:
                            decap = d_[:, j - ib * 4, :]
                            c.op("dve", lambda e: e.tensor_tensor(out=p_[:], in0=ps[:], in1=decap, op=ALU.mult), r=[ps, d_], w=[p_])
                        else:
                            if offv > 0:
                                de, fac = decF, math.exp(float(lgf[h]) * offv)
                            else:
                                de, fac = decB, math.exp(float(-lgb[h]) * offv)
                            c.op("dve", lambda e: e.scalar_tensor_tensor(out=p_[:], in0=ps[:], scalar=float(fac), in1=de[:], op0=ALU.mult, op1=ALU.mult), r=[ps, de], w=[p_])
                        def ymm(py=py, v_=v_, j=j, p_=p_):
                            c.op("pe", lambda e: e.matmul(py[:], v_[:, j, :], p_[:], start=(j == 0), stop=(j == 15)), r=[v_, p_], w=[py], signal=(j == 15))
                        if ypend:
                            ypend.pop(0)()
                        ypend.append(ymm)
                    while ypend:
                        ypend.pop(0)()
                    y_ = yT[ib % 2]
                    c.op("act", lambda e: e.copy(out=y_[:], in_=py[:]), r=[py], w=[y_])

                    def epilogue(h=h, ib=ib, y_=y_):
                        k_ = (ib + 4 * h) % 2
                        pt4 = ptt[k_]
                        for q in range(4):
                            c.op("pe", lambda e: e.transpose(pt4[:, q, :], y_[:, q * 128:(q + 1) * 128], self.identf[:]), r=[y_, self.identf], w=[pt4], signal=(q == 3))
                        g_, s_, o_, st_, m_ = gt[k_], sg[k_], yo[k_], stt[k_], mv[k_]
                        rows = slice(ib * 512, (ib + 1) * 512)
                        c.dma("sp", g_[:], T["rg"].ap()[rows, h * 128:(h + 1) * 128].rearrange("(q p) d -> p q d", p=128), w=[g_], sbuf=g_)
                        c.op("act", lambda e: e.activation(out=s_[:], in_=g_[:], func=AF.Silu), r=[g_], w=[s_])
                        for q in range(4):
                            c.op("dve", lambda e: e.bn_stats(out=st_[:, q, :], in_=pt4[:, q, :]), r=[pt4], w=[st_])
                        for q in range(4):
                            c.op("dve", lambda e: e.bn_aggr(out=m_[:, q, 0:2], in_=st_[:, q, :]), r=[st_], w=[m_])
                        c.op("act", lambda e: e.activation(out=m_[:, :, 2], in_=m_[:, :, 1], func=AF.Sqrt, bias=gne[:, 0:1]), r=[m_, gne], w=[m_])
                        c.op("dve", lambda e: e.reciprocal(out=m_[:, :, 3], in_=m_[:, :, 2]), r=[m_], w=[m_])
                        c.op("dve", lambda e: e.tensor_tensor(out=o_[:], in0=pt4[:], in1=m_[:, :, 0:1].to_broadcast([128, 4, 128]), op=ALU.subtract), r=[pt4, m_], w=[o_])
                        c.op("dve", lambda e: e.tensor_tensor(out=o_[:], in0=o_[:], in1=m_[:, :, 3:4].to_broadcast([128, 4, 128]), op=ALU.mult), r=[o_, m_], w=[o_])
                        c.op("pool", lambda e: e.tensor_tensor(out=o_[:], in0=o_[:], in1=s_[:], op=ALU.mult), r=[o_, s_], w=[o_])
                        c.dma("pool", T["ymix"].ap()[rows, 1280 + h * 128:1280 + (h + 1) * 128].rearrange("(q p) d -> p q d", p=128), o_[:], r=[o_], sbuf=o_)
                    if pend:
                        pend.pop(0)()
                    pend.append(epilogue)
            while pend:
                pend.pop(0)()

    def ret_bias(self, st, val):
        c = self.c
        key = (id(st), round(val, 9))
        if not hasattr(self, "_rb"):
            self._rb = {}
        if key not in self._rb:
            b = c.sb(st, "rb%d" % len(self._rb), [128, 1], F32)
            c.op("pool", lambda e: e.memset(b[:], float(val)), w=[b])
            self._rb[key] = b
        return self._rb[key]

    def stage_outproj(self, l, xsrc):
        c, T = self.c, self.T
        with ExitStack() as st:
            wbufs, pbanks = self.common_alloc(st)
            gain = self.load_bc(st, "gain", T["branch_norm"], l * D, D)
            ginvw = c.sb(st, "ginvw", [128, 3], F32)
            c.dma("sp", ginvw[:], T["ginvw"].ap(), w=[ginvw], sbuf=ginvw)
            xTs = [c.sb(st, "xT%d" % i, [128, 16, 1024], BF16) for i in range(2)]
            xts = [c.sb(st, "xt%d" % i, [128, D], F32) for i in range(2)]
            hbs = [c.sb(st, "hb%d" % i, [128, D], BF16) for i in range(2)]
            ptrs = [c.ps(st, "ptr%d" % i, [128, 4, 128], BF16) for i in range(2)]
            sm = [c.sb(st, "sm%d" % i, [128, 12], F32) for i in range(2)]
            xs = [c.sb(st, "xs%d" % i, [128, 512], F32) for i in range(4)]
            so = [c.sb(st, "so%d" % i, [128, 512], F32) for i in range(4)]
            cnt = [0]
            segs = [(0, 768), (768, 1280), (1280, 2048)]

            def A(half, tt):
                t0 = half * 1024
                xt, hb, s_ = xts[tt % 2], hbs[tt % 2], sm[tt % 2]
                c.dma("sp", xt[:], T["ymix"].ap()[t0 + tt * 128:t0 + (tt + 1) * 128, :], w=[xt], sbuf=xt)
                for g, (a, b) in enumerate(segs):
                    c.op("act", lambda e: e.activation(out=hb[:, a:b], in_=xt[:, a:b], func=AF.Square, accum_out=s_[:, g:g + 1]), r=[xt], w=[hb, s_])
                c.op("dve", lambda e: e.tensor_tensor(out=s_[:, 3:6], in0=s_[:, 0:3], in1=ginvw[:], op=ALU.mult), r=[s_, ginvw], w=[s_])
                c.op("act", lambda e: e.activation(out=s_[:, 6:9], in_=s_[:, 3:6], func=AF.Sqrt, bias=self.epsb[:, 0:1]), r=[s_, self.epsb], w=[s_])
                c.op("dve", lambda e: e.reciprocal(out=s_[:, 9:12], in_=s_[:, 6:9]), r=[s_], w=[s_])
                for g, (a, b) in enumerate(segs):
                    c.op("dve", lambda e: e.scalar_tensor_tensor(out=hb[:, a:b], in0=xt[:, a:b], scalar=s_[:, 9 + g:10 + g], in1=gain[:, a:b], op0=ALU.mult, op1=ALU.mult), r=[xt, s_, gain], w=[hb])

            def B(half, tt):
                self.transpose_into(hbs[tt % 2], xTs[half % 2], tt * 128, ptrs)

            def gemm_half(half, hook):
                self.gemm(xTs[half % 2], 16, 1024, T["w_out"].ap()[l], D, 512, "TM", self.mk_resid_evac(xsrc, half * 1024, xs, so, cnt), wbufs, pbanks, after_w=hook)
            self.run_halves(2, A, B, gemm_half)

    def mk_resid_evac(self, xsrc, t0, xs, so, cnt):
        c, T = self.c, self.T

        def evac(nb, tt, pb):
            x_ = xs[cnt[0] % len(xs)]
            o_ = so[cnt[0] % len(so)]
            cnt[0] += 1
            rows = slice(t0 + tt * 128, t0 + (tt + 1) * 128)
            cols = slice(nb * 512, (nb + 1) * 512)
            c.dma("act", x_[:], xsrc.ap()[rows, cols], r=[self.xres_buf] if xsrc is T["xres"] else [], w=[x_], sbuf=x_)
            c.op("dve", lambda e: e.tensor_tensor(out=o_[:], in0=pb[:], in1=x_[:], op=ALU.add), r=[pb, x_], w=[o_])
            c.dma("sp", T["xres"].ap()[rows, cols], o_[:], r=[o_], sbuf=o_)
        return evac

    def stage_ckv(self, l):
        c, T = self.c, self.T
        with ExitStack() as st:
            wbufs, pbanks = self.common_alloc(st)
            gain = self.load_bc(st, "gain", T["mem_norm"], 0, D)
            xT = c.sb(st, "xT", [128, 16, 256], BF16)
            xts = [c.sb(st, "xt%d" % i, [128, D], F32) for i in range(2)]
            hbs = [c.sb(st, "hb%d" % i, [128, D], BF16) for i in range(2)]
            ptrs = [c.ps(st, "ptr%d" % i, [128, 4, 128], BF16) for i in range(2)]
            smalls = [(c.sb(st, "ss%d" % i, [128, 2], F32), c.sb(st, "rs%d" % i, [128, 1], F32)) for i in range(2)]
            sg = [c.sb(st, "sg%d" % i, [128, 512], BF16) for i in range(4)]
            cnt = [0]
            for tt in range(2):
                xt = xts[tt]
                c.dma("sp", xt[:], T["mem"].ap()[tt * 128:(tt + 1) * 128, :], w=[xt], sbuf=xt)
                self.rms_to_T(xt, gain, hbs[tt], xT, tt * 128, ptrs, smalls[tt])

            def evac_k(fc, tb, pb):
                s_ = sg[cnt[0] % 4]
                cnt[0] += 1
                copy_on(c, c.evac_eng(), s_[:, 0:256], pb[:, 0:256], r=[pb], w=[s_])
                c.dma("sp", T["ckT"].ap()[fc * 128:(fc + 1) * 128, :], s_[:, 0:256], r=[s_], sbuf=s_)

            def evac_v(nb, tt, pb):
                s_ = sg[cnt[0] % 4]
                cnt[0] += 1
                copy_on(c, c.evac_eng(), s_[:], pb[:], r=[pb], w=[s_])
                c.dma("sp", T["cv"].ap()[tt * 128:(tt + 1) * 128, nb * 512:(nb + 1) * 512], s_[:], r=[s_], sbuf=s_)
            wk = T["w_ckv"].ap()[l]
            self.gemm(xT, 16, 256, wk[:, 0:D], D, 512, "FM", evac_k, wbufs, pbanks)
            self.gemm(xT, 16, 256, wk[:, D:2 * D], D, 512, "TM", evac_v, wbufs, pbanks)

    def stage_cq(self, l):
        c, T = self.c, self.T
        with ExitStack() as st:
            wbufs, pbanks = self.common_alloc(st)
            gain = self.load_bc(st, "gain", T["norm_cross"], l * D, D)
            xTs = [c.sb(st, "xT%d" % i, [128, 16, 1024], BF16) for i in range(2)]
            xts = [c.sb(st, "xt%d" % i, [128, D], F32) for i in range(2)]
            hbs = [c.sb(st, "hb%d" % i, [128, D], BF16) for i in range(2)]
            ptrs = [c.ps(st, "ptr%d" % i, [128, 4, 128], BF16) for i in range(2)]
            smalls = [(c.sb(st, "ss%d" % i, [128, 2], F32), c.sb(st, "rs%d" % i, [128, 1], F32)) for i in range(2)]
            sg = [c.sb(st, "sg%d" % i, [128, 512], BF16) for i in range(4)]
            cnt = [0]

            def A(half, tt):
                xt = xts[tt % 2]
                c.dma("sp", xt[:], T["xres"].ap()[half * 1024 + tt * 128:half * 1024 + (tt + 1) * 128, :], w=[xt], sbuf=xt)
                self.rms_to_T(xt, gain, hbs[tt % 2], None, 0, ptrs, smalls[tt % 2], no_T=True)

            def B(half, tt):
                self.transpose_into(hbs[tt % 2], xTs[half % 2], tt * 128, ptrs)

            def gemm_half(half, hook):
                t0 = half * 1024

                def evac(fc, tb, pb):
                    s_ = sg[cnt[0] % 4]
                    cnt[0] += 1
                    copy_on(c, c.evac_eng(), s_[:], pb[:], r=[pb], w=[s_], scale=512 ** -0.5)
                    c.dma("sp", T["cqT"].ap()[fc * 128:(fc + 1) * 128, t0 + tb * 512:t0 + (tb + 1) * 512], s_[:], r=[s_], sbuf=s_)
                self.gemm(xTs[half % 2], 16, 1024, T["w_cq"].ap()[l], D, 512, "FM", evac, wbufs, pbanks, after_w=hook)
            self.run_halves(2, A, B, gemm_half)

    def stage_cattn(self, l):
        c, T = self.c, self.T
        with ExitStack() as st:
            kT = c.sb(st, "kT", [128, 16, 256], BF16)
            v = c.sb(st, "v", [128, 2, D], BF16)
            c.dma("sp", kT[:], T["ckT"].ap().rearrange("(k p) m -> p k m", p=128), w=[kT], sbuf=kT)
            c.dma("sp", v[:], T["cv"].ap().rearrange("(j p) d -> p j d", p=128), w=[v], sbuf=v)
            qTs = [c.sb(st, "qT%d" % i, [128, 16, 512], BF16) for i in range(2)]
            oTs = [c.sb(st, "oT%d" % i, [128, 16, 512], BF16) for i in range(2)]
            pT = [c.sb(st, "pT%d" % i, [128, 2, 512], BF16) for i in range(2)]
            rd = [c.sb(st, "rd%d" % i, [128, 512], F32) for i in range(2)]
            pss = [c.ps(st, "pss%d" % i, [128, 512], F32) for i in range(3)]
            psd = c.ps(st, "psd", [128, 512], F32)
            pso = [c.ps(st, "pso%d" % i, [128, 512], F32) for i in range(3)]
            n = 0
            m = 0
            for tb in range(4):
                q_, o_ = qTs[tb % 2], oTs[tb % 2]
                c.dma("sp", q_[:], T["cqT"].ap()[:, tb * 512:(tb + 1) * 512].rearrange("(k p) t -> p k t", p=128), w=[q_], sbuf=q_)
                for hh in range(4):
                    p_ = pT[hh % 2]
                    for mh in range(2):
                        ps = pss[n % 3]
                        n += 1
                        for dc in range(4):
                            c.op("pe", lambda e: e.matmul(ps[:], kT[:, hh * 4 + dc, mh * 128:(mh + 1) * 128], q_[:, hh * 4 + dc, :], start=(dc == 0), stop=(dc == 3)), r=[kT, q_], w=[ps], signal=(dc == 3))
                        c.op("act", lambda e: e.activation(out=p_[:, mh, :], in_=ps[:], func=AF.Exp), r=[ps], w=[p_])
                    for mh in range(2):
                        c.op("pe", lambda e: e.matmul(psd[:], self.ones[:], p_[:, mh, :], start=(mh == 0), stop=(mh == 1)), r=[self.ones, p_], w=[psd], signal=(mh == 1))
                    r_ = rd[hh % 2]
                    c.op("dve", lambda e: e.reciprocal(out=r_[:], in_=psd[:]), r=[psd], w=[r_])
                    for dvc in range(4):
                        po = pso[m % 3]
                        m += 1
                        for mh in range(2):
                            c.op("pe", lambda e: e.matmul(po[:], v[:, mh, hh * 512 + dvc * 128:hh * 512 + (dvc + 1) * 128], p_[:, mh, :], start=(mh == 0), stop=(mh == 1)), r=[v, p_], w=[po], signal=(mh == 1))
                        c.op("dve", lambda e: e.tensor_tensor(out=o_[:, hh * 4 + dvc, :], in0=po[:], in1=r_[:], op=ALU.mult), r=[po, r_], w=[o_])
                c.dma("pool", T["coT"].ap()[:, tb * 512:(tb + 1) * 512].rearrange("(k p) t -> p k t", p=128), o_[:], r=[o_], sbuf=o_)

    def stage_co(self, l):
        c, T = self.c, self.T
        with ExitStack() as st:
            wbufs, pbanks = self.common_alloc(st)
            xT = c.sb(st, "xT", [128, 16, 1024], BF16)
            xs = [c.sb(st, "xs%d" % i, [128, 512], F32) for i in range(4)]
            so = [c.sb(st, "so%d" % i, [128, 512], F32) for i in range(4)]
            cnt = [0]
            for half in range(2):
                t0 = half * 1024
                c.dma("sp", xT[:], T["coT"].ap()[:, t0:t0 + 1024].rearrange("(k p) t -> p k t", p=128), w=[xT], sbuf=xT)
                self.gemm(xT, 16, 1024, T["w_co"].ap()[l], D, 512, "TM", self.mk_resid_evac(T["xres"], t0, xs, so, cnt), wbufs, pbanks)

    def stage_moe_h(self, l):
        c, T = self.c, self.T
        with ExitStack() as st:
            self.common_alloc(st, nw=0, npb=0)
            gain = self.load_bc(st, "gain", T["norm_moe"], l * D, D)
            wr = c.sb(st, "wr", [128, 16, NE], F32)
            wrh = c.sb(st, "wrh", [128, 16, NE], BF16)
            wrl = c.sb(st, "wrl", [128, 16, NE], BF16)
            wtmp = c.sb(st, "wtmp", [128, 16, NE], F32)
            c.dma("sp", wr[:], T["w_router"].ap()[l].rearrange("(k p) e -> p k e", p=128), w=[wr], sbuf=wr)
            c.op("dve", lambda e: e.tensor_copy(out=wrh[:], in_=wr[:]), r=[wr], w=[wrh])
            c.op("dve", lambda e: e.tensor_copy(out=wtmp[:], in_=wrh[:]), r=[wrh], w=[wtmp])
            c.op("dve", lambda e: e.tensor_tensor(out=wrl[:], in0=wr[:], in1=wtmp[:], op=ALU.subtract), r=[wr, wtmp], w=[wrl])
            xts = [c.sb(st, "xt%d" % i, [128, D], F32) for i in range(2)]
            hfs = [c.sb(st, "hf%d" % i, [128, D], F32) for i in range(2)]
            hbs = [c.sb(st, "hb%d" % i, [128, D], BF16) for i in range(2)]
            hls = [c.sb(st, "hl%d" % i, [128, D], BF16) for i in range(2)]
            hT = [c.sb(st, "hT%d" % i, [128, 16, 128], BF16) for i in range(2)]
            lT = [c.sb(st, "lT%d" % i, [128, 16, 128], BF16) for i in range(2)]
            ptrs = [c.ps(st, "ptr%d" % i, [128, 4, 128], BF16) for i in range(2)]
            pl = [c.ps(st, "pl%d" % i, [128, NE], F32) for i in range(2)]
            pa = c.ps(st, "pa", [NE, 128], F32)
            smalls = [(c.sb(st, "ss%d" % i, [128, 2], F32), c.sb(st, "rs%d" % i, [128, 1], F32)) for i in range(2)]
            ex = [c.sb(st, "ex%d" % i, [128, NE], F32) for i in range(2)]
            se = [c.sb(st, "se%d" % i, [128, 2], F32) for i in range(2)]
            af = [c.sb(st, "af%d" % i, [128, NE], F32) for i in range(2)]
            aT = c.sb(st, "aT", [NE, L], F32)
            for tt in range(16):
                xt, hf, hb, hl = xts[tt % 2], hfs[tt % 2], hbs[tt % 2], hls[tt % 2]
                ss, rstd = smalls[tt % 2]
                c.dma("sp", xt[:], T["xres"].ap()[tt * 128:(tt + 1) * 128, :], r=[self.xres_buf], w=[xt], sbuf=xt)
                c.op("act", lambda e: e.activation(out=hb[:], in_=xt[:], func=AF.Square, accum_out=ss[:, 0:1]), r=[xt], w=[hb, ss])
                c.op("act", lambda e: e.activation(out=ss[:, 1:2], in_=ss[:, 0:1], func=AF.Sqrt, scale=1.0 / D, bias=self.epsb[:, 0:1]), r=[ss, self.epsb], w=[ss])
                c.op("dve", lambda e: e.reciprocal(out=rstd[:, 0:1], in_=ss[:, 1:2]), r=[ss], w=[rstd])
                c.op("dve", lambda e: e.scalar_tensor_tensor(out=hf[:], in0=xt[:], scalar=rstd[:, 0:1], in1=gain[:], op0=ALU.mult, op1=ALU.mult), r=[xt, rstd, gain], w=[hf])
                c.op("act", lambda e: e.copy(out=hb[:], in_=hf[:]), r=[hf], w=[hb])
                c.op("pool", lambda e: e.tensor_tensor(out=hl[:], in0=hf[:], in1=hb[:], op=ALU.subtract), r=[hf, hb], w=[hl])
                c.dma("pool", T["hmoe"].ap()[tt * 128:(tt + 1) * 128, :], hb[:], r=[hb], w=[self.hmoe_buf], sbuf=hb)
                h_, l_ = hT[tt % 2], lT[tt % 2]
                self.transpose_into(hb, h_, 0, ptrs)
                self.transpose_into(hl, l_, 0, ptrs)
                p_ = pl[tt % 2]
                combos = [(h_, wrh), (l_, wrh), (h_, wrl)]
                for ci, (a_, w_) in enumerate(combos):
                    for k in range(16):
                        c.op("pe", lambda e: e.matmul(p_[:], a_[:, k, :], w_[:, k, :], start=(ci == 0 and k == 0), stop=(ci == 2 and k == 15)), r=[a_, w_], w=[p_], signal=(ci == 2 and k == 15))
                e_, s_, a2 = ex[tt % 2], se[tt % 2], af[tt % 2]
                c.op("act", lambda e: e.activation(out=e_[:], in_=p_[:], func=AF.Exp, accum_out=s_[:, 0:1]), r=[p_], w=[e_, s_])
                c.op("dve", lambda e: e.reciprocal(out=s_[:, 1:2], in_=s_[:, 0:1]), r=[s_], w=[s_])
                c.op("dve", lambda e: e.tensor_scalar(out=a2[:], in0=e_[:], scalar1=s_[:, 1:2], scalar2=None, op0=ALU.mult), r=[e_, s_], w=[a2])
                c.dma("pool", T["affc"].ap()[tt * 128:(tt + 1) * 128, :], a2[:], r=[a2], sbuf=a2)
                c.op("pe", lambda e: e.transpose(pa[:], a2[:], self.identf[:]), r=[a2, self.identf], w=[pa])
                c.op("act", lambda e: e.copy(out=aT[:, tt * 128:(tt + 1) * 128], in_=pa[:]), r=[pa], w=[aT])
            c.dma("sp", T["affT"].ap(), aT[:], r=[aT], w=[self.afft_buf], sbuf=aT)

    def stage_moe_x(self, l):
        c, T = self.c, self.T
        with ExitStack() as st:
            wbufs, pbanks = self.common_alloc(st, nw=4, npb=4)
            io = c.sb(st, "io", [128, 256], F32)
            pj = c.sb(st, "pj", [128, 16, 4], F32)
            pjb = [c.sb(st, "pjb%d" % i, [128, 16, 4], BF16) for i in range(2)]
            Sall = [c.sb(st, "Sall%d" % i, [128, 16, 256], BF16) for i in range(2)]
            c.dma("sp", io[:], T["iota256"].ap(), w=[io], sbuf=io)
            c.dma("sp", pj[:, :, 0:3], T["pj"].ap(), w=[pj], sbuf=pj)
            affc = c.sb(st, "affc", [128, 16, NE], F32)
            c.dma("sp", affc[:], T["affc"].ap().rearrange("(j p) e -> p j e", p=128), w=[affc], sbuf=affc)
            arow = [c.sb(st, "arow%d" % i, [128, L], F32) for i in range(2)]
            junk = c.sb(st, "junk", [128, L], BF16)
            rank = [c.sb(st, "rank%d" % i, [128, 16], F32) for i in range(2)]
            pidx = [c.ps(st, "pidx%d" % i, [128, 2, 4], F32) for i in range(2)]
            idf = [c.sb(st, "idf%d" % i, [128, 2, 6], F32) for i in range(2)]
            idxi = [[c.sb(st, "idx%d_%d" % (i, ch), [128, 1], I32) for ch in range(2)] for i in range(2)]
            xe = [c.sb(st, "xe%d" % i, [128, D], BF16) for i in range(2)]
            xeT = [c.sb(st, "xeT%d" % i, [128, 16, 256], BF16) for i in range(2)]
            hm = [c.sb(st, "hm%d" % i, [128, D], BF16) for i in range(2)]
            hmT = c.sb(st, "hmT", [128, 16, 256], BF16)
            sa = [c.sb(st, "sa%d" % i, [128, 512], F32) for i in range(2)]
            ysb = [c.sb(st, "ysb%d" % i, [128, D], F32) for i in range(2)]
            ptrs = [c.ps(st, "ptr%d" % i, [128, 4, 128], BF16) for i in range(2)]

            def rank_thunks(ex_):
                eb = ex_ % 2
                ar, rk = arow[eb], rank[eb]
                th = []

                def t0():
                    c.dma("sp", ar[:], row_bc(T["affT"], ex_ * L, L), r=[self.afft_buf], w=[ar], sbuf=ar)
                th.append(t0)
                for j in range(16):
                    def tj(j=j):
                        c.op("dve", lambda e: e.tensor_scalar(out=junk[:], in0=ar[:], scalar1=affc[:, j, ex_:ex_ + 1], scalar2=0.0, op0=ALU.is_gt, op1=ALU.add, accum_out=rk[:, j:j + 1]),
                             r=[ar, affc], w=[junk, rk])
                    th.append(tj)
                return th

            def slots(ex_):
                eb = ex_ % 2
                rk, S_, pb_, pi_, f_ = rank[eb], Sall[eb], pjb[eb], pidx[eb], idf[eb]
                c.op("dve", lambda e: e.tensor_copy(out=pj[:, :, 2], in_=affc[:, :, ex_]), r=[affc], w=[pj])
                c.op("dve", lambda e: e.tensor_copy(out=pb_[:, :, 0:3], in_=pj[:, :, 0:3]), r=[pj], w=[pb_])
                c.op("dve", lambda e: e.tensor_copy(out=pj[:, :, 3], in_=pb_[:, :, 2]), r=[pb_], w=[pj])
                c.op("dve", lambda e: e.tensor_tensor(out=pb_[:, :, 3], in0=pj[:, :, 2], in1=pj[:, :, 3], op=ALU.subtract), r=[pj], w=[pb_])
                for j in range(16):
                    c.op("dve", lambda e: e.tensor_scalar(out=S_[:, j, :], in0=io[:], scalar1=rk[:, j:j + 1], scalar2=None, op0=ALU.is_equal), r=[io, rk], w=[S_])
                for ch in range(2):
                    for j in range(16):
                        c.op("pe", lambda e: e.matmul(pi_[:, ch, 0:4], S_[:, j, ch * 128:(ch + 1) * 128], pb_[:, j, :], start=(j == 0), stop=(j == 15)), r=[S_, pb_], w=[pi_], signal=(j == 15))
                c.op("dve", lambda e: e.tensor_copy(out=f_[:, :, 0:4], in_=pi_[:, :, 0:4]), r=[pi_], w=[f_])
                c.op("dve", lambda e: e.scalar_tensor_tensor(out=f_[:, :, 4], in0=f_[:, :, 1], scalar=128.0, in1=f_[:, :, 0], op0=ALU.mult, op1=ALU.add), r=[f_], w=[f_])
                c.op("dve", lambda e: e.tensor_tensor(out=f_[:, :, 5], in0=f_[:, :, 2], in1=f_[:, :, 3], op=ALU.add), r=[f_], w=[f_])
                for ch in range(2):
                    ix = idxi[eb][ch]
                    c.op("dve", lambda e: e.tensor_copy(out=ix[:], in_=f_[:, ch, 4:5]), r=[f_], w=[ix])
                    x_ = xe[ch]
                    c.custom_dma("pool", lambda e: e.indirect_dma_start(out=x_[:], out_offset=None, in_=T["hmoe"].ap(), in_offset=bass.IndirectOffsetOnAxis(ap=ix[:, 0:1], axis=0)),
                                 r=[ix, self.hmoe_buf], w=[x_], sbuf=x_)

            def gather_T(ex_):
                for ch in range(2):
                    self.transpose_into(xe[ch], xeT[ex_ % 2], ch * 128, ptrs)

            def scatter(ex_):
                eb = ex_ % 2
                for ch in range(2):
                    ix = idxi[eb][ch]
                    y_ = ysb[ch]
                    c.custom_dma("pool", lambda e: e.indirect_dma_start(out=T["xres"].ap(), out_offset=bass.IndirectOffsetOnAxis(ap=ix[:, 0:1], axis=0), in_=y_[:], in_offset=None, compute_op=ALU.add),
                                 r=[ix, y_, self.xres_buf], w=[self.xres_buf], sbuf=y_)

            for t in rank_thunks(0):
                t()
            slots(0)
            gather_T(0)
            for ex_ in range(NE):
                eb = ex_ % 2
                xT_ = xeT[eb]
                f_ = idf[eb]
                wg = T["w_gate"].ap()[l, ex_]
                wu = T["w_up"].ap()[l, ex_]
                wd = T["w_down"].ap()[l, ex_]
                nxt = rank_thunks(ex_ + 1) if ex_ + 1 < NE else []
                wcount = [0]

                def after_w(_nb):
                    wcount[0] += 1
                    if wcount[0] == 3 and ex_ > 0:
                        scatter(ex_ - 1)
                for nb in range(4):
                    def ev_gate(_nb, tt, pb, nb=nb):
                        s2 = sa[tt]
                        c.op("act", lambda e: e.activation(out=s2[:], in_=pb[:], func=AF.Silu), r=[pb], w=[s2])

                    def ev_up(_nb, tt, pb, nb=nb):
                        s2 = sa[tt]
                        c.op("dve", lambda e: e.tensor_tensor(out=hm[tt][:, nb * 512:(nb + 1) * 512], in0=pb[:], in1=s2[:], op=ALU.mult), r=[pb, s2], w=[hm[tt]])
                    self.gemm(xT_, 16, 256, wg[:, nb * 512:(nb + 1) * 512], 512, 512, "TM", ev_gate, wbufs, pbanks, after_w=after_w)
                    for _ in range(2):
                        if nxt:
                            nxt.pop(0)()
                    self.gemm(xT_, 16, 256, wu[:, nb * 512:(nb + 1) * 512], 512, 512, "TM", ev_up, wbufs, pbanks, after_w=after_w)
                    for _ in range(3):
                        if nxt:
                            nxt.pop(0)()
                while nxt:
                    nxt.pop(0)()
                for ch in range(2):
                    self.transpose_into(hm[ch], hmT, ch * 128, ptrs)

                def after_wd(nb):
                    if nb == 2 and ex_ + 1 < NE:
                        slots(ex_ + 1)

                def ev_down(nb, tt, pb):
                    y_ = ysb[tt]
                    if nb % 2 == 0:
                        c.op("act", lambda e: e.activation(out=y_[:, nb * 512:(nb + 1) * 512], in_=pb[:], func=AF.Copy, scale=f_[:, tt, 5:6]), r=[pb, f_], w=[y_])
                    else:
                        c.op("dve", lambda e: e.tensor_scalar(out=y_[:, nb * 512:(nb + 1) * 512], in0=pb[:], scalar1=f_[:, tt, 5:6], scalar2=None, op0=ALU.mult), r=[pb, f_], w=[y_])
                self.gemm(hmT, 16, 256, wd, D, 512, "TM", ev_down, wbufs, pbanks, after_w=after_wd)
                if ex_ + 1 < NE:
                    gather_T(ex_ + 1)
            scatter(NE - 1)

    def stage_final(self):
        c, T = self.c, self.T
        with ExitStack() as st:
            self.common_alloc(st, nw=0, npb=0)
            gain = self.load_bc(st, "gain", T["final_norm"], 0, D)
            xts = [c.sb(st, "xt%d" % i, [128, D], F32) for i in range(3)]
            jk = [c.sb(st, "jk%d" % i, [128, D], BF16) for i in range(2)]
            os_ = [c.sb(st, "os%d" % i, [128, D], F32) for i in range(2)]
            sm = [c.sb(st, "sm%d" % i, [128, 3], F32) for i in range(2)]
            for tt in range(16):
                xt, j_, o_, s_ = xts[tt % 3], jk[tt % 2], os_[tt % 2], sm[tt % 2]
                c.dma("sp", xt[:], T["xres"].ap()[tt * 128:(tt + 1) * 128, :], r=[self.xres_buf], w=[xt], sbuf=xt)
                c.op("act", lambda e: e.activation(out=j_[:], in_=xt[:], func=AF.Square, accum_out=s_[:, 0:1]), r=[xt], w=[j_, s_])
                c.op("act", lambda e: e.activation(out=s_[:, 1:2], in_=s_[:, 0:1], func=AF.Sqrt, scale=1.0 / D, bias=self.epsb[:, 0:1]), r=[s_, self.epsb], w=[s_])
                c.op("dve", lambda e: e.reciprocal(out=s_[:, 2:3], in_=s_[:, 1:2]), r=[s_], w=[s_])
                c.op("dve", lambda e: e.scalar_tensor_tensor(out=o_[:], in0=xt[:], scalar=s_[:, 2:3], in1=gain[:], op0=ALU.mult, op1=ALU.mult), r=[xt, s_, gain], w=[o_])
                c.dma("pool", T["out"].ap()[tt * 128:(tt + 1) * 128, :], o_[:], r=[o_], sbuf=o_)


_CACHE = {}


def host_inputs(inputs, consts, b):
    m = {"x": np.ascontiguousarray(inputs["x"][b]), "mem": np.ascontiguousarray(inputs["mem"][b])}
    for k in WEIGHT_SHAPES:
        if k == "rpbY":
            continue
        m[k] = np.ascontiguousarray(np.asarray(inputs[k], np.float32))
    m["rpbY"] = rpb_layout(np.asarray(inputs["na_rpb"], np.float32))
    for k in CONST_DTYPES:
        m[k] = consts[k]
    return m


def kernel(**inputs):
    inputs = {k: np.asarray(v) for k, v in inputs.items()}
    if "prog" not in _CACHE:
        p = Prog()
        p.build()
        _CACHE["prog"] = p
    p = _CACHE["prog"]
    B = inputs["x"].shape[0]
    in_maps = [host_inputs(inputs, p.consts, b) for b in range(B)]
    res = run_bass_kernel_spmd(p.nc, in_maps, core_ids=[0, 2, 4, 6][:B])
    out = np.stack([np.asarray(r["out"], np.float32) for r in res.results], axis=0)
    return out
```

```python
import math
import numpy as np
import ml_dtypes
import concourse.bass as bass
import concourse.mybir as mybir
from concourse.bass_utils import run_bass_kernel_spmd
from contextlib import ExitStack

dt = mybir.dt
F32, BF16, I32 = dt.float32, dt.bfloat16, dt.int32
AF = mybir.ActivationFunctionType
ALU = mybir.AluOpType
AX = mybir.AxisListType

SAME_ENGINE_SYNC = True

D = 2048
L = 2048
DEPTH = 2
NMEM = 256
NA_H = 12
P_IN = 6912
NE = 16
CAP = 256
RMS_EPS = 1e-6
GN_EPS = 1e-5
PI = math.pi


class Sem:
    def __init__(self, h):
        self.h = h
        self.n = 0


class Buf:
    def __init__(self, t=None, name=""):
        self.t = t
        self.name = name
        self.w = None
        self.r = {}
        self.dsem = None

    def __getitem__(self, idx):
        return self.t[idx]


class Eng:
    def __init__(self, h, sem, name):
        self.h = h
        self.sem = sem
        self.name = name
        self.known = {}


class Ctx:
    def __init__(self, nc):
        self.nc = nc
        self.es = ExitStack()
        self.eng = {}
        for name, h in (("pe", nc.tensor), ("dve", nc.vector), ("act", nc.scalar),
                        ("pool", nc.gpsimd), ("sp", nc.sync)):
            s = Sem(self.es.enter_context(nc.semaphore("es_" + name)))
            self.eng[name] = Eng(h, s, name)
        self.free_sems = []
        self.all_dsems = []
        self.stage_bufs = []
        self.ninst = 0
        self.flip = 0

    def new_dsem(self):
        if self.free_sems:
            return self.free_sems.pop()
        s = Sem(self.es.enter_context(self.nc.semaphore("ds%d" % len(self.all_dsems))))
        self.all_dsems.append(s)
        return s

    def sb(self, st, name, shape, dtype):
        self.uid = getattr(self, "uid", 0) + 1
        name = "s%d_%s" % (self.uid, name)
        t = st.enter_context(self.nc.sbuf_tensor(name, list(shape), dtype))
        b = Buf(t, name)
        self.stage_bufs.append(b)
        return b

    def ps(self, st, name, shape, dtype=F32):
        self.uid = getattr(self, "uid", 0) + 1
        name = "p%d_%s" % (self.uid, name)
        t = st.enter_context(self.nc.psum_tensor(name, list(shape), dtype))
        b = Buf(t, name)
        self.stage_bufs.append(b)
        return b

    def _waits(self, E, r, w):
        waits = {}

        def need(s, v):
            if waits.get(s, 0) < v:
                waits[s] = v
        for b in r:
            if b.w is not None:
                need(*b.w)
        for b in w:
            if b.w is not None:
                need(*b.w)
            for s, v in b.r.items():
                need(s, v)
        for s, v in waits.items():
            if E.known.get(s, 0) >= v:
                continue
            if s is E.sem and (E.name == "pe" or not SAME_ENGINE_SYNC):
                continue
            E.h.wait_ge(s.h, v)
            self.ninst += 1
            E.known[s] = v

    def _record(self, dep, r, w):
        s, v = dep
        for b in r:
            if b.r.get(s, 0) < v:
                b.r[s] = v
        for b in w:
            b.w = dep
            b.r = {}

    def op(self, eng, fn, r=(), w=(), signal=True):
        E = self.eng[eng]
        self._waits(E, r, w)
        inst = fn(E.h)
        self.ninst += 1
        if signal:
            E.sem.n += 1
            inst.then_inc(E.sem.h, 1)
            dep = (E.sem, E.sem.n)
        else:
            dep = (E.sem, E.sem.n + 1)
        self._record(dep, r, w)
        return inst

    def dma(self, q, out, in_, r=(), w=(), sbuf=None, **kw):
        return self.custom_dma(q, lambda e: e.dma_start(out=out, in_=in_, **kw), r=r, w=w, sbuf=sbuf)

    def custom_dma(self, q, fn, r=(), w=(), sbuf=None):
        E = self.eng[q]
        self._waits(E, r, w)
        inst = fn(E.h)
        self.ninst += 1
        if sbuf.dsem is None:
            sbuf.dsem = self.new_dsem()
        s = sbuf.dsem
        s.n += 16
        inst.then_inc(s.h, 16)
        self._record((s, s.n), r, w)
        return inst

    def barrier(self):
        targets = [(e.sem, e.sem.n) for e in self.eng.values() if e.sem.n > 0]
        targets += [(s, s.n) for s in self.all_dsems if s.n > 0]
        for E in self.eng.values():
            for s, v in targets:
                if s is E.sem or E.known.get(s, 0) >= v:
                    continue
                E.h.wait_ge(s.h, v)
                self.ninst += 1
                E.known[s] = v

    def end_stage(self, dram_bufs=()):
        self.barrier()
        for b in self.stage_bufs:
            if b.dsem is not None:
                self.free_sems.append(b.dsem)
                b.dsem = None
        self.stage_bufs = []
        for b in dram_bufs:
            b.w = None
            b.r = {}

    def evac_eng(self):
        self.flip ^= 1
        return "act" if self.flip else "dve"


def copy_on(c, eng, out, in_, r, w, scale=None):
    if eng == "act":
        if scale is None:
            c.op("act", lambda e: e.copy(out=out, in_=in_), r=r, w=w)
        else:
            c.op("act", lambda e: e.mul(out, in_, float(scale)), r=r, w=w)
    else:
        if scale is None:
            c.op(eng, lambda e: e.tensor_copy(out=out, in_=in_), r=r, w=w)
        else:
            c.op(eng, lambda e: e.tensor_scalar(out=out, in0=in_, scalar1=float(scale), scalar2=None, op0=ALU.mult), r=r, w=w)


def bf(a):
    return np.ascontiguousarray(np.asarray(a, np.float32).astype(ml_dtypes.bfloat16))


def na_geometry():
    rows = 32

    def rs(r):
        return min(max(r - 4, 0), rows - 8)
    pairs = {}
    pats = {}
    c = np.arange(64)
    cs = np.clip(c - 8, 0, 48)
    colok = (c[:, None] >= cs[None, :]) & (c[:, None] < cs[None, :] + 16)
    tiles = []
    for i in range(16):
        lst = []
        for j in range(16):
            v = [[rs(2 * i + b) <= 2 * j + a < rs(2 * i + b) + 8 for b in range(2)] for a in range(2)]
            if not any(v[0] + v[1]):
                continue
            key = tuple(v[0] + v[1])
            if key not in pats:
                pats[key] = len(tiles)
                m = np.full((128, 128), -30000.0, np.float32)
                for a in range(2):
                    for b in range(2):
                        if v[a][b]:
                            blk = np.where(colok, 0.0, -30000.0)
                            m[a * 64:(a + 1) * 64, b * 64:(b + 1) * 64] = blk
                tiles.append(m)
            lst.append((j, 2 * j - 2 * i, pats[key]))
        pairs[i] = lst
    masks = np.stack(tiles, 0)
    return pairs, masks


def make_consts():
    cst = {}
    t = np.arange(L, dtype=np.float64)
    f = np.arange(L, dtype=np.float64)
    th = 2.0 * np.pi * (f[None, :] + 0.5) * t[:, None] / (2 * L)
    C = np.cos(th)
    S = np.sin(th)
    def fwd_layout(M):
        return M.reshape(16, 128, 16, 128).transpose(2, 1, 0, 3)
    cst["dftC"] = bf(fwd_layout(C))
    cst["dftS"] = bf(fwd_layout(S))
    def inv_layout(M):
        MT = M.T
        return MT.reshape(16, 128, 16, 128).transpose(2, 1, 0, 3)
    cst["dftCT"] = bf(inv_layout(C))
    cst["dftnST"] = bf(inv_layout(-S))
    tt = np.arange(L, dtype=np.float32)
    t01 = tt / (L - 1)
    bands = np.linspace(1e-4, 8 - 1, 8, dtype=np.float32)
    ang = (2.0 * np.float32(np.pi)) * (tt[:, None] / L) * bands[None, :]
    feats = np.concatenate([t01[:, None], np.cos(ang), -np.sin(ang)], axis=-1).astype(np.float32)
    cst["featsT"] = np.ascontiguousarray(feats.T)
    min_decay = math.log(1e-2) / 1.5
    max_decay = math.log(1e-2) / 0.3
    deltas = np.abs(np.linspace(min_decay, max_decay, 512, dtype=np.float32))
    cst["window"] = np.exp(-t01[:, None] * deltas[None, :]).astype(np.float32)
    inv = 1.0 / (10000.0 ** np.linspace(0.0, 1.0, 64, dtype=np.float32))
    angr = tt[:, None] * inv[None, :]
    cosT = np.cos(angr).T.astype(np.float32)
    sinT = np.sin(angr).T.astype(np.float32)
    cst["rotcos"] = np.ascontiguousarray(np.concatenate([cosT, cosT], 0))
    cst["rotsin"] = np.ascontiguousarray(np.concatenate([sinT, sinT], 0))
    R = np.zeros((128, 128), np.float32)
    for m in range(64):
        R[m + 64, m] = -1.0
    for m in range(64, 128):
        R[m - 64, m] = 1.0
    cst["rotR"] = bf(R)
    hidx = np.arange(6, dtype=np.float64)
    lgf = np.log1p(-np.exp2(-5.0 - hidx))
    lgb = np.log1p(-np.exp2(-5.5 - hidx))
    dd = np.zeros((6, 4, 128, 512), np.float32)
    jj = np.arange(128)[:, None]
    ii = np.arange(512)[None, :]
    for h in range(6):
        for pos in range(4):
            diff = ii - (jj + pos * 128)
            dd[h, pos] = np.where(diff >= 0, np.exp(lgf[h] * np.maximum(diff, 0)), np.exp(lgb[h] * np.maximum(-diff, 0)))
    cst["retD"] = bf(dd.transpose(0, 2, 1, 3))
    cst["retE"] = (ii - jj).astype(np.float32)
    cst["_lgf"] = lgf
    cst["_lgb"] = lgb
    pairs, masks = na_geometry()
    cst["namask"] = bf(masks.transpose(1, 0, 2))
    J = np.zeros((128, 128), np.float32)
    for a in range(2):
        for k in range(64):
            J[a * 64 + 63 - k, a * 64 + k] = 1.0
    cst["naJ"] = bf(J)
    cst["ident"] = bf(np.eye(128))
    cst["identf"] = np.eye(128, dtype=np.float32)
    cst["ones"] = bf(np.ones((128, 128)))
    cst["iota256"] = np.tile(np.arange(256, dtype=np.float32)[None, :], (128, 1))
    pj = np.zeros((128, 16, 3), np.float32)
    pj[:, :, 0] = np.arange(128)[:, None]
    pj[:, :, 1] = np.arange(16)[None, :]
    cst["pj"] = pj
    gw = np.zeros((128, 3), np.float32)
    gw[:, 0] = 1.0 / 768
    gw[:, 1] = 1.0 / 512
    gw[:, 2] = 1.0 / 768
    cst["ginvw"] = gw
    return cst, pairs


def rpb_layout(na_rpb):
    Y = np.zeros((DEPTH, NA_H, 15, 128), np.float32)
    for m in range(15):
        dri = 14 - m
        Y[:, :, m, 48:79] = na_rpb[:, :, dri, ::-1]
    return Y


CONST_DTYPES = {"dftC": BF16, "dftS": BF16, "dftCT": BF16, "dftnST": BF16, "featsT": F32, "window": F32,
                "rotcos": F32, "rotsin": F32, "rotR": BF16, "retD": BF16, "retE": F32, "namask": BF16,
                "naJ": BF16, "ident": BF16, "identf": F32, "ones": BF16, "iota256": F32, "pj": F32,
                "ginvw": F32}

WEIGHT_SHAPES = {
    "norm_mix": (DEPTH, D), "w_in": (DEPTH, D, P_IN), "rpbY": (DEPTH, NA_H, 15, 128),
    "hy_conv_w": (DEPTH, 3, 1536), "hy_conv_b": (DEPTH, 1536), "hy_filt_w1": (DEPTH, 17, 64),
    "hy_filt_b1": (DEPTH, 64), "hy_filt_w2": (DEPTH, 64, 64), "hy_filt_b2": (DEPTH, 64),
    "hy_filt_w3": (DEPTH, 64, 2048), "hy_sin_freq": (DEPTH, 64), "hy_skip_d": (DEPTH, 2, 512),
    "branch_norm": (DEPTH, D), "w_out": (DEPTH, D, D), "norm_cross": (DEPTH, D), "mem_norm": (D,),
    "w_cq": (DEPTH, D, D), "w_ckv": (DEPTH, D, 2 * D), "w_co": (DEPTH, D, D), "norm_moe": (DEPTH, D),
    "w_router": (DEPTH, D, NE), "w_gate": (DEPTH, NE, D, D), "w_up": (DEPTH, NE, D, D),
    "w_down": (DEPTH, NE, D, D), "final_norm": (D,),
}


def row_bc(t, off, n, parts=128):
    return bass.AP(t, off, [[0, parts], [1, n]])


class Prog:
    def __init__(self, debug=False, stop_after=None, nlayers=DEPTH):
        self.debug = debug
        self.stop_after = stop_after
        self.nlayers = nlayers
        self.consts, self.na_pairs = make_consts()
        self.npat = self.consts["namask"].shape[1]
        nc = bass.Bass("TRN2", target_bir_lowering=False)
        self.nc = nc
        self.c = Ctx(nc)
        T = {}
        T["x"] = nc.dram_tensor("x", [L, D], F32, kind="ExternalInput")
        T["mem"] = nc.dram_tensor("mem", [NMEM, D], F32, kind="ExternalInput")
        for k, shp in WEIGHT_SHAPES.items():
            T[k] = nc.dram_tensor(k, list(shp), F32, kind="ExternalInput")
        for k, dty in CONST_DTYPES.items():
            T[k] = nc.dram_tensor(k, list(self.consts[k].shape), dty, kind="ExternalInput")
        T["out"] = nc.dram_tensor("out", [L, D], F32, kind="ExternalOutput")
        sk = "ExternalOutput" if debug else "Internal"
        for name, shp, dty in [
            ("xres", [L, D], F32), ("naqT", [768, L], BF16), ("nakT", [768, L], BF16), ("nav", [L, 768], BF16),
            ("hyp", [L, 1536], F32), ("hyx", [L, 1024], F32), ("rqT", [768, L], BF16), ("rkT", [768, L], BF16),
            ("rv", [L, 768], BF16), ("rg", [L, 768], F32), ("ymix", [L, D], F32), ("cqT", [D, L], BF16),
            ("coT", [D, L], BF16), ("ckT", [D, NMEM], BF16), ("cv", [NMEM, D], BF16), ("hmoe", [L, D], BF16),
            ("affT", [NE, L], F32), ("affc", [L, NE], F32),
        ]:
            T[name] = nc.dram_tensor(name, shp, dty, kind=sk)
        self.T = T
        self.xres_buf = Buf(None, "xres")
        self.hmoe_buf = Buf(None, "hmoe")
        self.afft_buf = Buf(None, "affT")

    def build(self):
        c = self.c
        with ExitStack() as gst:
            self.ident = c.sb(gst, "ident", [128, 128], BF16)
            self.identf = c.sb(gst, "identf", [128, 128], F32)
            self.ones = c.sb(gst, "ones", [128, 128], BF16)
            c.dma("sp", self.ident[:], self.T["ident"].ap(), w=[self.ident], sbuf=self.ident)
            c.dma("sp", self.identf[:], self.T["identf"].ap(), w=[self.identf], sbuf=self.identf)
            c.dma("sp", self.ones[:], self.T["ones"].ap(), w=[self.ones], sbuf=self.ones)
            c.stage_bufs = []
            stages = []
            for l in range(self.nlayers):
                xsrc = self.T["x"] if l == 0 else self.T["xres"]
                stages += [
                    ("inproj%d" % l, lambda l=l, xsrc=xsrc: self.stage_inproj(l, xsrc)),
                    ("na%d" % l, lambda l=l: self.stage_na(l)),
                    ("hy%d" % l, lambda l=l: self.stage_hyena(l)),
                    ("ret%d" % l, lambda l=l: self.stage_ret(l)),
                    ("outproj%d" % l, lambda l=l, xsrc=xsrc: self.stage_outproj(l, xsrc)),
                    ("ckv%d" % l, lambda l=l: self.stage_ckv(l)),
                    ("cq%d" % l, lambda l=l: self.stage_cq(l)),
                    ("cattn%d" % l, lambda l=l: self.stage_cattn(l)),
                    ("co%d" % l, lambda l=l: self.stage_co(l)),
                    ("moeh%d" % l, lambda l=l: self.stage_moe_h(l)),
                    ("moex%d" % l, lambda l=l: self.stage_moe_x(l)),
                ]
            stages.append(("final", self.stage_final))
            for name, fn in stages:
                fn()
                c.end_stage([self.xres_buf, self.hmoe_buf, self.afft_buf])
                if self.stop_after == name:
                    break
            c.barrier()
        return self.nc

    def load_bc(self, st, name, t, off, n, dtype=F32):
        b = self.c.sb(st, name, [128, n], dtype)
        self.c.dma("sp", b[:], row_bc(t, off, n), w=[b], sbuf=b)
        return b

    def rms_to_T(self, xt, gain, hb, xT, col0, ptrs, small, eps=RMS_EPS, width=D, no_T=False):
        c = self.c
        ss, rstd = small
        c.op("act", lambda e: e.activation(out=hb[:], in_=xt[:], func=AF.Square, accum_out=ss[:, 0:1]), r=[xt], w=[hb, ss])
        c.op("act", lambda e: e.activation(out=ss[:, 1:2], in_=ss[:, 0:1], func=AF.Sqrt, scale=1.0 / width, bias=self.epsb[:, 0:1]), r=[ss, self.epsb], w=[ss])
        c.op("dve", lambda e: e.reciprocal(out=rstd[:, 0:1], in_=ss[:, 1:2]), r=[ss], w=[rstd])
        c.op("dve", lambda e: e.scalar_tensor_tensor(out=hb[:], in0=xt[:], scalar=rstd[:, 0:1], in1=gain[:], op0=ALU.mult, op1=ALU.mult), r=[xt, rstd, gain], w=[hb])
        if not no_T:
            self.transpose_into(hb, xT, col0, ptrs)

    def transpose_into(self, hb, xT, col0, ptrs, nk=16):
        c = self.c
        for k4 in range(nk // 4):
            pt = ptrs[k4 % len(ptrs)]
            for q in range(4):
                k = k4 * 4 + q
                c.op("pe", lambda e: e.transpose(pt[:, q, :], hb[:, k * 128:(k + 1) * 128], self.ident[:]),
                     r=[hb, self.ident], w=[pt], signal=(q == 3))
            eng = c.evac_eng()
            copy_on(c, eng, xT[:, k4 * 4:(k4 + 1) * 4, col0:col0 + 128], pt[:, :, :], r=[pt], w=[xT])

    def gemm(self, xT, KC, Tn, w_ap, N, bw, mode, evac, wbufs, pbanks, after_w=None):
        c = self.c
        for nb in range(N // bw):
            wb = wbufs[self.wctr % len(wbufs)]
            self.wctr += 1
            c.dma("pool", wb[:, 0:KC, 0:bw], w_ap[:, nb * bw:(nb + 1) * bw].rearrange("(k p) n -> p k n", p=128), w=[wb], sbuf=wb)
            if after_w is not None:
                after_w(nb)
            if mode == "TM":
                for tt in range(Tn // 128):
                    pb = pbanks[self.pctr % len(pbanks)]
                    self.pctr += 1
                    for k in range(KC):
                        c.op("pe", lambda e: e.matmul(pb[:, 0:bw], xT[:, k, tt * 128:(tt + 1) * 128], wb[:, k, 0:bw], start=(k == 0), stop=(k == KC - 1)),
                             r=[xT, wb], w=[pb], signal=(k == KC - 1))
                    evac(nb, tt, pb)
            else:
                tbs = min(512, Tn)
                for sub in range(bw // 128):
                    for tb in range(Tn // tbs):
                        pb = pbanks[self.pctr % len(pbanks)]
                        self.pctr += 1
                        for k in range(KC):
                            c.op("pe", lambda e: e.matmul(pb[:, 0:tbs], wb[:, k, sub * 128:(sub + 1) * 128], xT[:, k, tb * tbs:(tb + 1) * tbs], start=(k == 0), stop=(k == KC - 1)),
                                 r=[xT, wb], w=[pb], signal=(k == KC - 1))
                        evac(nb * (bw // 128) + sub, tb, pb)


    def run_halves(self, nhalf, A, B, gemm_half):
        for tt in range(8):
            A(0, tt)
            B(0, tt)
        for half in range(nhalf):
            sched = []
            if half + 1 < nhalf:
                hn = half + 1
                sched = [[("A", 0), ("A", 1)], [("B", 0), ("B", 1), ("A", 2), ("A", 3)], [("B", 2), ("B", 3), ("A", 4), ("A", 5)],
                         [("B", 4), ("B", 5), ("A", 6), ("A", 7)], [("B", 6), ("B", 7)]]
            state = [0]

            def hook(_nb, half=half):
                if state[0] < len(sched):
                    for kind, tt in sched[state[0]]:
                        (A if kind == "A" else B)(half + 1, tt)
                    state[0] += 1
            gemm_half(half, hook)
            while state[0] < len(sched):
                hook(0)

    def common_alloc(self, st, nw=3, npb=4, wk=16):
        c = self.c
        self.wctr = 0
        self.pctr = 0
        wbufs = [c.sb(st, "wb%d" % i, [128, wk, 512], BF16) for i in range(nw)]
        pbanks = [c.ps(st, "pb%d" % i, [128, 512], F32) for i in range(npb)]
        self.epsb = c.sb(st, "epsb", [128, 1], F32)
        c.op("dve", lambda e: e.memset(self.epsb[:], RMS_EPS), w=[self.epsb])
        return wbufs, pbanks

    def stage_inproj(self, l, xsrc):
        c, T = self.c, self.T
        with ExitStack() as st:
            wbufs, pbanks = self.common_alloc(st)
            gain = self.load_bc(st, "gain", T["norm_mix"], l * D, D)
            xTs = [c.sb(st, "xT%d" % i, [128, 16, 1024], BF16) for i in range(2)]
            xts = [c.sb(st, "xt%d" % i, [128, D], F32) for i in range(2)]
            hbs = [c.sb(st, "hb%d" % i, [128, D], BF16) for i in range(2)]
            ptrs = [c.ps(st, "ptr%d" % i, [128, 4, 128], BF16) for i in range(2)]
            smalls = [(c.sb(st, "ss%d" % i, [128, 2], F32), c.sb(st, "rs%d" % i, [128, 1], F32)) for i in range(2)]
            stg_bf = [c.sb(st, "sgb%d" % i, [128, 512], BF16) for i in range(4)]
            stg_f = [c.sb(st, "sgf%d" % i, [128, 512], F32) for i in range(3)]
            cnt = [0, 0]
            w_in = T["w_in"].ap()[l]

            def A(half, tt):
                xt = xts[tt % 2]
                c.dma("sp", xt[:], xsrc.ap()[half * 1024 + tt * 128:half * 1024 + (tt + 1) * 128, :], w=[xt], sbuf=xt)
                self.rms_to_T(xt, gain, hbs[tt % 2], None, 0, ptrs, smalls[tt % 2], no_T=True)

            def B(half, tt):
                self.transpose_into(hbs[tt % 2], xTs[half % 2], tt * 128, ptrs)

            def gemm_half(half, hook):
                t0 = half * 1024
                xT = xTs[half % 2]

                def mk_evac(dst, col_off, fm, dtype, scale, bw):
                    def evac(i0, i1, pb):
                        if dtype == BF16:
                            sg = stg_bf[cnt[0] % len(stg_bf)]
                            cnt[0] += 1
                        else:
                            sg = stg_f[cnt[1] % len(stg_f)]
                            cnt[1] += 1
                        eng = c.evac_eng()
                        if fm:
                            copy_on(c, eng, sg[:, 0:512], pb[:, 0:512], r=[pb], w=[sg], scale=scale)
                            c.dma("sp", dst.ap()[i0 * 128:(i0 + 1) * 128, t0 + i1 * 512:t0 + (i1 + 1) * 512], sg[:, 0:512], r=[sg], sbuf=sg)
                        else:
                            copy_on(c, eng, sg[:, 0:bw], pb[:, 0:bw], r=[pb], w=[sg], scale=scale)
                            c.dma("sp", dst.ap()[t0 + i1 * 128:t0 + (i1 + 1) * 128, col_off + i0 * bw:col_off + (i0 + 1) * bw], sg[:, 0:bw], r=[sg], sbuf=sg)
                    return evac
                groups = [
                    (0, 768, "FM", T["naqT"], BF16, 0.125, 384),
                    (768, 768, "FM", T["nakT"], BF16, None, 384),
                    (1536, 768, "TM", T["nav"], BF16, None, 384),
                    (2304, 1536, "TM", T["hyp"], F32, None, 512),
                    (3840, 768, "FM", T["rqT"], BF16, 128 ** -0.5, 384),
                    (4608, 768, "FM", T["rkT"], BF16, None, 384),
                    (5376, 768, "TM", T["rv"], BF16, None, 384),
                    (6144, 768, "TM", T["rg"], F32, None, 384),
                ]
                for (c0, n, mode, dst, dty, scale, bw) in groups:
                    self.gemm(xT, 16, 1024, w_in[:, c0:c0 + n], n, bw, mode, mk_evac(dst, 0, mode == "FM", dty, scale, bw), wbufs, pbanks, after_w=hook)
            self.run_halves(2, A, B, gemm_half)

    def stage_na(self, l):
        c, T = self.c, self.T
        DELTAS = [-6, -4, -2, 0, 2, 4, 6]
        with ExitStack() as st:
            qT = c.sb(st, "qT", [128, 6, L], BF16)
            kT = c.sb(st, "kT", [128, 6, L], BF16)
            vx = c.sb(st, "vx", [128, 16, 12, 65], BF16)
            COMBOS = sorted(set((dl, pat) for i in range(16) for (j, dl, pat) in self.na_pairs[i]))
            bt = c.sb(st, "bt", [128, NA_H * len(COMBOS), 128], BF16)
            bp = [c.sb(st, "bp%d" % i, [128, 7, 2, 64], BF16) for i in range(3)]
            mt = c.sb(st, "mt", [128, self.npat, 128], BF16)
            jm = c.sb(st, "jm", [128, 128], BF16)
            psc = [c.ps(st, "psc%d" % i, [128, 1024], F32) for i in range(2)]
            pso = [c.ps(st, "pso%d" % i, [128, 2, 512], F32) for i in range(2)]
            pT = [c.sb(st, "pT%d" % i, [128, 640], BF16) for i in range(3)]
            rden = [c.sb(st, "rden%d" % i, [128, 12], F32) for i in range(2)]
            yt = [c.sb(st, "yt%d" % i, [128, 768], F32) for i in range(2)]
            c.dma("sp", qT[:], T["naqT"].ap().rearrange("(k p) t -> p k t", p=128), w=[qT], sbuf=qT)
            c.dma("sp", kT[:], T["nakT"].ap().rearrange("(k p) t -> p k t", p=128), w=[kT], sbuf=kT)
            c.op("pool", lambda e: e.memset(vx[:], 1.0), w=[vx])
            for j in range(16):
                c.dma("sp", vx[:, j, :, 0:64], T["nav"].ap()[j * 128:(j + 1) * 128, :].rearrange("p (h d) -> p h d", d=64), w=[vx], sbuf=vx)
            c.dma("sp", mt[:], T["namask"].ap(), w=[mt], sbuf=mt)
            c.dma("sp", jm[:], T["naJ"].ap(), w=[jm], sbuf=jm)
            n = 0
            for h in range(NA_H):
                b = bp[h % 3]
                for a in range(2):
                    off = ((l * NA_H + h) * 15 + (1 - a)) * 128
                    src = bass.AP(T["rpbY"], off, [[1, 64], [256, 7], [128, 2], [1, 64]])
                    c.dma("pool", b[a * 64:(a + 1) * 64, :, :, :], src, w=[b], sbuf=b)
                for ci, (dl, pat) in enumerate(COMBOS):
                    slot = h * len(COMBOS) + ci
                    dd = (6 - dl) // 2
                    n += 1
                    pbb = psc[n % 2]
                    c.op("pe", lambda e: e.matmul(pbb[:, 0:128], jm[:], b[:, dd, :, :].rearrange("p b q -> p (b q)"), start=True, stop=False), r=[jm, b], w=[pbb], signal=False)
                    c.op("pe", lambda e: e.matmul(pbb[:, 0:128], self.ident[:], mt[:, pat, :], start=False, stop=True), r=[self.ident, mt], w=[pbb])
                    copy_on(c, c.evac_eng(), bt[:, slot, :], pbb[:, 0:128], r=[pbb], w=[bt])
            n = 0
            pend = []

            def epilogue(i, po):
                rd = rden[i % 2]
                y = yt[i % 2]
                for g in range(2):
                    c.op("dve", lambda e: e.reciprocal(out=rd[:, g * 6:(g + 1) * 6], in_=po[:, g, 0:390].rearrange("p (h d) -> p h d", d=65)[:, :, 64]), r=[po], w=[rd])
                for h in range(NA_H):
                    src = po[:, h // 6, (h % 6) * 65:(h % 6) * 65 + 64]
                    if h % 2 == 0:
                        c.op("dve", lambda e: e.tensor_scalar(out=y[:, h * 64:(h + 1) * 64], in0=src, scalar1=rd[:, h:h + 1], scalar2=None, op0=ALU.mult), r=[po, rd], w=[y])
                    else:
                        c.op("act", lambda e: e.activation(out=y[:, h * 64:(h + 1) * 64], in_=src, func=AF.Copy, scale=rd[:, h:h + 1]), r=[po, rd], w=[y])
                c.dma("sp", T["ymix"].ap()[i * 128:(i + 1) * 128, 0:768], y[:], r=[y], sbuf=y)

            for i in range(16):
                pairs = self.na_pairs[i]
                nk = len(pairs)
                po = pso[i % 2]
                for h in range(NA_H):
                    hp, off = h // 2, (h % 2) * 64
                    ps = psc[n % 2]
                    pt_ = pT[n % 3]
                    n += 1
                    for jj, (j, dl, pat) in enumerate(pairs):
                        reg = ps[:, jj * 128:(jj + 1) * 128]
                        slot = h * len(COMBOS) + COMBOS.index((dl, pat))
                        c.op("pe", lambda e: e.matmul(reg, kT[off:off + 64, hp, j * 128:(j + 1) * 128], qT[off:off + 64, hp, i * 128:(i + 1) * 128], start=True, stop=False),
                             r=[kT, qT], w=[ps], signal=False)
                        c.op("pe", lambda e: e.matmul(reg, self.ident[:], bt[:, slot, :], start=False, stop=True), r=[self.ident, bt], w=[ps], signal=(jj == nk - 1))
                    c.op("act", lambda e: e.activation(out=pt_[:, 0:nk * 128], in_=ps[:, 0:nk * 128], func=AF.Exp), r=[ps], w=[pt_])

                    def pv(i=i, h=h, pairs=pairs, nk=nk, pt_=pt_, po=po):
                        oreg = po[:, h // 6, (h % 6) * 65:(h % 6) * 65 + 65]
                        for jj, (j, dl, pat) in enumerate(pairs):
                            c.op("pe", lambda e: e.matmul(oreg, pt_[:, jj * 128:(jj + 1) * 128], vx[:, j, h, :], start=(jj == 0), stop=(jj == nk - 1)),
                                 r=[pt_, vx], w=[po], signal=(jj == nk - 1))
                        if h == NA_H - 1:
                            epilogue(i, po)
                    if pend:
                        pend.pop(0)()
                    pend.append(pv)
            while pend:
                pend.pop(0)()

    def stage_hyena(self, l):
        c, T = self.c, self.T
        with ExitStack() as st:
            z = [c.sb(st, "z%d" % i, [128, 16, 512], BF16) for i in range(2)]
            h2b = c.sb(st, "h2b", [64, L], BF16)
            w3b = c.sb(st, "w3b", [64, 2048], BF16)
            dsk = [self.load_bc(st, "dsk%d" % o, T["hy_skip_d"], (l * 2 + o) * 512, 512) for o in range(2)]
            c.dma("pool", w3b[:], T["hy_filt_w3"].ap()[l], w=[w3b], sbuf=w3b)
            with ExitStack() as s1:
                cw = [self.load_bc(s1, "cw%d" % k, T["hy_conv_w"], (l * 3 + k) * 1536, 1536) for k in range(3)]
                cb = self.load_bc(s1, "cb", T["hy_conv_b"], l * 1536, 1536)
                pm = [c.sb(s1, "pm%d" % i, [128, 1536], F32) for i in range(2)]
                p0 = [c.sb(s1, "p0%d" % i, [128, 1536], F32) for i in range(2)]
                pp = [c.sb(s1, "pp%d" % i, [128, 1536], F32) for i in range(2)]
                hyp = T["hyp"].ap()
                for tt in range(16):
                    a, b, d = pm[tt % 2], p0[tt % 2], pp[tt % 2]
                    r0 = tt * 128
                    if tt == 0:
                        c.op("dve", lambda e: e.memset(a[:], 0.0), w=[a])
                        c.dma("sp", a[1:128, :], hyp[0:127, :], w=[a], sbuf=a)
                    else:
                        c.dma("sp", a[:], hyp[r0 - 1:r0 + 127, :], w=[a], sbuf=a)
                    c.dma("sp", b[:], hyp[r0:r0 + 128, :], w=[b], sbuf=b)
                    if tt == 15:
                        c.op("dve", lambda e: e.memset(d[:], 0.0), w=[d])
                        c.dma("sp", d[0:127, :], hyp[r0 + 1:r0 + 128, :], w=[d], sbuf=d)
                    else:
                        c.dma("sp", d[:], hyp[r0 + 1:r0 + 129, :], w=[d], sbuf=d)
                    c.op("pool", lambda e: e.tensor_tensor(out=a[:], in0=a[:], in1=cw[0][:], op=ALU.mult), r=[a, cw[0]], w=[a])
                    c.op("dve", lambda e: e.tensor_tensor(out=b[:], in0=b[:], in1=cw[1][:], op=ALU.mult), r=[b, cw[1]], w=[b])
                    c.op("pool", lambda e: e.tensor_tensor(out=d[:], in0=d[:], in1=cw[2][:], op=ALU.mult), r=[d, cw[2]], w=[d])
                    c.op("dve", lambda e: e.tensor_tensor(out=b[:], in0=b[:], in1=cb[:], op=ALU.add), r=[b, cb], w=[b])
                    c.op("pool", lambda e: e.tensor_tensor(out=a[:], in0=a[:], in1=d[:], op=ALU.add), r=[a, d], w=[a])
                    c.op("dve", lambda e: e.tensor_tensor(out=b[:, 0:1024], in0=b[:, 0:1024], in1=a[:, 0:1024], op=ALU.add), r=[a, b], w=[b])
                    c.op("dve", lambda e: e.tensor_tensor(out=z[0][:, tt, :], in0=b[:, 1024:1536], in1=a[:, 1024:1536], op=ALU.add), r=[a, b], w=[z[0]])
                    c.dma("act", T["hyx"].ap()[r0:r0 + 128, :], b[:, 0:1024], r=[b], sbuf=b)
                fT = c.sb(s1, "fT", [17, L], F32)
                w1 = c.sb(s1, "w1", [17, 64], F32)
                w2 = c.sb(s1, "w2", [64, 64], F32)
                cols = c.sb(s1, "cols", [64, 6], F32)
                h1 = c.sb(s1, "h1", [64, L], F32)
                pre = [c.sb(s1, "pre%d" % i, [64, 512], F32) for i in range(2)]
                tmp = [c.sb(s1, "tmpm%d" % i, [64, 512], F32) for i in range(2)]
                pm_ = [c.ps(s1, "pmlp%d" % i, [64, 512], F32) for i in range(2)]
                c.dma("sp", fT[:], T["featsT"].ap(), w=[fT], sbuf=fT)
                c.dma("sp", w1[:], T["hy_filt_w1"].ap()[l], w=[w1], sbuf=w1)
                c.dma("sp", w2[:], T["hy_filt_w2"].ap()[l], w=[w2], sbuf=w2)
                c.dma("sp", cols[:, 0:1], T["hy_sin_freq"].ap()[l].rearrange("(p o) -> p o", o=1), w=[cols], sbuf=cols)
                c.dma("sp", cols[:, 1:2], T["hy_filt_b1"].ap()[l].rearrange("(p o) -> p o", o=1), w=[cols], sbuf=cols)
                c.dma("sp", cols[:, 2:3], T["hy_filt_b2"].ap()[l].rearrange("(p o) -> p o", o=1), w=[cols], sbuf=cols)
                c.op("dve", lambda e: e.tensor_tensor(out=cols[:, 3:4], in0=cols[:, 0:1], in1=cols[:, 1:2], op=ALU.mult), r=[cols], w=[cols])
                c.op("dve", lambda e: e.tensor_tensor(out=cols[:, 4:5], in0=cols[:, 0:1], in1=cols[:, 2:3], op=ALU.mult), r=[cols], w=[cols])

                def sin_layer(wt, kdim, src, dst, fbcol):
                    for tb in range(4):
                        pmm = pm_[tb % 2]
                        x_ = pre[tb % 2]
                        t_ = tmp[tb % 2]
                        c.op("pe", lambda e: e.matmul(pmm[:], wt[0:kdim, :], src[0:kdim, tb * 512:(tb + 1) * 512], start=True, stop=True), r=[wt, src], w=[pmm])
                        c.op("dve", lambda e: e.tensor_scalar(out=x_[:], in0=pmm[:], scalar1=cols[:, 0:1], scalar2=cols[:, fbcol:fbcol + 1], op0=ALU.mult, op1=ALU.add), r=[pmm, cols], w=[x_])
                        c.op("dve", lambda e: e.tensor_scalar(out=t_[:], in0=x_[:], scalar1=PI, scalar2=-2 * PI, op0=ALU.is_gt, op1=ALU.mult), r=[x_], w=[t_])
                        c.op("dve", lambda e: e.tensor_tensor(out=x_[:], in0=x_[:], in1=t_[:], op=ALU.add), r=[x_, t_], w=[x_])
                        c.op("dve", lambda e: e.tensor_scalar(out=t_[:], in0=x_[:], scalar1=-PI, scalar2=2 * PI, op0=ALU.is_lt, op1=ALU.mult), r=[x_], w=[t_])
                        c.op("dve", lambda e: e.tensor_tensor(out=x_[:], in0=x_[:], in1=t_[:], op=ALU.add), r=[x_, t_], w=[x_])
                        c.op("dve", lambda e: e.tensor_scalar(out=x_[:], in0=x_[:], scalar1=3.1415925, scalar2=-3.1415925, op0=ALU.min, op1=ALU.max), r=[x_], w=[x_])
                        c.op("act", lambda e: e.activation(out=dst[:, tb * 512:(tb + 1) * 512], in_=x_[:], func=AF.Sin), r=[x_], w=[dst])
                sin_layer(w1, 17, fT, h1, 3)
                sin_layer(w2, 64, h1, h2b, 4)
            c.barrier()
            with ExitStack() as s2:
                ksum = c.sb(s2, "ksum", [128, 16, 512], BF16)
                kdif = c.sb(s2, "kdif", [128, 16, 512], BF16)
                Pr = c.sb(s2, "Pr", [128, 16, 512], BF16)
                Pi = c.sb(s2, "Pi", [128, 16, 512], BF16)
                tabs = [(c.sb(s2, "tc%d" % i, [128, 16, 128], BF16), c.sb(s2, "ts%d" % i, [128, 16, 128], BF16)) for i in range(2)]
                pk = [c.ps(s2, "pk%d" % i, [128, 512], F32) for i in range(4)]
                pinv = [c.ps(s2, "pinv%d" % i, [128, 512], F32) for i in range(2)]
                pf = c.ps(s2, "pf", [128, 2, 512], F32)
                win = [c.sb(s2, "win%d" % i, [128, 512], F32) for i in range(2)]
                ff = [c.sb(s2, "ff%d" % i, [128, 512], F32) for i in range(2)]
                fb = [c.sb(s2, "fb%d" % i, [128, 512], F32) for i in range(2)]
                ksb = [c.sb(s2, "ksb%d" % i, [128, 2, 512], F32) for i in range(2)]
                ta = [c.sb(s2, "ta%d" % i, [128, 512], F32) for i in range(2)]
                tb_ = [c.sb(s2, "tbb%d" % i, [128, 512], F32) for i in range(2)]
                xg = [c.sb(s2, "xg%d" % i, [128, 512], F32) for i in range(2)]
                og = [c.sb(s2, "og%d" % i, [128, 512], F32) for i in range(2)]
                for o in range(2):
                    zin = z[o]
                    for tt in range(16):
                        w_ = win[tt % 2]
                        f_, b_ = ff[tt % 2], fb[tt % 2]
                        c.dma("sp", w_[:], T["window"].ap()[tt * 128:(tt + 1) * 128, :], w=[w_], sbuf=w_)
                        for dr in range(2):
                            c.op("pe", lambda e: e.matmul(pf[:, dr, :], h2b[:, tt * 128:(tt + 1) * 128], w3b[:, (o * 2 + dr) * 512:(o * 2 + dr + 1) * 512], start=True, stop=True),
                                 r=[h2b, w3b], w=[pf], signal=(dr == 1))
                        c.op("dve", lambda e: e.tensor_tensor(out=f_[:], in0=pf[:, 0, :], in1=w_[:], op=ALU.mult), r=[pf, w_], w=[f_])
                        c.op("dve", lambda e: e.tensor_tensor(out=b_[:], in0=pf[:, 1, :], in1=w_[:], op=ALU.mult), r=[pf, w_], w=[b_])
                        if tt == 0:
                            c.op("dve", lambda e: e.memset(b_[0:1, :], 0.0), w=[b_])
                        c.op("pool", lambda e: e.tensor_tensor(out=ksum[:, tt, :], in0=f_[:], in1=b_[:], op=ALU.add), r=[f_, b_], w=[ksum])
                        c.op("dve", lambda e: e.tensor_tensor(out=kdif[:, tt, :], in0=b_[:], in1=f_[:], op=ALU.subtract), r=[f_, b_], w=[kdif])
                    for fc in range(16):
                        tcb, tsb = tabs[fc % 2]
                        c.dma("sp", tcb[:], T["dftC"].ap()[fc], w=[tcb], sbuf=tcb)
                        c.dma("sp", tsb[:], T["dftS"].ap()[fc], w=[tsb], sbuf=tsb)
                        for gi, (tab, rhs) in enumerate([(tcb, ksum), (tsb, kdif), (tcb, zin), (tsb, zin)]):
                            for k in range(16):
                                c.op("pe", lambda e: e.matmul(pk[gi][:], tab[:, k, :], rhs[:, k, :], start=(k == 0), stop=(k == 15)), r=[tab, rhs], w=[pk[gi]], signal=(k == 15))
                        ks = ksb[fc % 2]
                        c.op("act", lambda e: e.copy(out=ks[:, 0, :], in_=pk[0][:]), r=[pk[0]], w=[ks])
                        c.op("act", lambda e: e.copy(out=ks[:, 1, :], in_=pk[1][:]), r=[pk[1]], w=[ks])
                        q0, q1, q2, q3 = ta[0], ta[1], tb_[0], tb_[1]
                        c.op("dve", lambda e: e.tensor_tensor(out=q0[:], in0=pk[2][:], in1=ks[:, 0, :], op=ALU.mult), r=[pk[2], ks], w=[q0])
                        c.op("dve", lambda e: e.tensor_tensor(out=q1[:], in0=pk[2][:], in1=ks[:, 1, :], op=ALU.mult), r=[pk[2], ks], w=[q1])
                        c.op("dve", lambda e: e.tensor_tensor(out=q2[:], in0=pk[3][:], in1=ks[:, 1, :], op=ALU.mult), r=[pk[3], ks], w=[q2])
                        c.op("dve", lambda e: e.tensor_tensor(out=q3[:], in0=pk[3][:], in1=ks[:, 0, :], op=ALU.mult), r=[pk[3], ks], w=[q3])
                        c.op("pool", lambda e: e.tensor_tensor(out=Pr[:, fc, :], in0=q0[:], in1=q2[:], op=ALU.add), r=[q0, q2], w=[Pr])
                        c.op("pool", lambda e: e.tensor_tensor(out=Pi[:, fc, :], in0=q1[:], in1=q3[:], op=ALU.subtract), r=[q1, q3], w=[Pi])
                    for tt in range(16):
                        tcb, tsb = tabs[tt % 2]
                        c.dma("sp", tcb[:], T["dftCT"].ap()[tt], w=[tcb], sbuf=tcb)
                        c.dma("sp", tsb[:], T["dftnST"].ap()[tt], w=[tsb], sbuf=tsb)
                        pv = pinv[tt % 2]
                        for k in range(16):
                            c.op("pe", lambda e: e.matmul(pv[:], tcb[:, k, :], Pr[:, k, :], start=(k == 0), stop=False), r=[tcb, Pr], w=[pv], signal=False)
                        for k in range(16):
                            c.op("pe", lambda e: e.matmul(pv[:], tsb[:, k, :], Pi[:, k, :], start=False, stop=(k == 15)), r=[tsb, Pi], w=[pv], signal=(k == 15))
                        x_ = xg[tt % 2]
                        a_ = ta[tt % 2]
                        c.dma("sp", x_[:], T["hyx"].ap()[tt * 128:(tt + 1) * 128, o * 512:(o + 1) * 512], w=[x_], sbuf=x_)
                        c.op("pool", lambda e: e.tensor_tensor(out=a_[:], in0=zin[:, tt, :], in1=dsk[o][:], op=ALU.mult), r=[zin, dsk[o]], w=[a_])
                        c.op("dve", lambda e: e.scalar_tensor_tensor(out=a_[:], in0=pv[:], scalar=1.0 / L, in1=a_[:], op0=ALU.mult, op1=ALU.add), r=[pv, a_], w=[a_])
                        if o == 0:
                            c.op("dve", lambda e: e.tensor_tensor(out=z[1][:, tt, :], in0=a_[:], in1=x_[:], op=ALU.mult), r=[a_, x_], w=[z[1]])
                        else:
                            o_ = og[tt % 2]
                            c.op("dve", lambda e: e.tensor_tensor(out=o_[:], in0=a_[:], in1=x_[:], op=ALU.mult), r=[a_, x_], w=[o_])
                            c.dma("act", T["ymix"].ap()[tt * 128:(tt + 1) * 128, 768:1280], o_[:], r=[o_], sbuf=o_)

    def stage_ret(self, l):
        c, T = self.c, self.T
        lgf, lgb = self.consts["_lgf"], self.consts["_lgb"]
        with ExitStack() as st:
            rc = c.sb(st, "rc", [128, L], F32)
            rs_ = c.sb(st, "rs", [128, L], F32)
            rR = c.sb(st, "rR", [128, 128], BF16)
            E = c.sb(st, "E", [128, 512], F32)
            gne = c.sb(st, "gne", [128, 1], F32)
            c.op("dve", lambda e: e.memset(gne[:], GN_EPS), w=[gne])
            c.dma("sp", rc[:], T["rotcos"].ap(), w=[rc], sbuf=rc)
            c.dma("sp", rs_[:], T["rotsin"].ap(), w=[rs_], sbuf=rs_)
            c.dma("sp", rR[:], T["rotR"].ap(), w=[rR], sbuf=rR)
            c.dma("sp", E[:], T["retE"].ap(), w=[E], sbuf=E)
            raw = [c.sb(st, "raw%d" % i, [128, L], BF16) for i in range(2)]
            qk = [[c.sb(st, "qk%d_%d" % (i, j), [128, L], BF16) for j in range(2)] for i in range(2)]
            vh = [c.sb(st, "vh%d" % i, [128, 16, 128], BF16) for i in range(2)]
            dg = [c.sb(st, "dg%d" % i, [128, 4, 512], BF16) for i in range(2)]
            prot = [c.ps(st, "prot%d" % i, [128, 512], F32) for i in range(2)]
            pss = [c.ps(st, "pss%d" % i, [128, 512], F32) for i in range(2)]
            psy = [c.ps(st, "psy%d" % i, [128, 512], F32) for i in range(2)]
            ptt = [c.ps(st, "ptt%d" % i, [128, 4, 128], F32) for i in range(2)]
            t1 = [c.sb(st, "t1_%d" % i, [128, 512], F32) for i in range(2)]
            t2 = [c.sb(st, "t2_%d" % i, [128, 512], F32) for i in range(2)]
            dec = [c.sb(st, "dec%d" % i, [128, 512], BF16) for i in range(3)]
            pT = [c.sb(st, "pT%d" % i, [128, 512], BF16) for i in range(3)]
            yT = [c.sb(st, "yT%d" % i, [128, 512], F32) for i in range(2)]
            gt = [c.sb(st, "gt%d" % i, [128, 4, 128], F32) for i in range(2)]
            sg = [c.sb(st, "sg%d" % i, [128, 4, 128], F32) for i in range(2)]
            yo = [c.sb(st, "yo%d" % i, [128, 4, 128], F32) for i in range(2)]
            stt = [c.sb(st, "stt%d" % i, [128, 4, 6], F32) for i in range(2)]
            mv = [c.sb(st, "mv%d" % i, [128, 4, 4], F32) for i in range(2)]
            n = 0
            pend = []
            ypend = []
            for h in range(6):
                hb = h % 2
                for wi, src in enumerate([T["rqT"], T["rkT"]]):
                    rw = raw[wi]
                    dstb = qk[hb][wi]
                    c.dma("sp", rw[:], src.ap()[h * 128:(h + 1) * 128, :], w=[rw], sbuf=rw)
                    for tb in range(4):
                        sl = slice(tb * 512, (tb + 1) * 512)
                        pr = prot[tb % 2]
                        a_, b_ = t1[tb % 2], t2[tb % 2]
                        c.op("pe", lambda e: e.matmul(pr[:], rR[:], rw[:, sl], start=True, stop=True), r=[rR, rw], w=[pr])
                        c.op("dve", lambda e: e.tensor_tensor(out=a_[:], in0=pr[:], in1=rs_[:, sl], op=ALU.mult), r=[pr, rs_], w=[a_])
                        c.op("pool", lambda e: e.tensor_tensor(out=b_[:], in0=rw[:, sl], in1=rc[:, sl], op=ALU.mult), r=[rw, rc], w=[b_])
                        c.op("dve", lambda e: e.tensor_tensor(out=dstb[:, sl], in0=a_[:], in1=b_[:], op=ALU.add), r=[a_, b_], w=[dstb])
                qr, kr = qk[hb]
                v_ = vh[hb]
                d_ = dg[hb]
                c.dma("sp", v_[:], T["rv"].ap()[:, h * 128:(h + 1) * 128].rearrange("(j p) d -> p j d", p=128), w=[v_], sbuf=v_)
                c.dma("sp", d_[:], T["retD"].ap()[h], w=[d_], sbuf=d_)
                decF, decB = dec[0], dec[1]
                c.op("act", lambda e: e.activation(out=decF[:], in_=E[:], func=AF.Exp, scale=float(lgf[h])), r=[E], w=[decF])
                c.op("act", lambda e: e.activation(out=decB[:], in_=E[:], func=AF.Exp, scale=float(-lgb[h])), r=[E], w=[decB])
                for ib in range(4):
                    py = psy[ib % 2]
                    for j in range(16):
                        ps = pss[n % 2]
                        p_ = pT[n % 3]
                        n += 1
                        c.op("pe", lambda e: e.matmul(ps[:], kr[:, j * 128:(j + 1) * 128], qr[:, ib * 512:(ib + 1) * 512], start=True, stop=True), r=[kr, qr], w=[ps])
                        offv = ib * 512 - j * 128
                        if 0 <= j - ib * 4 < 4:
                            decap = d_[:, j - ib * 4, :]
                            c.op("dve", lambda e: e.tensor_tensor(out=p_[:], in0=ps[:], in1=decap, op=ALU.mult), r=[ps, d_], w=[p_])
                        else:
                            if offv > 0:
                                de, fac = decF, math.exp(float(lgf[h]) * offv)
                            else:
                                de, fac = decB, math.exp(float(-lgb[h]) * offv)
                            c.op("dve", lambda e: e.scalar_tensor_tensor(out=p_[:], in0=ps[:], scalar=float(fac), in1=de[:], op0=ALU.mult, op1=ALU.mult), r=[ps, de], w=[p_])
                        def ymm(py=py, v_=v_, j=j, p_=p_):
                            c.op("pe", lambda e: e.matmul(py[:], v_[:, j, :], p_[:], start=(j == 0), stop=(j == 15)), r=[v_, p_], w=[py], signal=(j == 15))
                        if ypend:
                            ypend.pop(0)()
                        ypend.append(ymm)
                    while ypend:
                        ypend.pop(0)()
                    y_ = yT[ib % 2]
                    c.op("act", lambda e: e.copy(out=y_[:], in_=py[:]), r=[py], w=[y_])

                    def epilogue(h=h, ib=ib, y_=y_):
                        k_ = (ib + 4 * h) % 2
                        pt4 = ptt[k_]
                        for q in range(4):
                            c.op("pe", lambda e: e.transpose(pt4[:, q, :], y_[:, q * 128:(q + 1) * 128], self.identf[:]), r=[y_, self.identf], w=[pt4], signal=(q == 3))
                        g_, s_, o_, st_, m_ = gt[k_], sg[k_], yo[k_], stt[k_], mv[k_]
                        rows = slice(ib * 512, (ib + 1) * 512)
                        c.dma("sp", g_[:], T["rg"].ap()[rows, h * 128:(h + 1) * 128].rearrange("(q p) d -> p q d", p=128), w=[g_], sbuf=g_)
                        c.op("act", lambda e: e.activation(out=s_[:], in_=g_[:], func=AF.Silu), r=[g_], w=[s_])
                        for q in range(4):
                            c.op("dve", lambda e: e.bn_stats(out=st_[:, q, :], in_=pt4[:, q, :]), r=[pt4], w=[st_])
                        for q in range(4):
                            c.op("dve", lambda e: e.bn_aggr(out=m_[:, q, 0:2], in_=st_[:, q, :]), r=[st_], w=[m_])
                        c.op("act", lambda e: e.activation(out=m_[:, :, 2], in_=m_[:, :, 1], func=AF.Sqrt, bias=gne[:, 0:1]), r=[m_, gne], w=[m_])
                        c.op("dve", lambda e: e.reciprocal(out=m_[:, :, 3], in_=m_[:, :, 2]), r=[m_], w=[m_])
                        c.op("dve", lambda e: e.tensor_tensor(out=o_[:], in0=pt4[:], in1=m_[:, :, 0:1].to_broadcast([128, 4, 128]), op=ALU.subtract), r=[pt4, m_], w=[o_])
                        c.op("dve", lambda e: e.tensor_tensor(out=o_[:], in0=o_[:], in1=m_[:, :, 3:4].to_broadcast([128, 4, 128]), op=ALU.mult), r=[o_, m_], w=[o_])
                        c.op("pool", lambda e: e.tensor_tensor(out=o_[:], in0=o_[:], in1=s_[:], op=ALU.mult), r=[o_, s_], w=[o_])
                        c.dma("pool", T["ymix"].ap()[rows, 1280 + h * 128:1280 + (h + 1) * 128].rearrange("(q p) d -> p q d", p=128), o_[:], r=[o_], sbuf=o_)
                    if pend:
                        pend.pop(0)()
                    pend.append(epilogue)
            while pend:
                pend.pop(0)()

    def ret_bias(self, st, val):
        c = self.c
        key = (id(st), round(val, 9))
        if not hasattr(self, "_rb"):
            self._rb = {}
        if key not in self._rb:
            b = c.sb(st, "rb%d" % len(self._rb), [128, 1], F32)
            c.op("pool", lambda e: e.memset(b[:], float(val)), w=[b])
            self._rb[key] = b
        return self._rb[key]

    def stage_outproj(self, l, xsrc):
        c, T = self.c, self.T
        with ExitStack() as st:
            wbufs, pbanks = self.common_alloc(st)
            gain = self.load_bc(st, "gain", T["branch_norm"], l * D, D)
            ginvw = c.sb(st, "ginvw", [128, 3], F32)
            c.dma("sp", ginvw[:], T["ginvw"].ap(), w=[ginvw], sbuf=ginvw)
            xTs = [c.sb(st, "xT%d" % i, [128, 16, 1024], BF16) for i in range(2)]
            xts = [c.sb(st, "xt%d" % i, [128, D], F32) for i in range(2)]
            hbs = [c.sb(st, "hb%d" % i, [128, D], BF16) for i in range(2)]
            ptrs = [c.ps(st, "ptr%d" % i, [128, 4, 128], BF16) for i in range(2)]
            sm = [c.sb(st, "sm%d" % i, [128, 12], F32) for i in range(2)]
            xs = [c.sb(st, "xs%d" % i, [128, 512], F32) for i in range(4)]
            so = [c.sb(st, "so%d" % i, [128, 512], F32) for i in range(4)]
            cnt = [0]
            segs = [(0, 768), (768, 1280), (1280, 2048)]

            def A(half, tt):
                t0 = half * 1024
                xt, hb, s_ = xts[tt % 2], hbs[tt % 2], sm[tt % 2]
                c.dma("sp", xt[:], T["ymix"].ap()[t0 + tt * 128:t0 + (tt + 1) * 128, :], w=[xt], sbuf=xt)
                for g, (a, b) in enumerate(segs):
                    c.op("act", lambda e: e.activation(out=hb[:, a:b], in_=xt[:, a:b], func=AF.Square, accum_out=s_[:, g:g + 1]), r=[xt], w=[hb, s_])
                c.op("dve", lambda e: e.tensor_tensor(out=s_[:, 3:6], in0=s_[:, 0:3], in1=ginvw[:], op=ALU.mult), r=[s_, ginvw], w=[s_])
                c.op("act", lambda e: e.activation(out=s_[:, 6:9], in_=s_[:, 3:6], func=AF.Sqrt, bias=self.epsb[:, 0:1]), r=[s_, self.epsb], w=[s_])
                c.op("dve", lambda e: e.reciprocal(out=s_[:, 9:12], in_=s_[:, 6:9]), r=[s_], w=[s_])
                for g, (a, b) in enumerate(segs):
                    c.op("dve", lambda e: e.scalar_tensor_tensor(out=hb[:, a:b], in0=xt[:, a:b], scalar=s_[:, 9 + g:10 + g], in1=gain[:, a:b], op0=ALU.mult, op1=ALU.mult), r=[xt, s_, gain], w=[hb])

            def B(half, tt):
                self.transpose_into(hbs[tt % 2], xTs[half % 2], tt * 128, ptrs)

            def gemm_half(half, hook):
                self.gemm(xTs[half % 2], 16, 1024, T["w_out"].ap()[l], D, 512, "TM", self.mk_resid_evac(xsrc, half * 1024, xs, so, cnt), wbufs, pbanks, after_w=hook)
            self.run_halves(2, A, B, gemm_half)

    def mk_resid_evac(self, xsrc, t0, xs, so, cnt):
        c, T = self.c, self.T

        def evac(nb, tt, pb):
            x_ = xs[cnt[0] % len(xs)]
            o_ = so[cnt[0] % len(so)]
            cnt[0] += 1
            rows = slice(t0 + tt * 128, t0 + (tt + 1) * 128)
            cols = slice(nb * 512, (nb + 1) * 512)
            c.dma("act", x_[:], xsrc.ap()[rows, cols], r=[self.xres_buf] if xsrc is T["xres"] else [], w=[x_], sbuf=x_)
            c.op("dve", lambda e: e.tensor_tensor(out=o_[:], in0=pb[:], in1=x_[:], op=ALU.add), r=[pb, x_], w=[o_])
            c.dma("sp", T["xres"].ap()[rows, cols], o_[:], r=[o_], sbuf=o_)
        return evac

    def stage_ckv(self, l):
        c, T = self.c, self.T
        with ExitStack() as st:
            wbufs, pbanks = self.common_alloc(st)
            gain = self.load_bc(st, "gain", T["mem_norm"], 0, D)
            xT = c.sb(st, "xT", [128, 16, 256], BF16)
            xts = [c.sb(st, "xt%d" % i, [128, D], F32) for i in range(2)]
            hbs = [c.sb(st, "hb%d" % i, [128, D], BF16) for i in range(2)]
            ptrs = [c.ps(st, "ptr%d" % i, [128, 4, 128], BF16) for i in range(2)]
            smalls = [(c.sb(st, "ss%d" % i, [128, 2], F32), c.sb(st, "rs%d" % i, [128, 1], F32)) for i in range(2)]
            sg = [c.sb(st, "sg%d" % i, [128, 512], BF16) for i in range(4)]
            cnt = [0]
            for tt in range(2):
                xt = xts[tt]
                c.dma("sp", xt[:], T["mem"].ap()[tt * 128:(tt + 1) * 128, :], w=[xt], sbuf=xt)
                self.rms_to_T(xt, gain, hbs[tt], xT, tt * 128, ptrs, smalls[tt])

            def evac_k(fc, tb, pb):
                s_ = sg[cnt[0] % 4]
                cnt[0] += 1
                copy_on(c, c.evac_eng(), s_[:, 0:256], pb[:, 0:256], r=[pb], w=[s_])
                c.dma("sp", T["ckT"].ap()[fc * 128:(fc + 1) * 128, :], s_[:, 0:256], r=[s_], sbuf=s_)

            def evac_v(nb, tt, pb):
                s_ = sg[cnt[0] % 4]
                cnt[0] += 1
                copy_on(c, c.evac_eng(), s_[:], pb[:], r=[pb], w=[s_])
                c.dma("sp", T["cv"].ap()[tt * 128:(tt + 1) * 128, nb * 512:(nb + 1) * 512], s_[:], r=[s_], sbuf=s_)
            wk = T["w_ckv"].ap()[l]
            self.gemm(xT, 16, 256, wk[:, 0:D], D, 512, "FM", evac_k, wbufs, pbanks)
            self.gemm(xT, 16, 256, wk[:, D:2 * D], D, 512, "TM", evac_v, wbufs, pbanks)

    def stage_cq(self, l):
        c, T = self.c, self.T
        with ExitStack() as st:
            wbufs, pbanks = self.common_alloc(st)
            gain = self.load_bc(st, "gain", T["norm_cross"], l * D, D)
            xTs = [c.sb(st, "xT%d" % i, [128, 16, 1024], BF16) for i in range(2)]
            xts = [c.sb(st, "xt%d" % i, [128, D], F32) for i in range(2)]
            hbs = [c.sb(st, "hb%d" % i, [128, D], BF16) for i in range(2)]
            ptrs = [c.ps(st, "ptr%d" % i, [128, 4, 128], BF16) for i in range(2)]
            smalls = [(c.sb(st, "ss%d" % i, [128, 2], F32), c.sb(st, "rs%d" % i, [128, 1], F32)) for i in range(2)]
            sg = [c.sb(st, "sg%d" % i, [128, 512], BF16) for i in range(4)]
            cnt = [0]

            def A(half, tt):
                xt = xts[tt % 2]
                c.dma("sp", xt[:], T["xres"].ap()[half * 1024 + tt * 128:half * 1024 + (tt + 1) * 128, :], w=[xt], sbuf=xt)
                self.rms_to_T(xt, gain, hbs[tt % 2], None, 0, ptrs, smalls[tt % 2], no_T=True)

            def B(half, tt):
                self.transpose_into(hbs[tt % 2], xTs[half % 2], tt * 128, ptrs)

            def gemm_half(half, hook):
                t0 = half * 1024

                def evac(fc, tb, pb):
                    s_ = sg[cnt[0] % 4]
                    cnt[0] += 1
                    copy_on(c, c.evac_eng(), s_[:], pb[:], r=[pb], w=[s_], scale=512 ** -0.5)
                    c.dma("sp", T["cqT"].ap()[fc * 128:(fc + 1) * 128, t0 + tb * 512:t0 + (tb + 1) * 512], s_[:], r=[s_], sbuf=s_)
                self.gemm(xTs[half % 2], 16, 1024, T["w_cq"].ap()[l], D, 512, "FM", evac, wbufs, pbanks, after_w=hook)
            self.run_halves(2, A, B, gemm_half)

    def stage_cattn(self, l):
        c, T = self.c, self.T
        with ExitStack() as st:
            kT = c.sb(st, "kT", [128, 16, 256], BF16)
            v = c.sb(st, "v", [128, 2, D], BF16)
            c.dma("sp", kT[:], T["ckT"].ap().rearrange("(k p) m -> p k m", p=128), w=[kT], sbuf=kT)
            c.dma("sp", v[:], T["cv"].ap().rearrange("(j p) d -> p j d", p=128), w=[v], sbuf=v)
            qTs = [c.sb(st, "qT%d" % i, [128, 16, 512], BF16) for i in range(2)]
            oTs = [c.sb(st, "oT%d" % i, [128, 16, 512], BF16) for i in range(2)]
            pT = [c.sb(st, "pT%d" % i, [128, 2, 512], BF16) for i in range(2)]
            rd = [c.sb(st, "rd%d" % i, [128, 512], F32) for i in range(2)]
            pss = [c.ps(st, "pss%d" % i, [128, 512], F32) for i in range(3)]
            psd = c.ps(st, "psd", [128, 512], F32)
            pso = [c.ps(st, "pso%d" % i, [128, 512], F32) for i in range(3)]
            n = 0
            m = 0
            for tb in range(4):
                q_, o_ = qTs[tb % 2], oTs[tb % 2]
                c.dma("sp", q_[:], T["cqT"].ap()[:, tb * 512:(tb + 1) * 512].rearrange("(k p) t -> p k t", p=128), w=[q_], sbuf=q_)
                for hh in range(4):
                    p_ = pT[hh % 2]
                    for mh in range(2):
                        ps = pss[n % 3]
                        n += 1
                        for dc in range(4):
                            c.op("pe", lambda e: e.matmul(ps[:], kT[:, hh * 4 + dc, mh * 128:(mh + 1) * 128], q_[:, hh * 4 + dc, :], start=(dc == 0), stop=(dc == 3)), r=[kT, q_], w=[ps], signal=(dc == 3))
                        c.op("act", lambda e: e.activation(out=p_[:, mh, :], in_=ps[:], func=AF.Exp), r=[ps], w=[p_])
                    for mh in range(2):
                        c.op("pe", lambda e: e.matmul(psd[:], self.ones[:], p_[:, mh, :], start=(mh == 0), stop=(mh == 1)), r=[self.ones, p_], w=[psd], signal=(mh == 1))
                    r_ = rd[hh % 2]
                    c.op("dve", lambda e: e.reciprocal(out=r_[:], in_=psd[:]), r=[psd], w=[r_])
                    for dvc in range(4):
                        po = pso[m % 3]
                        m += 1
                        for mh in range(2):
                            c.op("pe", lambda e: e.matmul(po[:], v[:, mh, hh * 512 + dvc * 128:hh * 512 + (dvc + 1) * 128], p_[:, mh, :], start=(mh == 0), stop=(mh == 1)), r=[v, p_], w=[po], signal=(mh == 1))
                        c.op("dve", lambda e: e.tensor_tensor(out=o_[:, hh * 4 + dvc, :], in0=po[:], in1=r_[:], op=ALU.mult), r=[po, r_], w=[o_])
                c.dma("pool", T["coT"].ap()[:, tb * 512:(tb + 1) * 512].rearrange("(k p) t -> p k t", p=128), o_[:], r=[o_], sbuf=o_)

    def stage_co(self, l):
        c, T = self.c, self.T
        with ExitStack() as st:
            wbufs, pbanks = self.common_alloc(st)
            xT = c.sb(st, "xT", [128, 16, 1024], BF16)
            xs = [c.sb(st, "xs%d" % i, [128, 512], F32) for i in range(4)]
            so = [c.sb(st, "so%d" % i, [128, 512], F32) for i in range(4)]
            cnt = [0]
            for half in range(2):
                t0 = half * 1024
                c.dma("sp", xT[:], T["coT"].ap()[:, t0:t0 + 1024].rearrange("(k p) t -> p k t", p=128), w=[xT], sbuf=xT)
                self.gemm(xT, 16, 1024, T["w_co"].ap()[l], D, 512, "TM", self.mk_resid_evac(T["xres"], t0, xs, so, cnt), wbufs, pbanks)

    def stage_moe_h(self, l):
        c, T = self.c, self.T
        with ExitStack() as st:
            self.common_alloc(st, nw=0, npb=0)
            gain = self.load_bc(st, "gain", T["norm_moe"], l * D, D)
            wr = c.sb(st, "wr", [128, 16, NE], F32)
            wrh = c.sb(st, "wrh", [128, 16, NE], BF16)
            wrl = c.sb(st, "wrl", [128, 16, NE], BF16)
            wtmp = c.sb(st, "wtmp", [128, 16, NE], F32)
            c.dma("sp", wr[:], T["w_router"].ap()[l].rearrange("(k p) e -> p k e", p=128), w=[wr], sbuf=wr)
            c.op("dve", lambda e: e.tensor_copy(out=wrh[:], in_=wr[:]), r=[wr], w=[wrh])
            c.op("dve", lambda e: e.tensor_copy(out=wtmp[:], in_=wrh[:]), r=[wrh], w=[wtmp])
            c.op("dve", lambda e: e.tensor_tensor(out=wrl[:], in0=wr[:], in1=wtmp[:], op=ALU.subtract), r=[wr, wtmp], w=[wrl])
            xts = [c.sb(st, "xt%d" % i, [128, D], F32) for i in range(2)]
            hfs = [c.sb(st, "hf%d" % i, [128, D], F32) for i in range(2)]
            hbs = [c.sb(st, "hb%d" % i, [128, D], BF16) for i in range(2)]
            hls = [c.sb(st, "hl%d" % i, [128, D], BF16) for i in range(2)]
            hT = [c.sb(st, "hT%d" % i, [128, 16, 128], BF16) for i in range(2)]
            lT = [c.sb(st, "lT%d" % i, [128, 16, 128], BF16) for i in range(2)]
            ptrs = [c.ps(st, "ptr%d" % i, [128, 4, 128], BF16) for i in range(2)]
            pl = [c.ps(st, "pl%d" % i, [128, NE], F32) for i in range(2)]
            pa = c.ps(st, "pa", [NE, 128], F32)
            smalls = [(c.sb(st, "ss%d" % i, [128, 2], F32), c.sb(st, "rs%d" % i, [128, 1], F32)) for i in range(2)]
            ex = [c.sb(st, "ex%d" % i, [128, NE], F32) for i in range(2)]
            se = [c.sb(st, "se%d" % i, [128, 2], F32) for i in range(2)]
            af = [c.sb(st, "af%d" % i, [128, NE], F32) for i in range(2)]
            aT = c.sb(st, "aT", [NE, L], F32)
            for tt in range(16):
                xt, hf, hb, hl = xts[tt % 2], hfs[tt % 2], hbs[tt % 2], hls[tt % 2]
                ss, rstd = smalls[tt % 2]
                c.dma("sp", xt[:], T["xres"].ap()[tt * 128:(tt + 1) * 128, :], r=[self.xres_buf], w=[xt], sbuf=xt)
                c.op("act", lambda e: e.activation(out=hb[:], in_=xt[:], func=AF.Square, accum_out=ss[:, 0:1]), r=[xt], w=[hb, ss])
                c.op("act", lambda e: e.activation(out=ss[:, 1:2], in_=ss[:, 0:1], func=AF.Sqrt, scale=1.0 / D, bias=self.epsb[:, 0:1]), r=[ss, self.epsb], w=[ss])
                c.op("dve", lambda e: e.reciprocal(out=rstd[:, 0:1], in_=ss[:, 1:2]), r=[ss], w=[rstd])
                c.op("dve", lambda e: e.scalar_tensor_tensor(out=hf[:], in0=xt[:], scalar=rstd[:, 0:1], in1=gain[:], op0=ALU.mult, op1=ALU.mult), r=[xt, rstd, gain], w=[hf])
                c.op("act", lambda e: e.copy(out=hb[:], in_=hf[:]), r=[hf], w=[hb])
                c.op("pool", lambda e: e.tensor_tensor(out=hl[:], in0=hf[:], in1=hb[:], op=ALU.subtract), r=[hf, hb], w=[hl])
                c.dma("pool", T["hmoe"].ap()[tt * 128:(tt + 1) * 128, :], hb[:], r=[hb], w=[self.hmoe_buf], sbuf=hb)
                h_, l_ = hT[tt % 2], lT[tt % 2]
                self.transpose_into(hb, h_, 0, ptrs)
                self.transpose_into(hl, l_, 0, ptrs)
                p_ = pl[tt % 2]
                combos = [(h_, wrh), (l_, wrh), (h_, wrl)]
                for ci, (a_, w_) in enumerate(combos):
                    for k in range(16):
                        c.op("pe", lambda e: e.matmul(p_[:], a_[:, k, :], w_[:, k, :], start=(ci == 0 and k == 0), stop=(ci == 2 and k == 15)), r=[a_, w_], w=[p_], signal=(ci == 2 and k == 15))
                e_, s_, a2 = ex[tt % 2], se[tt % 2], af[tt % 2]
                c.op("act", lambda e: e.activation(out=e_[:], in_=p_[:], func=AF.Exp, accum_out=s_[:, 0:1]), r=[p_], w=[e_, s_])
                c.op("dve", lambda e: e.reciprocal(out=s_[:, 1:2], in_=s_[:, 0:1]), r=[s_], w=[s_])
                c.op("dve", lambda e: e.tensor_scalar(out=a2[:], in0=e_[:], scalar1=s_[:, 1:2], scalar2=None, op0=ALU.mult), r=[e_, s_], w=[a2])
                c.dma("pool", T["affc"].ap()[tt * 128:(tt + 1) * 128, :], a2[:], r=[a2], sbuf=a2)
                c.op("pe", lambda e: e.transpose(pa[:], a2[:], self.identf[:]), r=[a2, self.identf], w=[pa])
                c.op("act", lambda e: e.copy(out=aT[:, tt * 128:(tt + 1) * 128], in_=pa[:]), r=[pa], w=[aT])
            c.dma("sp", T["affT"].ap(), aT[:], r=[aT], w=[self.afft_buf], sbuf=aT)

    def stage_moe_x(self, l):
        c, T = self.c, self.T
        with ExitStack() as st:
            wbufs, pbanks = self.common_alloc(st, nw=4, npb=4)
            io = c.sb(st, "io", [128, 256], F32)
            pj = c.sb(st, "pj", [128, 16, 4], F32)
            pjb = [c.sb(st, "pjb%d" % i, [128, 16, 4], BF16) for i in range(2)]
            Sall = [c.sb(st, "Sall%d" % i, [128, 16, 256], BF16) for i in range(2)]
            c.dma("sp", io[:], T["iota256"].ap(), w=[io], sbuf=io)
            c.dma("sp", pj[:, :, 0:3], T["pj"].ap(), w=[pj], sbuf=pj)
            affc = c.sb(st, "affc", [128, 16, NE], F32)
            c.dma("sp", affc[:], T["affc"].ap().rearrange("(j p) e -> p j e", p=128), w=[affc], sbuf=affc)
            arow = [c.sb(st, "arow%d" % i, [128, L], F32) for i in range(2)]
            junk = c.sb(st, "junk", [128, L], BF16)
            rank = [c.sb(st, "rank%d" % i, [128, 16], F32) for i in range(2)]
            pidx = [c.ps(st, "pidx%d" % i, [128, 2, 4], F32) for i in range(2)]
            idf = [c.sb(st, "idf%d" % i, [128, 2, 6], F32) for i in range(2)]
            idxi = [[c.sb(st, "idx%d_%d" % (i, ch), [128, 1], I32) for ch in range(2)] for i in range(2)]
            xe = [c.sb(st, "xe%d" % i, [128, D], BF16) for i in range(2)]
            xeT = [c.sb(st, "xeT%d" % i, [128, 16, 256], BF16) for i in range(2)]
            hm = [c.sb(st, "hm%d" % i, [128, D], BF16) for i in range(2)]
            hmT = c.sb(st, "hmT", [128, 16, 256], BF16)
            sa = [c.sb(st, "sa%d" % i, [128, 512], F32) for i in range(2)]
            ysb = [c.sb(st, "ysb%d" % i, [128, D], F32) for i in range(2)]
            ptrs = [c.ps(st, "ptr%d" % i, [128, 4, 128], BF16) for i in range(2)]

            def rank_thunks(ex_):
                eb = ex_ % 2
                ar, rk = arow[eb], rank[eb]
                th = []

                def t0():
                    c.dma("sp", ar[:], row_bc(T["affT"], ex_ * L, L), r=[self.afft_buf], w=[ar], sbuf=ar)
                th.append(t0)
                for j in range(16):
                    def tj(j=j):
                        c.op("dve", lambda e: e.tensor_scalar(out=junk[:], in0=ar[:], scalar1=affc[:, j, ex_:ex_ + 1], scalar2=0.0, op0=ALU.is_gt, op1=ALU.add, accum_out=rk[:, j:j + 1]),
                             r=[ar, affc], w=[junk, rk])
                    th.append(tj)
                return th

            def slots(ex_):
                eb = ex_ % 2
                rk, S_, pb_, pi_, f_ = rank[eb], Sall[eb], pjb[eb], pidx[eb], idf[eb]
                c.op("dve", lambda e: e.tensor_copy(out=pj[:, :, 2], in_=affc[:, :, ex_]), r=[affc], w=[pj])
                c.op("dve", lambda e: e.tensor_copy(out=pb_[:, :, 0:3], in_=pj[:, :, 0:3]), r=[pj], w=[pb_])
                c.op("dve", lambda e: e.tensor_copy(out=pj[:, :, 3], in_=pb_[:, :, 2]), r=[pb_], w=[pj])
                c.op("dve", lambda e: e.tensor_tensor(out=pb_[:, :, 3], in0=pj[:, :, 2], in1=pj[:, :, 3], op=ALU.subtract), r=[pj], w=[pb_])
                for j in range(16):
                    c.op("dve", lambda e: e.tensor_scalar(out=S_[:, j, :], in0=io[:], scalar1=rk[:, j:j + 1], scalar2=None, op0=ALU.is_equal), r=[io, rk], w=[S_])
                for ch in range(2):
                    for j in range(16):
                        c.op("pe", lambda e: e.matmul(pi_[:, ch, 0:4], S_[:, j, ch * 128:(ch + 1) * 128], pb_[:, j, :], start=(j == 0), stop=(j == 15)), r=[S_, pb_], w=[pi_], signal=(j == 15))
                c.op("dve", lambda e: e.tensor_copy(out=f_[:, :, 0:4], in_=pi_[:, :, 0:4]), r=[pi_], w=[f_])
                c.op("dve", lambda e: e.scalar_tensor_tensor(out=f_[:, :, 4], in0=f_[:, :, 1], scalar=128.0, in1=f_[:, :, 0], op0=ALU.mult, op1=ALU.add), r=[f_], w=[f_])
                c.op("dve", lambda e: e.tensor_tensor(out=f_[:, :, 5], in0=f_[:, :, 2], in1=f_[:, :, 3], op=ALU.add), r=[f_], w=[f_])
                for ch in range(2):
                    ix = idxi[eb][ch]
                    c.op("dve", lambda e: e.tensor_copy(out=ix[:], in_=f_[:, ch, 4:5]), r=[f_], w=[ix])
                    x_ = xe[ch]
                    c.custom_dma("pool", lambda e: e.indirect_dma_start(out=x_[:], out_offset=None, in_=T["hmoe"].ap(), in_offset=bass.IndirectOffsetOnAxis(ap=ix[:, 0:1], axis=0)),
                                 r=[ix, self.hmoe_buf], w=[x_], sbuf=x_)

            def gather_T(ex_):
                for ch in range(2):
                    self.transpose_into(xe[ch], xeT[ex_ % 2], ch * 128, ptrs)

            def scatter(ex_):
                eb = ex_ % 2
                for ch in range(2):
                    ix = idxi[eb][ch]
                    y_ = ysb[ch]
                    c.custom_dma("pool", lambda e: e.indirect_dma_start(out=T["xres"].ap(), out_offset=bass.IndirectOffsetOnAxis(ap=ix[:, 0:1], axis=0), in_=y_[:], in_offset=None, compute_op=ALU.add),
                                 r=[ix, y_, self.xres_buf], w=[self.xres_buf], sbuf=y_)

            for t in rank_thunks(0):
                t()
            slots(0)
            gather_T(0)
            for ex_ in range(NE):
                eb = ex_ % 2
                xT_ = xeT[eb]
                f_ = idf[eb]
                wg = T["w_gate"].ap()[l, ex_]
                wu = T["w_up"].ap()[l, ex_]
                wd = T["w_down"].ap()[l, ex_]
                nxt = rank_thunks(ex_ + 1) if ex_ + 1 < NE else []
                wcount = [0]

                def after_w(_nb):
                    wcount[0] += 1
                    if wcount[0] == 3 and ex_ > 0:
                        scatter(ex_ - 1)
                for nb in range(4):
                    def ev_gate(_nb, tt, pb, nb=nb):
                        s2 = sa[tt]
                        c.op("act", lambda e: e.activation(out=s2[:], in_=pb[:], func=AF.Silu), r=[pb], w=[s2])

                    def ev_up(_nb, tt, pb, nb=nb):
                        s2 = sa[tt]
                        c.op("dve", lambda e: e.tensor_tensor(out=hm[tt][:, nb * 512:(nb + 1) * 512], in0=pb[:], in1=s2[:], op=ALU.mult), r=[pb, s2], w=[hm[tt]])
                    self.gemm(xT_, 16, 256, wg[:, nb * 512:(nb + 1) * 512], 512, 512, "TM", ev_gate, wbufs, pbanks, after_w=after_w)
                    for _ in range(2):
                        if nxt:
                            nxt.pop(0)()
                    self.gemm(xT_, 16, 256, wu[:, nb * 512:(nb + 1) * 512], 512, 512, "TM", ev_up, wbufs, pbanks, after_w=after_w)
                    for _ in range(3):
                        if nxt:
                            nxt.pop(0)()
                while nxt:
                    nxt.pop(0)()
                for ch in range(2):
                    self.transpose_into(hm[ch], hmT, ch * 128, ptrs)

                def after_wd(nb):
                    if nb == 2 and ex_ + 1 < NE:
                        slots(ex_ + 1)

                def ev_down(nb, tt, pb):
                    y_ = ysb[tt]
                    if nb % 2 == 0:
                        c.op("act", lambda e: e.activation(out=y_[:, nb * 512:(nb + 1) * 512], in_=pb[:], func=AF.Copy, scale=f_[:, tt, 5:6]), r=[pb, f_], w=[y_])
                    else:
                        c.op("dve", lambda e: e.tensor_scalar(out=y_[:, nb * 512:(nb + 1) * 512], in0=pb[:], scalar1=f_[:, tt, 5:6], scalar2=None, op0=ALU.mult), r=[pb, f_], w=[y_])
                self.gemm(hmT, 16, 256, wd, D, 512, "TM", ev_down, wbufs, pbanks, after_w=after_wd)
                if ex_ + 1 < NE:
                    gather_T(ex_ + 1)
            scatter(NE - 1)

    def stage_final(self):
        c, T = self.c, self.T
        with ExitStack() as st:
            self.common_alloc(st, nw=0, npb=0)
            gain = self.load_bc(st, "gain", T["final_norm"], 0, D)
            xts = [c.sb(st, "xt%d" % i, [128, D], F32) for i in range(3)]
            jk = [c.sb(st, "jk%d" % i, [128, D], BF16) for i in range(2)]
            os_ = [c.sb(st, "os%d" % i, [128, D], F32) for i in range(2)]
            sm = [c.sb(st, "sm%d" % i, [128, 3], F32) for i in range(2)]
            for tt in range(16):
                xt, j_, o_, s_ = xts[tt % 3], jk[tt % 2], os_[tt % 2], sm[tt % 2]
                c.dma("sp", xt[:], T["xres"].ap()[tt * 128:(tt + 1) * 128, :], r=[self.xres_buf], w=[xt], sbuf=xt)
                c.op("act", lambda e: e.activation(out=j_[:], in_=xt[:], func=AF.Square, accum_out=s_[:, 0:1]), r=[xt], w=[j_, s_])
                c.op("act", lambda e: e.activation(out=s_[:, 1:2], in_=s_[:, 0:1], func=AF.Sqrt, scale=1.0 / D, bias=self.epsb[:, 0:1]), r=[s_, self.epsb], w=[s_])
                c.op("dve", lambda e: e.reciprocal(out=s_[:, 2:3], in_=s_[:, 1:2]), r=[s_], w=[s_])
                c.op("dve", lambda e: e.scalar_tensor_tensor(out=o_[:], in0=xt[:], scalar=s_[:, 2:3], in1=gain[:], op0=ALU.mult, op1=ALU.mult), r=[xt, s_, gain], w=[o_])
                c.dma("pool", T["out"].ap()[tt * 128:(tt + 1) * 128, :], o_[:], r=[o_], sbuf=o_)


_CACHE = {}


def host_inputs(inputs, consts, b):
    m = {"x": np.ascontiguousarray(inputs["x"][b]), "mem": np.ascontiguousarray(inputs["mem"][b])}
    for k in WEIGHT_SHAPES:
        if k == "rpbY":
            continue
        m[k] = np.ascontiguousarray(np.asarray(inputs[k], np.float32))
    m["rpbY"] = rpb_layout(np.asarray(inputs["na_rpb"], np.float32))
    for k in CONST_DTYPES:
        m[k] = consts[k]
    return m


def kernel(**inputs):
    inputs = {k: np.asarray(v) for k, v in inputs.items()}
    if "prog" not in _CACHE:
        p = Prog()
        p.build()
        _CACHE["prog"] = p
    p = _CACHE["prog"]
    B = inputs["x"].shape[0]
    in_maps = [host_inputs(inputs, p.consts, b) for b in range(B)]
    res = run_bass_kernel_spmd(p.nc, in_maps, core_ids=[0, 2, 4, 6][:B])
    out = np.stack([np.asarray(r["out"], np.float32) for r in res.results], axis=0)
    return out
```

```python
import math
import numpy as np
import ml_dtypes
import concourse.bass as bass
import concourse.mybir as mybir
from concourse.bass_utils import run_bass_kernel_spmd
from contextlib import ExitStack

dt = mybir.dt
F32, BF16, I32 = dt.float32, dt.bfloat16, dt.int32
AF = mybir.ActivationFunctionType
ALU = mybir.AluOpType
AX = mybir.AxisListType

SAME_ENGINE_SYNC = True

D = 2048
L = 2048
DEPTH = 2
NMEM = 256
NA_H = 12
P_IN = 6912
NE = 16
CAP = 256
RMS_EPS = 1e-6
GN_EPS = 1e-5
PI = math.pi


class Sem:
    def __init__(self, h):
        self.h = h
        self.n = 0


class Buf:
    def __init__(self, t=None, name=""):
        self.t = t
        self.name = name
        self.w = None
        self.r = {}
        self.dsem = None

    def __getitem__(self, idx):
        return self.t[idx]


class Eng:
    def __init__(self, h, sem, name):
        self.h = h
        self.sem = sem
        self.name = name
        self.known = {}


class Ctx:
    def __init__(self, nc):
        self.nc = nc
        self.es = ExitStack()
        self.eng = {}
        for name, h in (("pe", nc.tensor), ("dve", nc.vector), ("act", nc.scalar),
                        ("pool", nc.gpsimd), ("sp", nc.sync)):
            s = Sem(self.es.enter_context(nc.semaphore("es_" + name)))
            self.eng[name] = Eng(h, s, name)
        self.free_sems = []
        self.all_dsems = []
        self.stage_bufs = []
        self.ninst = 0
        self.flip = 0

    def new_dsem(self):
        if self.free_sems:
            return self.free_sems.pop()
        s = Sem(self.es.enter_context(self.nc.semaphore("ds%d" % len(self.all_dsems))))
        self.all_dsems.append(s)
        return s

    def sb(self, st, name, shape, dtype):
        self.uid = getattr(self, "uid", 0) + 1
        name = "s%d_%s" % (self.uid, name)
        t = st.enter_context(self.nc.sbuf_tensor(name, list(shape), dtype))
        b = Buf(t, name)
        self.stage_bufs.append(b)
        return b

    def ps(self, st, name, shape, dtype=F32):
        self.uid = getattr(self, "uid", 0) + 1
        name = "p%d_%s" % (self.uid, name)
        t = st.enter_context(self.nc.psum_tensor(name, list(shape), dtype))
        b = Buf(t, name)
        self.stage_bufs.append(b)
        return b

    def _waits(self, E, r, w):
        waits = {}

        def need(s, v):
            if waits.get(s, 0) < v:
                waits[s] = v
        for b in r:
            if b.w is not None:
                need(*b.w)
        for b in w:
            if b.w is not None:
                need(*b.w)
            for s, v in b.r.items():
                need(s, v)
        for s, v in waits.items():
            if E.known.get(s, 0) >= v:
                continue
            if s is E.sem and (E.name == "pe" or not SAME_ENGINE_SYNC):
                continue
            E.h.wait_ge(s.h, v)
            self.ninst += 1
            E.known[s] = v

    def _record(self, dep, r, w):
        s, v = dep
        for b in r:
            if b.r.get(s, 0) < v:
                b.r[s] = v
        for b in w:
            b.w = dep
            b.r = {}

    def op(self, eng, fn, r=(), w=(), signal=True):
        E = self.eng[eng]
        self._waits(E, r, w)
        inst = fn(E.h)
        self.ninst += 1
        if signal:
            E.sem.n += 1
            inst.then_inc(E.sem.h, 1)
            dep = (E.sem, E.sem.n)
        else:
            dep = (E.sem, E.sem.n + 1)
        self._record(dep, r, w)
        return inst

    def dma(self, q, out, in_, r=(), w=(), sbuf=None, **kw):
        return self.custom_dma(q, lambda e: e.dma_start(out=out, in_=in_, **kw), r=r, w=w, sbuf=sbuf)

    def custom_dma(self, q, fn, r=(), w=(), sbuf=None):
        E = self.eng[q]
        self._waits(E, r, w)
        inst = fn(E.h)
        self.ninst += 1
        if sbuf.dsem is None:
            sbuf.dsem = self.new_dsem()
        s = sbuf.dsem
        s.n += 16
        inst.then_inc(s.h, 16)
        self._record((s, s.n), r, w)
        return inst

    def barrier(self):
        targets = [(e.sem, e.sem.n) for e in self.eng.values() if e.sem.n > 0]
        targets += [(s, s.n) for s in self.all_dsems if s.n > 0]
        for E in self.eng.values():
            for s, v in targets:
                if s is E.sem or E.known.get(s, 0) >= v:
                    continue
                E.h.wait_ge(s.h, v)
                self.ninst += 1
                E.known[s] = v

    def end_stage(self, dram_bufs=()):
        self.barrier()
        for b in self.stage_bufs:
            if b.dsem is not None:
                self.free_sems.append(b.dsem)
                b.dsem = None
        self.stage_bufs = []
        for b in dram_bufs:
            b.w = None
            b.r = {}

    def evac_eng(self):
        self.flip ^= 1
        return "act" if self.flip else "dve"


def copy_on(c, eng, out, in_, r, w, scale=None):
    if eng == "act":
        if scale is None:
            c.op("act", lambda e: e.copy(out=out, in_=in_), r=r, w=w)
        else:
            c.op("act", lambda e: e.mul(out, in_, float(scale)), r=r, w=w)
    else:
        if scale is None:
            c.op(eng, lambda e: e.tensor_copy(out=out, in_=in_), r=r, w=w)
        else:
            c.op(eng, lambda e: e.tensor_scalar(out=out, in0=in_, scalar1=float(scale), scalar2=None, op0=ALU.mult), r=r, w=w)


def bf(a):
    return np.ascontiguousarray(np.asarray(a, np.float32).astype(ml_dtypes.bfloat16))


def na_geometry():
    rows = 32

    def rs(r):
        return min(max(r - 4, 0), rows - 8)
    pairs = {}
    pats = {}
    c = np.arange(64)
    cs = np.clip(c - 8, 0, 48)
    colok = (c[:, None] >= cs[None, :]) & (c[:, None] < cs[None, :] + 16)
    tiles = []
    for i in range(16):
        lst = []
        for j in range(16):
            v = [[rs(2 * i + b) <= 2 * j + a < rs(2 * i + b) + 8 for b in range(2)] for a in range(2)]
            if not any(v[0] + v[1]):
                continue
            key = tuple(v[0] + v[1])
            if key not in pats:
                pats[key] = len(tiles)
                m = np.full((128, 128), -30000.0, np.float32)
                for a in range(2):
                    for b in range(2):
                        if v[a][b]:
                            blk = np.where(colok, 0.0, -30000.0)
                            m[a * 64:(a + 1) * 64, b * 64:(b + 1) * 64] = blk
                tiles.append(m)
            lst.append((j, 2 * j - 2 * i, pats[key]))
        pairs[i] = lst
    masks = np.stack(tiles, 0)
    return pairs, masks


def make_consts():
    cst = {}
    t = np.arange(L, dtype=np.float64)
    f = np.arange(L, dtype=np.float64)
    th = 2.0 * np.pi * (f[None, :] + 0.5) * t[:, None] / (2 * L)
    C = np.cos(th)
    S = np.sin(th)
    def fwd_layout(M):
        return M.reshape(16, 128, 16, 128).transpose(2, 1, 0, 3)
    cst["dftC"] = bf(fwd_layout(C))
    cst["dftS"] = bf(fwd_layout(S))
    def inv_layout(M):
        MT = M.T
        return MT.reshape(16, 128, 16, 128).transpose(2, 1, 0, 3)
    cst["dftCT"] = bf(inv_layout(C))
    cst["dftnST"] = bf(inv_layout(-S))
    tt = np.arange(L, dtype=np.float32)
    t01 = tt / (L - 1)
    bands = np.linspace(1e-4, 8 - 1, 8, dtype=np.float32)
    ang = (2.0 * np.float32(np.pi)) * (tt[:, None] / L) * bands[None, :]
    feats = np.concatenate([t01[:, None], np.cos(ang), -np.sin(ang)], axis=-1).astype(np.float32)
    cst["featsT"] = np.ascontiguousarray(feats.T)
    min_decay = math.log(1e-2) / 1.5
    max_decay = math.log(1e-2) / 0.3
    deltas = np.abs(np.linspace(min_decay, max_decay, 512, dtype=np.float32))
    cst["window"] = np.exp(-t01[:, None] * deltas[None, :]).astype(np.float32)
    inv = 1.0 / (10000.0 ** np.linspace(0.0, 1.0, 64, dtype=np.float32))
    angr = tt[:, None] * inv[None, :]
    cosT = np.cos(angr).T.astype(np.float32)
    sinT = np.sin(angr).T.astype(np.float32)
    cst["rotcos"] = np.ascontiguousarray(np.concatenate([cosT, cosT], 0))
    cst["rotsin"] = np.ascontiguousarray(np.concatenate([sinT, sinT], 0))
    R = np.zeros((128, 128), np.float32)
    for m in range(64):
        R[m + 64, m] = -1.0
    for m in range(64, 128):
        R[m - 64, m] = 1.0
    cst["rotR"] = bf(R)
    hidx = np.arange(6, dtype=np.float64)
    lgf = np.log1p(-np.exp2(-5.0 - hidx))
    lgb = np.log1p(-np.exp2(-5.5 - hidx))
    dd = np.zeros((6, 4, 128, 512), np.float32)
    jj = np.arange(128)[:, None]
    ii = np.arange(512)[None, :]
    for h in range(6):
        for pos in range(4):
            diff = ii - (jj + pos * 128)
            dd[h, pos] = np.where(diff >= 0, np.exp(lgf[h] * np.maximum(diff, 0)), np.exp(lgb[h] * np.maximum(-diff, 0)))
    cst["retD"] = bf(dd.transpose(0, 2, 1, 3))
    cst["retE"] = (ii - jj).astype(np.float32)
    cst["_lgf"] = lgf
    cst["_lgb"] = lgb
    pairs, masks = na_geometry()
    cst["namask"] = bf(masks.transpose(1, 0, 2))
    J = np.zeros((128, 128), np.float32)
    for a in range(2):
        for k in range(64):
            J[a * 64 + 63 - k, a * 64 + k] = 1.0
    cst["naJ"] = bf(J)
    cst["ident"] = bf(np.eye(128))
    cst["identf"] = np.eye(128, dtype=np.float32)
    cst["ones"] = bf(np.ones((128, 128)))
    cst["iota256"] = np.tile(np.arange(256, dtype=np.float32)[None, :], (128, 1))
    pj = np.zeros((128, 16, 3), np.float32)
    pj[:, :, 0] = np.arange(128)[:, None]
    pj[:, :, 1] = np.arange(16)[None, :]
    cst["pj"] = pj
    gw = np.zeros((128, 3), np.float32)
    gw[:, 0] = 1.0 / 768
    gw[:, 1] = 1.0 / 512
    gw[:, 2] = 1.0 / 768
    cst["ginvw"] = gw
    return cst, pairs


def rpb_layout(na_rpb):
    Y = np.zeros((DEPTH, NA_H, 15, 128), np.float32)
    for m in range(15):
        dri = 14 - m
        Y[:, :, m, 48:79] = na_rpb[:, :, dri, ::-1]
    return Y


CONST_DTYPES = {"dftC": BF16, "dftS": BF16, "dftCT": BF16, "dftnST": BF16, "featsT": F32, "window": F32,
                "rotcos": F32, "rotsin": F32, "rotR": BF16, "retD": BF16, "retE": F32, "namask": BF16,
                "naJ": BF16, "ident": BF16, "identf": F32, "ones": BF16, "iota256": F32, "pj": F32,
                "ginvw": F32}

WEIGHT_SHAPES = {
    "norm_mix": (DEPTH, D), "w_in": (DEPTH, D, P_IN), "rpbY": (DEPTH, NA_H, 15, 128),
    "hy_conv_w": (DEPTH, 3, 1536), "hy_conv_b": (DEPTH, 1536), "hy_filt_w1": (DEPTH, 17, 64),
    "hy_filt_b1": (DEPTH, 64), "hy_filt_w2": (DEPTH, 64, 64), "hy_filt_b2": (DEPTH, 64),
    "hy_filt_w3": (DEPTH, 64, 2048), "hy_sin_freq": (DEPTH, 64), "hy_skip_d": (DEPTH, 2, 512),
    "branch_norm": (DEPTH, D), "w_out": (DEPTH, D, D), "norm_cross": (DEPTH, D), "mem_norm": (D,),
    "w_cq": (DEPTH, D, D), "w_ckv": (DEPTH, D, 2 * D), "w_co": (DEPTH, D, D), "norm_moe": (DEPTH, D),
    "w_router": (DEPTH, D, NE), "w_gate": (DEPTH, NE, D, D), "w_up": (DEPTH, NE, D, D),
    "w_down": (DEPTH, NE, D, D), "final_norm": (D,),
}


def row_bc(t, off, n, parts=128):
    return bass.AP(t, off, [[0, parts], [1, n]])


class Prog:
    def __init__(self, debug=False, stop_after=None, nlayers=DEPTH):
        self.debug = debug
        self.stop_after = stop_after
        self.nlayers = nlayers
        self.consts, self.na_pairs = make_consts()
        self.npat = self.consts["namask"].shape[1]
        nc = bass.Bass("TRN2", target_bir_lowering=False)
        self.nc = nc
        self.c = Ctx(nc)
        T = {}
        T["x"] = nc.dram_tensor("x", [L, D], F32, kind="ExternalInput")
        T["mem"] = nc.dram_tensor("mem", [NMEM, D], F32, kind="ExternalInput")
        for k, shp in WEIGHT_SHAPES.items():
            T[k] = nc.dram_tensor(k, list(shp), F32, kind="ExternalInput")
        for k, dty in CONST_DTYPES.items():
            T[k] = nc.dram_tensor(k, list(self.consts[k].shape), dty, kind="ExternalInput")
        T["out"] = nc.dram_tensor("out", [L, D], F32, kind="ExternalOutput")
        sk = "ExternalOutput" if debug else "Internal"
        for name, shp, dty in [
            ("xres", [L, D], F32), ("naqT", [768, L], BF16), ("nakT", [768, L], BF16), ("nav", [L, 768], BF16),
            ("hyp", [L, 1536], F32), ("hyx", [L, 1024], F32), ("rqT", [768, L], BF16), ("rkT", [768, L], BF16),
            ("rv", [L, 768], BF16), ("rg", [L, 768], F32), ("ymix", [L, D], F32), ("cqT", [D, L], BF16),
            ("coT", [D, L], BF16), ("ckT", [D, NMEM], BF16), ("cv", [NMEM, D], BF16), ("hmoe", [L, D], BF16),
            ("affT", [NE, L], F32), ("affc", [L, NE], F32), ("z0d", [L, 512], BF16),
        ]:
            T[name] = nc.dram_tensor(name, shp, dty, kind=sk)
        self.T = T
        self.xres_buf = Buf(None, "xres")
        self.hmoe_buf = Buf(None, "hmoe")
        self.afft_buf = Buf(None, "affT")

    def build(self):
        c = self.c
        with ExitStack() as gst:
            self.ident = c.sb(gst, "ident", [128, 128], BF16)
            self.identf = c.sb(gst, "identf", [128, 128], F32)
            self.ones = c.sb(gst, "ones", [128, 128], BF16)
            c.dma("sp", self.ident[:], self.T["ident"].ap(), w=[self.ident], sbuf=self.ident)
            c.dma("sp", self.identf[:], self.T["identf"].ap(), w=[self.identf], sbuf=self.identf)
            c.dma("sp", self.ones[:], self.T["ones"].ap(), w=[self.ones], sbuf=self.ones)
            c.stage_bufs = []
            stages = []
            for l in range(self.nlayers):
                xsrc = self.T["x"] if l == 0 else self.T["xres"]
                stages += [
                    ("inproj%d" % l, lambda l=l, xsrc=xsrc: self.stage_inproj(l, xsrc)),
                    ("na%d" % l, lambda l=l: self.stage_na(l)),
                    ("hy%d" % l, lambda l=l: self.stage_hyena(l)),
                    ("ret%d" % l, lambda l=l: self.stage_ret(l)),
                    ("outproj%d" % l, lambda l=l, xsrc=xsrc: self.stage_outproj(l, xsrc)),
                    ("ckv%d" % l, lambda l=l: self.stage_ckv(l)),
                    ("cq%d" % l, lambda l=l: self.stage_cq(l)),
                    ("cattn%d" % l, lambda l=l: self.stage_cattn(l)),
                    ("co%d" % l, lambda l=l: self.stage_co(l)),
                    ("moeh%d" % l, lambda l=l: self.stage_moe_h(l)),
                    ("moex%d" % l, lambda l=l: self.stage_moe_x(l)),
                ]
            stages.append(("final", self.stage_final))
            for name, fn in stages:
                fn()
                c.end_stage([self.xres_buf, self.hmoe_buf, self.afft_buf])
                if self.stop_after == name:
                    break
            c.barrier()
        return self.nc

    def load_bc(self, st, name, t, off, n, dtype=F32):
        b = self.c.sb(st, name, [128, n], dtype)
        self.c.dma("sp", b[:], row_bc(t, off, n), w=[b], sbuf=b)
        return b

    def rms_to_T(self, xt, gain, hb, xT, col0, ptrs, small, eps=RMS_EPS, width=D, no_T=False):
        c = self.c
        ss, rstd = small
        c.op("act", lambda e: e.activation(out=hb[:], in_=xt[:], func=AF.Square, accum_out=ss[:, 0:1]), r=[xt], w=[hb, ss])
        c.op("act", lambda e: e.activation(out=ss[:, 1:2], in_=ss[:, 0:1], func=AF.Sqrt, scale=1.0 / width, bias=self.epsb[:, 0:1]), r=[ss, self.epsb], w=[ss])
        c.op("dve", lambda e: e.reciprocal(out=rstd[:, 0:1], in_=ss[:, 1:2]), r=[ss], w=[rstd])
        c.op("dve", lambda e: e.scalar_tensor_tensor(out=hb[:], in0=xt[:], scalar=rstd[:, 0:1], in1=gain[:], op0=ALU.mult, op1=ALU.mult), r=[xt, rstd, gain], w=[hb])
        if not no_T:
            self.transpose_into(hb, xT, col0, ptrs)

    def transpose_into(self, hb, xT, col0, ptrs, nk=16):
        c = self.c
        for k4 in range(nk // 4):
            pt = ptrs[k4 % len(ptrs)]
            for q in range(4):
                k = k4 * 4 + q
                c.op("pe", lambda e: e.transpose(pt[:, q, :], hb[:, k * 128:(k + 1) * 128], self.ident[:]),
                     r=[hb, self.ident], w=[pt], signal=(q == 3))
            eng = c.evac_eng()
            copy_on(c, eng, xT[:, k4 * 4:(k4 + 1) * 4, col0:col0 + 128], pt[:, :, :], r=[pt], w=[xT])

    def gemm(self, xT, KC, Tn, w_ap, N, bw, mode, evac, wbufs, pbanks, after_w=None):
        c = self.c
        for nb in range(N // bw):
            wb = wbufs[self.wctr % len(wbufs)]
            self.wctr += 1
            c.dma("pool", wb[:, 0:KC, 0:bw], w_ap[:, nb * bw:(nb + 1) * bw].rearrange("(k p) n -> p k n", p=128), w=[wb], sbuf=wb)
            if after_w is not None:
                after_w(nb)
            if mode == "TM":
                for tt in range(Tn // 128):
                    pb = pbanks[self.pctr % len(pbanks)]
                    self.pctr += 1
                    for k in range(KC):
                        c.op("pe", lambda e: e.matmul(pb[:, 0:bw], xT[:, k, tt * 128:(tt + 1) * 128], wb[:, k, 0:bw], start=(k == 0), stop=(k == KC - 1)),
                             r=[xT, wb], w=[pb], signal=(k == KC - 1))
                    evac(nb, tt, pb)
            else:
                tbs = min(512, Tn)
                for sub in range(bw // 128):
                    for tb in range(Tn // tbs):
                        pb = pbanks[self.pctr % len(pbanks)]
                        self.pctr += 1
                        for k in range(KC):
                            c.op("pe", lambda e: e.matmul(pb[:, 0:tbs], wb[:, k, sub * 128:(sub + 1) * 128], xT[:, k, tb * tbs:(tb + 1) * tbs], start=(k == 0), stop=(k == KC - 1)),
                                 r=[xT, wb], w=[pb], signal=(k == KC - 1))
                        evac(nb * (bw // 128) + sub, tb, pb)


    def run_halves(self, nhalf, A, B, gemm_half):
        for tt in range(8):
            A(0, tt)
            B(0, tt)
        for half in range(nhalf):
            sched = []
            if half + 1 < nhalf:
                hn = half + 1
                sched = [[("A", 0), ("A", 1)], [("B", 0), ("B", 1), ("A", 2), ("A", 3)], [("B", 2), ("B", 3), ("A", 4), ("A", 5)],
                         [("B", 4), ("B", 5), ("A", 6), ("A", 7)], [("B", 6), ("B", 7)]]
            state = [0]

            def hook(_nb, half=half):
                if state[0] < len(sched):
                    for kind, tt in sched[state[0]]:
                        (A if kind == "A" else B)(half + 1, tt)
                    state[0] += 1
            gemm_half(half, hook)
            while state[0] < len(sched):
                hook(0)


    def conv_thunks(self, st, l):
        c, T = self.c, self.T
        cw = [self.load_bc(st, "cw%d" % k, T["hy_conv_w"], (l * 3 + k) * 1536, 1536) for k in range(3)]
        cb = self.load_bc(st, "cb", T["hy_conv_b"], l * 1536, 1536)
        pm = c.sb(st, "pm", [128, 1536], F32)
        p0 = c.sb(st, "p0", [128, 1536], F32)
        pp = c.sb(st, "pp", [128, 1536], F32)
        zt = c.sb(st, "zt", [128, 512], BF16)
        hyp = T["hyp"].ap()
        tiles = []
        for tt in range(16):
            a, b, d = pm, p0, pp
            r0 = tt * 128
            th = []
            if tt == 0:
                th.append(lambda a=a: c.op("dve", lambda e: e.memset(a[:], 0.0), w=[a]))
                th.append(lambda a=a: c.dma("sp", a[1:128, :], hyp[0:127, :], w=[a], sbuf=a))
            else:
                th.append(lambda a=a, r0=r0: c.dma("sp", a[:], hyp[r0 - 1:r0 + 127, :], w=[a], sbuf=a))
            th.append(lambda b=b, r0=r0: c.dma("sp", b[:], hyp[r0:r0 + 128, :], w=[b], sbuf=b))
            if tt == 15:
                th.append(lambda d=d: c.op("dve", lambda e: e.memset(d[:], 0.0), w=[d]))
                th.append(lambda d=d, r0=r0: c.dma("sp", d[0:127, :], hyp[r0 + 1:r0 + 128, :], w=[d], sbuf=d))
            else:
                th.append(lambda d=d, r0=r0: c.dma("sp", d[:], hyp[r0 + 1:r0 + 129, :], w=[d], sbuf=d))
            th.append(lambda a=a: c.op("pool", lambda e: e.tensor_tensor(out=a[:], in0=a[:], in1=cw[0][:], op=ALU.mult), r=[a, cw[0]], w=[a]))
            th.append(lambda b=b: c.op("dve", lambda e: e.tensor_tensor(out=b[:], in0=b[:], in1=cw[1][:], op=ALU.mult), r=[b, cw[1]], w=[b]))
            th.append(lambda d=d: c.op("pool", lambda e: e.tensor_tensor(out=d[:], in0=d[:], in1=cw[2][:], op=ALU.mult), r=[d, cw[2]], w=[d]))
            th.append(lambda b=b: c.op("dve", lambda e: e.tensor_tensor(out=b[:], in0=b[:], in1=cb[:], op=ALU.add), r=[b, cb], w=[b]))
            th.append(lambda a=a, d=d: c.op("dve", lambda e: e.tensor_tensor(out=a[:], in0=a[:], in1=d[:], op=ALU.add), r=[a, d], w=[a]))
            th.append(lambda a=a, b=b: c.op("dve", lambda e: e.tensor_tensor(out=b[:, 0:1024], in0=b[:, 0:1024], in1=a[:, 0:1024], op=ALU.add), r=[a, b], w=[b]))
            th.append(lambda a=a, b=b: c.op("dve", lambda e: e.tensor_tensor(out=zt[:], in0=b[:, 1024:1536], in1=a[:, 1024:1536], op=ALU.add), r=[a, b], w=[zt]))
            th.append(lambda b=b, r0=r0: c.dma("pool", T["hyx"].ap()[r0:r0 + 128, :], b[:, 0:1024], r=[b], sbuf=b))
            th.append(lambda r0=r0: c.dma("pool", T["z0d"].ap()[r0:r0 + 128, :], zt[:], r=[zt], sbuf=zt))
            tiles.append(th)
        return tiles

    def common_alloc(self, st, nw=3, npb=4, wk=16):
        c = self.c
        self.wctr = 0
        self.pctr = 0
        wbufs = [c.sb(st, "wb%d" % i, [128, wk, 512], BF16) for i in range(nw)]
        pbanks = [c.ps(st, "pb%d" % i, [128, 512], F32) for i in range(npb)]
        self.epsb = c.sb(st, "epsb", [128, 1], F32)
        c.op("dve", lambda e: e.memset(self.epsb[:], RMS_EPS), w=[self.epsb])
        return wbufs, pbanks

    def stage_inproj(self, l, xsrc):
        c, T = self.c, self.T
        with ExitStack() as st:
            wbufs, pbanks = self.common_alloc(st)
            gain = self.load_bc(st, "gain", T["norm_mix"], l * D, D)
            xTs = [c.sb(st, "xT%d" % i, [128, 16, 1024], BF16) for i in range(2)]
            xts = [c.sb(st, "xt%d" % i, [128, D], F32) for i in range(2)]
            hbs = [c.sb(st, "hb%d" % i, [128, D], BF16) for i in range(2)]
            ptrs = [c.ps(st, "ptr%d" % i, [128, 4, 128], BF16) for i in range(2)]
            smalls = [(c.sb(st, "ss%d" % i, [128, 2], F32), c.sb(st, "rs%d" % i, [128, 1], F32)) for i in range(2)]
            stg_bf = [c.sb(st, "sgb%d" % i, [128, 512], BF16) for i in range(4)]
            stg_f = [c.sb(st, "sgf%d" % i, [128, 512], F32) for i in range(3)]
            cnt = [0, 0]
            w_in = T["w_in"].ap()[l]

            def A(half, tt):
                xt = xts[tt % 2]
                c.dma("sp", xt[:], xsrc.ap()[half * 1024 + tt * 128:half * 1024 + (tt + 1) * 128, :], w=[xt], sbuf=xt)
                self.rms_to_T(xt, gain, hbs[tt % 2], None, 0, ptrs, smalls[tt % 2], no_T=True)

            def B(half, tt):
                self.transpose_into(hbs[tt % 2], xTs[half % 2], tt * 128, ptrs)

            def gemm_half(half, hook):
                t0 = half * 1024
                xT = xTs[half % 2]

                def mk_evac(dst, col_off, fm, dtype, scale, bw):
                    def evac(i0, i1, pb):
                        if dtype == BF16:
                            sg = stg_bf[cnt[0] % len(stg_bf)]
                            cnt[0] += 1
                        else:
                            sg = stg_f[cnt[1] % len(stg_f)]
                            cnt[1] += 1
                        eng = c.evac_eng()
                        if fm:
                            copy_on(c, eng, sg[:, 0:512], pb[:, 0:512], r=[pb], w=[sg], scale=scale)
                            c.dma("sp", dst.ap()[i0 * 128:(i0 + 1) * 128, t0 + i1 * 512:t0 + (i1 + 1) * 512], sg[:, 0:512], r=[sg], sbuf=sg)
                        else:
                            copy_on(c, eng, sg[:, 0:bw], pb[:, 0:bw], r=[pb], w=[sg], scale=scale)
                            c.dma("sp", dst.ap()[t0 + i1 * 128:t0 + (i1 + 1) * 128, col_off + i0 * bw:col_off + (i0 + 1) * bw], sg[:, 0:bw], r=[sg], sbuf=sg)
                    return evac
                groups = [
                    (0, 768, "FM", T["naqT"], BF16, 0.125, 384),
                    (768, 768, "FM", T["nakT"], BF16, None, 384),
                    (1536, 768, "TM", T["nav"], BF16, None, 384),
                    (2304, 1536, "TM", T["hyp"], F32, None, 512),
                    (3840, 768, "FM", T["rqT"], BF16, 128 ** -0.5, 384),
                    (4608, 768, "FM", T["rkT"], BF16, None, 384),
                    (5376, 768, "TM", T["rv"], BF16, None, 384),
                    (6144, 768, "TM", T["rg"], F32, None, 384),
                ]
                for (c0, n, mode, dst, dty, scale, bw) in groups:
                    self.gemm(xT, 16, 1024, w_in[:, c0:c0 + n], n, bw, mode, mk_evac(dst, 0, mode == "FM", dty, scale, bw), wbufs, pbanks, after_w=hook)
            self.run_halves(2, A, B, gemm_half)

    def stage_na(self, l):
        c, T = self.c, self.T
        DELTAS = [-6, -4, -2, 0, 2, 4, 6]
        with ExitStack() as st:
            qT = c.sb(st, "qT", [128, 6, L], BF16)
            kT = c.sb(st, "kT", [128, 6, L], BF16)
            vx = c.sb(st, "vx", [128, 16, 12, 65], BF16)
            COMBOS = sorted(set((dl, pat) for i in range(16) for (j, dl, pat) in self.na_pairs[i]))
            bt = c.sb(st, "bt", [128, NA_H * len(COMBOS), 128], BF16)
            bp = [c.sb(st, "bp%d" % i, [128, 7, 2, 64], BF16) for i in range(3)]
            mt = c.sb(st, "mt", [128, self.npat, 128], BF16)
            jm = c.sb(st, "jm", [128, 128], BF16)
            psc = [c.ps(st, "psc%d" % i, [128, 1024], F32) for i in range(2)]
            pso = [c.ps(st, "pso%d" % i, [128, 2, 512], F32) for i in range(2)]
            pT = [c.sb(st, "pT%d" % i, [128, 640], BF16) for i in range(3)]
            rden = [c.sb(st, "rden%d" % i, [128, 12], F32) for i in range(2)]
            yt = [c.sb(st, "yt%d" % i, [128, 768], F32) for i in range(2)]
            c.dma("sp", qT[:], T["naqT"].ap().rearrange("(k p) t -> p k t", p=128), w=[qT], sbuf=qT)
            c.dma("sp", kT[:], T["nakT"].ap().rearrange("(k p) t -> p k t", p=128), w=[kT], sbuf=kT)
            c.op("pool", lambda e: e.memset(vx[:], 1.0), w=[vx])
            for j in range(16):
                c.dma("sp", vx[:, j, :, 0:64], T["nav"].ap()[j * 128:(j + 1) * 128, :].rearrange("p (h d) -> p h d", d=64), w=[vx], sbuf=vx)
            c.dma("sp", mt[:], T["namask"].ap(), w=[mt], sbuf=mt)
            c.dma("sp", jm[:], T["naJ"].ap(), w=[jm], sbuf=jm)
            n = 0
            for h in range(NA_H):
                b = bp[h % 3]
                for a in range(2):
                    off = ((l * NA_H + h) * 15 + (1 - a)) * 128
                    src = bass.AP(T["rpbY"], off, [[1, 64], [256, 7], [128, 2], [1, 64]])
                    c.dma("pool", b[a * 64:(a + 1) * 64, :, :, :], src, w=[b], sbuf=b)
                for ci, (dl, pat) in enumerate(COMBOS):
                    slot = h * len(COMBOS) + ci
                    dd = (6 - dl) // 2
                    n += 1
                    pbb = psc[n % 2]
                    c.op("pe", lambda e: e.matmul(pbb[:, 0:128], jm[:], b[:, dd, :, :].rearrange("p b q -> p (b q)"), start=True, stop=False), r=[jm, b], w=[pbb], signal=False)
                    c.op("pe", lambda e: e.matmul(pbb[:, 0:128], self.ident[:], mt[:, pat, :], start=False, stop=True), r=[self.ident, mt], w=[pbb])
                    copy_on(c, c.evac_eng(), bt[:, slot, :], pbb[:, 0:128], r=[pbb], w=[bt])
            n = 0
            pend = []

            def epilogue(i, po):
                rd = rden[i % 2]
                y = yt[i % 2]
                for g in range(2):
                    c.op("dve", lambda e: e.reciprocal(out=rd[:, g * 6:(g + 1) * 6], in_=po[:, g, 0:390].rearrange("p (h d) -> p h d", d=65)[:, :, 64]), r=[po], w=[rd])
                for h in range(NA_H):
                    src = po[:, h // 6, (h % 6) * 65:(h % 6) * 65 + 64]
                    if h % 2 == 0:
                        c.op("dve", lambda e: e.tensor_scalar(out=y[:, h * 64:(h + 1) * 64], in0=src, scalar1=rd[:, h:h + 1], scalar2=None, op0=ALU.mult), r=[po, rd], w=[y])
                    else:
                        c.op("act", lambda e: e.activation(out=y[:, h * 64:(h + 1) * 64], in_=src, func=AF.Copy, scale=rd[:, h:h + 1]), r=[po, rd], w=[y])
                c.dma("sp", T["ymix"].ap()[i * 128:(i + 1) * 128, 0:768], y[:], r=[y], sbuf=y)

            conv = self.conv_thunks(st, l)
            for i in range(16):
                pairs = self.na_pairs[i]
                nk = len(pairs)
                po = pso[i % 2]
                cth = conv[i]
                for h in range(NA_H):
                    for _ in range(2):
                        if cth:
                            cth.pop(0)()
                    hp, off = h // 2, (h % 2) * 64
                    ps = psc[n % 2]
                    pt_ = pT[n % 3]
                    n += 1
                    for jj, (j, dl, pat) in enumerate(pairs):
                        reg = ps[:, jj * 128:(jj + 1) * 128]
                        slot = h * len(COMBOS) + COMBOS.index((dl, pat))
                        c.op("pe", lambda e: e.matmul(reg, kT[off:off + 64, hp, j * 128:(j + 1) * 128], qT[off:off + 64, hp, i * 128:(i + 1) * 128], start=True, stop=False),
                             r=[kT, qT], w=[ps], signal=False)
                        c.op("pe", lambda e: e.matmul(reg, self.ident[:], bt[:, slot, :], start=False, stop=True), r=[self.ident, bt], w=[ps], signal=(jj == nk - 1))
                    c.op("act", lambda e: e.activation(out=pt_[:, 0:nk * 128], in_=ps[:, 0:nk * 128], func=AF.Exp), r=[ps], w=[pt_])

                    def pv(i=i, h=h, pairs=pairs, nk=nk, pt_=pt_, po=po):
                        oreg = po[:, h // 6, (h % 6) * 65:(h % 6) * 65 + 65]
                        for jj, (j, dl, pat) in enumerate(pairs):
                            c.op("pe", lambda e: e.matmul(oreg, pt_[:, jj * 128:(jj + 1) * 128], vx[:, j, h, :], start=(jj == 0), stop=(jj == nk - 1)),
                                 r=[pt_, vx], w=[po], signal=(jj == nk - 1))
                        if h == NA_H - 1:
                            epilogue(i, po)
                    if pend:
                        pend.pop(0)()
                    pend.append(pv)
                while cth:
                    cth.pop(0)()
            while pend:
                pend.pop(0)()

    def stage_hyena(self, l):
        c, T = self.c, self.T
        with ExitStack() as st:
            z = [c.sb(st, "z%d" % i, [128, 16, 512], BF16) for i in range(2)]
            h2b = c.sb(st, "h2b", [64, L], BF16)
            w3b = c.sb(st, "w3b", [64, 2048], BF16)
            dsk = [self.load_bc(st, "dsk%d" % o, T["hy_skip_d"], (l * 2 + o) * 512, 512) for o in range(2)]
            c.dma("pool", w3b[:], T["hy_filt_w3"].ap()[l], w=[w3b], sbuf=w3b)
            c.dma("sp", z[0][:], T["z0d"].ap().rearrange("(j p) d -> p j d", p=128), w=[z[0]], sbuf=z[0])
            with ExitStack() as s1:
                fT = c.sb(s1, "fT", [17, L], F32)
                w1 = c.sb(s1, "w1", [17, 64], F32)
                w2 = c.sb(s1, "w2", [64, 64], F32)
                cols = c.sb(s1, "cols", [64, 6], F32)
                h1 = c.sb(s1, "h1", [64, L], F32)
                pre = [c.sb(s1, "pre%d" % i, [64, 512], F32) for i in range(2)]
                tmp = [c.sb(s1, "tmpm%d" % i, [64, 512], F32) for i in range(2)]
                pm_ = [c.ps(s1, "pmlp%d" % i, [64, 512], F32) for i in range(2)]
                c.dma("sp", fT[:], T["featsT"].ap(), w=[fT], sbuf=fT)
                c.dma("sp", w1[:], T["hy_filt_w1"].ap()[l], w=[w1], sbuf=w1)
                c.dma("sp", w2[:], T["hy_filt_w2"].ap()[l], w=[w2], sbuf=w2)
                c.dma("sp", cols[:, 0:1], T["hy_sin_freq"].ap()[l].rearrange("(p o) -> p o", o=1), w=[cols], sbuf=cols)
                c.dma("sp", cols[:, 1:2], T["hy_filt_b1"].ap()[l].rearrange("(p o) -> p o", o=1), w=[cols], sbuf=cols)
                c.dma("sp", cols[:, 2:3], T["hy_filt_b2"].ap()[l].rearrange("(p o) -> p o", o=1), w=[cols], sbuf=cols)
                c.op("dve", lambda e: e.tensor_tensor(out=cols[:, 3:4], in0=cols[:, 0:1], in1=cols[:, 1:2], op=ALU.mult), r=[cols], w=[cols])
                c.op("dve", lambda e: e.tensor_tensor(out=cols[:, 4:5], in0=cols[:, 0:1], in1=cols[:, 2:3], op=ALU.mult), r=[cols], w=[cols])

                def sin_layer(wt, kdim, src, dst, fbcol):
                    for tb in range(4):
                        pmm = pm_[tb % 2]
                        x_ = pre[tb % 2]
                        t_ = tmp[tb % 2]
                        c.op("pe", lambda e: e.matmul(pmm[:], wt[0:kdim, :], src[0:kdim, tb * 512:(tb + 1) * 512], start=True, stop=True), r=[wt, src], w=[pmm])
                        c.op("dve", lambda e: e.tensor_scalar(out=x_[:], in0=pmm[:], scalar1=cols[:, 0:1], scalar2=cols[:, fbcol:fbcol + 1], op0=ALU.mult, op1=ALU.add), r=[pmm, cols], w=[x_])
                        c.op("dve", lambda e: e.tensor_scalar(out=t_[:], in0=x_[:], scalar1=PI, scalar2=-2 * PI, op0=ALU.is_gt, op1=ALU.mult), r=[x_], w=[t_])
                        c.op("dve", lambda e: e.tensor_tensor(out=x_[:], in0=x_[:], in1=t_[:], op=ALU.add), r=[x_, t_], w=[x_])
                        c.op("dve", lambda e: e.tensor_scalar(out=t_[:], in0=x_[:], scalar1=-PI, scalar2=2 * PI, op0=ALU.is_lt, op1=ALU.mult), r=[x_], w=[t_])
                        c.op("dve", lambda e: e.tensor_tensor(out=x_[:], in0=x_[:], in1=t_[:], op=ALU.add), r=[x_, t_], w=[x_])
                        c.op("dve", lambda e: e.tensor_scalar(out=x_[:], in0=x_[:], scalar1=3.1415925, scalar2=-3.1415925, op0=ALU.min, op1=ALU.max), r=[x_], w=[x_])
                        c.op("act", lambda e: e.activation(out=dst[:, tb * 512:(tb + 1) * 512], in_=x_[:], func=AF.Sin), r=[x_], w=[dst])
                sin_layer(w1, 17, fT, h1, 3)
                sin_layer(w2, 64, h1, h2b, 4)
            c.barrier()
            with ExitStack() as s2:
                ksum = c.sb(s2, "ksum", [128, 16, 512], BF16)
                kdif = c.sb(s2, "kdif", [128, 16, 512], BF16)
                Pr = c.sb(s2, "Pr", [128, 16, 512], BF16)
                Pi = c.sb(s2, "Pi", [128, 16, 512], BF16)
                tabs = [(c.sb(s2, "tc%d" % i, [128, 16, 128], BF16), c.sb(s2, "ts%d" % i, [128, 16, 128], BF16)) for i in range(2)]
                pk = [c.ps(s2, "pk%d" % i, [128, 512], F32) for i in range(4)]
                pinv = [c.ps(s2, "pinv%d" % i, [128, 512], F32) for i in range(2)]
                pf = c.ps(s2, "pf", [128, 2, 512], F32)
                win = [c.sb(s2, "win%d" % i, [128, 512], F32) for i in range(2)]
                ff = [c.sb(s2, "ff%d" % i, [128, 512], F32) for i in range(2)]
                fb = [c.sb(s2, "fb%d" % i, [128, 512], F32) for i in range(2)]
                ksb = [c.sb(s2, "ksb%d" % i, [128, 2, 512], F32) for i in range(2)]
                ta = [c.sb(s2, "ta%d" % i, [128, 512], F32) for i in range(2)]
                tb_ = [c.sb(s2, "tbb%d" % i, [128, 512], F32) for i in range(2)]
                xg = [c.sb(s2, "xg%d" % i, [128, 512], F32) for i in range(2)]
                og = [c.sb(s2, "og%d" % i, [128, 512], F32) for i in range(2)]
                for o in range(2):
                    zin = z[o]
                    for tt in range(16):
                        w_ = win[tt % 2]
                        f_, b_ = ff[tt % 2], fb[tt % 2]
                        c.dma("sp", w_[:], T["window"].ap()[tt * 128:(tt + 1) * 128, :], w=[w_], sbuf=w_)
                        for dr in range(2):
                            c.op("pe", lambda e: e.matmul(pf[:, dr, :], h2b[:, tt * 128:(tt + 1) * 128], w3b[:, (o * 2 + dr) * 512:(o * 2 + dr + 1) * 512], start=True, stop=True),
                                 r=[h2b, w3b], w=[pf], signal=(dr == 1))
                        c.op("dve", lambda e: e.tensor_tensor(out=f_[:], in0=pf[:, 0, :], in1=w_[:], op=ALU.mult), r=[pf, w_], w=[f_])
                        c.op("dve", lambda e: e.tensor_tensor(out=b_[:], in0=pf[:, 1, :], in1=w_[:], op=ALU.mult), r=[pf, w_], w=[b_])
                        if tt == 0:
                            c.op("dve", lambda e: e.memset(b_[0:1, :], 0.0), w=[b_])
                        c.op("pool", lambda e: e.tensor_tensor(out=ksum[:, tt, :], in0=f_[:], in1=b_[:], op=ALU.add), r=[f_, b_], w=[ksum])
                        c.op("dve", lambda e: e.tensor_tensor(out=kdif[:, tt, :], in0=b_[:], in1=f_[:], op=ALU.subtract), r=[f_, b_], w=[kdif])
                    for fc in range(16):
                        tcb, tsb = tabs[fc % 2]
                        c.dma("sp", tcb[:], T["dftC"].ap()[fc], w=[tcb], sbuf=tcb)
                        c.dma("sp", tsb[:], T["dftS"].ap()[fc], w=[tsb], sbuf=tsb)
                        for gi, (tab, rhs) in enumerate([(tcb, ksum), (tsb, kdif), (tcb, zin), (tsb, zin)]):
                            for k in range(16):
                                c.op("pe", lambda e: e.matmul(pk[gi][:], tab[:, k, :], rhs[:, k, :], start=(k == 0), stop=(k == 15)), r=[tab, rhs], w=[pk[gi]], signal=(k == 15))
                        ks = ksb[fc % 2]
                        c.op("act", lambda e: e.copy(out=ks[:, 0, :], in_=pk[0][:]), r=[pk[0]], w=[ks])
                        c.op("act", lambda e: e.copy(out=ks[:, 1, :], in_=pk[1][:]), r=[pk[1]], w=[ks])
                        q0, q1, q2, q3 = ta[0], ta[1], tb_[0], tb_[1]
                        c.op("dve", lambda e: e.tensor_tensor(out=q0[:], in0=pk[2][:], in1=ks[:, 0, :], op=ALU.mult), r=[pk[2], ks], w=[q0])
                        c.op("dve", lambda e: e.tensor_tensor(out=q1[:], in0=pk[2][:], in1=ks[:, 1, :], op=ALU.mult), r=[pk[2], ks], w=[q1])
                        c.op("dve", lambda e: e.tensor_tensor(out=q2[:], in0=pk[3][:], in1=ks[:, 1, :], op=ALU.mult), r=[pk[3], ks], w=[q2])
                        c.op("dve", lambda e: e.tensor_tensor(out=q3[:], in0=pk[3][:], in1=ks[:, 0, :], op=ALU.mult), r=[pk[3], ks], w=[q3])
                        c.op("pool", lambda e: e.tensor_tensor(out=Pr[:, fc, :], in0=q0[:], in1=q2[:], op=ALU.add), r=[q0, q2], w=[Pr])
                        c.op("pool", lambda e: e.tensor_tensor(out=Pi[:, fc, :], in0=q1[:], in1=q3[:], op=ALU.subtract), r=[q1, q3], w=[Pi])
                    for tt in range(16):
                        tcb, tsb = tabs[tt % 2]
                        c.dma("sp", tcb[:], T["dftCT"].ap()[tt], w=[tcb], sbuf=tcb)
                        c.dma("sp", tsb[:], T["dftnST"].ap()[tt], w=[tsb], sbuf=tsb)
                        pv = pinv[tt % 2]
                        for k in range(16):
                            c.op("pe", lambda e: e.matmul(pv[:], tcb[:, k, :], Pr[:, k, :], start=(k == 0), stop=False), r=[tcb, Pr], w=[pv], signal=False)
                        for k in range(16):
                            c.op("pe", lambda e: e.matmul(pv[:], tsb[:, k, :], Pi[:, k, :], start=False, stop=(k == 15)), r=[tsb, Pi], w=[pv], signal=(k == 15))
                        x_ = xg[tt % 2]
                        a_ = ta[tt % 2]
                        c.dma("sp", x_[:], T["hyx"].ap()[tt * 128:(tt + 1) * 128, o * 512:(o + 1) * 512], w=[x_], sbuf=x_)
                        c.op("pool", lambda e: e.tensor_tensor(out=a_[:], in0=zin[:, tt, :], in1=dsk[o][:], op=ALU.mult), r=[zin, dsk[o]], w=[a_])
                        c.op("dve", lambda e: e.scalar_tensor_tensor(out=a_[:], in0=pv[:], scalar=1.0 / L, in1=a_[:], op0=ALU.mult, op1=ALU.add), r=[pv, a_], w=[a_])
                        if o == 0:
                            c.op("dve", lambda e: e.tensor_tensor(out=z[1][:, tt, :], in0=a_[:], in1=x_[:], op=ALU.mult), r=[a_, x_], w=[z[1]])
                        else:
                            o_ = og[tt % 2]
                            c.op("dve", lambda e: e.tensor_tensor(out=o_[:], in0=a_[:], in1=x_[:], op=ALU.mult), r=[a_, x_], w=[o_])
                            c.dma("act", T["ymix"].ap()[tt * 128:(tt + 1) * 128, 768:1280], o_[:], r=[o_], sbuf=o_)

    def stage_ret(self, l):
        c, T = self.c, self.T
        lgf, lgb = self.consts["_lgf"], self.consts["_lgb"]
        with ExitStack() as st:
            rc = c.sb(st, "rc", [128, L], F32)
            rs_ = c.sb(st, "rs", [128, L], F32)
            rR = c.sb(st, "rR", [128, 128], BF16)
            E = c.sb(st, "E", [128, 512], F32)
            gne = c.sb(st, "gne", [128, 1], F32)
            c.op("dve", lambda e: e.memset(gne[:], GN_EPS), w=[gne])
            c.dma("sp", rc[:], T["rotcos"].ap(), w=[rc], sbuf=rc)
            c.dma("sp", rs_[:], T["rotsin"].ap(), w=[rs_], sbuf=rs_)
            c.dma("sp", rR[:], T["rotR"].ap(), w=[rR], sbuf=rR)
            c.dma("sp", E[:], T["retE"].ap(), w=[E], sbuf=E)
            raw = [c.sb(st, "raw%d" % i, [128, L], BF16) for i in range(2)]
            qk = [[c.sb(st, "qk%d_%d" % (i, j), [128, L], BF16) for j in range(2)] for i in range(2)]
            vh = [c.sb(st, "vh%d" % i, [128, 16, 128], BF16) for i in range(2)]
            dg = [c.sb(st, "dg%d" % i, [128, 4, 512], BF16) for i in range(2)]
            prot = [c.ps(st, "prot%d" % i, [128, 512], F32) for i in range(2)]
            pss = [c.ps(st, "pss%d" % i, [128, 512], F32) for i in range(2)]
            psy = [c.ps(st, "psy%d" % i, [128, 512], F32) for i in range(2)]
            ptt = [c.ps(st, "ptt%d" % i, [128, 4, 128], F32) for i in range(2)]
            t1 = [c.sb(st, "t1_%d" % i, [128, 512], F32) for i in range(2)]
            t2 = [c.sb(st, "t2_%d" % i, [128, 512], F32) for i in range(2)]
            dec = [c.sb(st, "dec%d" % i, [128, 512], BF16) for i in range(3)]
            pT = [c.sb(st, "pT%d" % i, [128, 512], BF16) for i in range(3)]
            yT = [c.sb(st, "yT%d" % i, [128, 512], F32) for i in range(2)]
            gt = [c.sb(st, "gt%d" % i, [128, 4, 128], F32) for i in range(2)]
            sg = [c.sb(st, "sg%d" % i, [128, 4, 128], F32) for i in range(2)]
            yo = [c.sb(st, "yo%d" % i, [128, 4, 128], F32) for i in range(2)]
            stt = [c.sb(st, "stt%d" % i, [128, 4, 6], F32) for i in range(2)]
            mv = [c.sb(st, "mv%d" % i, [128, 4, 4], F32) for i in range(2)]
            n = 0
            pend = []
            ypend = []
            for h in range(6):
                hb = h % 2
                for wi, src in enumerate([T["rqT"], T["rkT"]]):
                    rw = raw[wi]
                    dstb = qk[hb][wi]
                    c.dma("sp", rw[:], src.ap()[h * 128:(h + 1) * 128, :], w=[rw], sbuf=rw)
                    for tb in range(4):
                        sl = slice(tb * 512, (tb + 1) * 512)
                        pr = prot[tb % 2]
                        a_, b_ = t1[tb % 2], t2[tb % 2]
                        c.op("pe", lambda e: e.matmul(pr[:], rR[:], rw[:, sl], start=True, stop=True), r=[rR, rw], w=[pr])
                        c.op("dve", lambda e: e.tensor_tensor(out=a_[:], in0=pr[:], in1=rs_[:, sl], op=ALU.mult), r=[pr, rs_], w=[a_])
                        c.op("pool", lambda e: e.tensor_tensor(out=b_[:], in0=rw[:, sl], in1=rc[:, sl], op=ALU.mult), r=[rw, rc], w=[b_])
                        c.op("dve", lambda e: e.tensor_tensor(out=dstb[:, sl], in0=a_[:], in1=b_[:], op=ALU.add), r=[a_, b_], w=[dstb])
                qr, kr = qk[hb]
                v_ = vh[hb]
                d_ = dg[hb]
                c.dma("sp", v_[:], T["rv"].ap()[:, h * 128:(h + 1) * 128].rearrange("(j p) d -> p j d", p=128), w=[v_], sbuf=v_)
                c.dma("sp", d_[:], T["retD"].ap()[h], w=[d_], sbuf=d_)
                decF, decB = dec[0], dec[1]
                c.op("act", lambda e: e.activation(out=decF[:], in_=E[:], func=AF.Exp, scale=float(lgf[h])), r=[E], w=[decF])
                c.op("act", lambda e: e.activation(out=decB[:], in_=E[:], func=AF.Exp, scale=float(-lgb[h])), r=[E], w=[decB])
                for ib in range(4):
                    py = psy[ib % 2]
                    for j in range(16):
                        ps = pss[n % 2]
                        p_ = pT[n % 3]
                        n += 1
                        c.op("pe", lambda e: e.matmul(ps[:], kr[:, j * 128:(j + 1) * 128], qr[:, ib * 512:(ib + 1) * 512], start=True, stop=True), r=[kr, qr], w=[ps])
                        offv = ib * 512 - j * 128
                        if 0 <= j - ib * 4 < 4:
                            decap = d_[:, j - ib * 4, :]
                            c.op("dve", lambda e: e.tensor_tensor(out=p_[:], in0=ps[:], in1=decap, op=ALU.mult), r=[ps, d_], w=[p_])
                        else:
                            if offv > 0:
                                de, fac = decF, math.exp(float(lgf[h]) * offv)
                            else:
                                de, fac = decB, math.exp(float(-lgb[h]) * offv)
                            c.op("dve", lambda e: e.scalar_tensor_tensor(out=p_[:], in0=ps[:], scalar=float(fac), in1=de[:], op0=ALU.mult, op1=ALU.mult), r=[ps, de], w=[p_])
                        def ymm(py=py, v_=v_, j=j, p_=p_):
                            c.op("pe", lambda e: e.matmul(py[:], v_[:, j, :], p_[:], start=(j == 0), stop=(j == 15)), r=[v_, p_], w=[py], signal=(j == 15))
                        if ypend:
                            ypend.pop(0)()
                        ypend.append(ymm)
                    while ypend:
                        ypend.pop(0)()
                    y_ = yT[ib % 2]
                    c.op("act", lambda e: e.copy(out=y_[:], in_=py[:]), r=[py], w=[y_])

                    def epilogue(h=h, ib=ib, y_=y_):
                        k_ = (ib + 4 * h) % 2
                        pt4 = ptt[k_]
                        for q in range(4):
                            c.op("pe", lambda e: e.transpose(pt4[:, q, :], y_[:, q * 128:(q + 1) * 128], self.identf[:]), r=[y_, self.identf], w=[pt4], signal=(q == 3))
                        g_, s_, o_, st_, m_ = gt[k_], sg[k_], yo[k_], stt[k_], mv[k_]
                        rows = slice(ib * 512, (ib + 1) * 512)
                        c.dma("sp", g_[:], T["rg"].ap()[rows, h * 128:(h + 1) * 128].rearrange("(q p) d -> p q d", p=128), w=[g_], sbuf=g_)
                        c.op("act", lambda e: e.activation(out=s_[:], in_=g_[:], func=AF.Silu), r=[g_], w=[s_])
                        for q in range(4):
                            c.op("dve", lambda e: e.bn_stats(out=st_[:, q, :], in_=pt4[:, q, :]), r=[pt4], w=[st_])
                        for q in range(4):
                            c.op("dve", lambda e: e.bn_aggr(out=m_[:, q, 0:2], in_=st_[:, q, :]), r=[st_], w=[m_])
                        c.op("act", lambda e: e.activation(out=m_[:, :, 2], in_=m_[:, :, 1], func=AF.Sqrt, bias=gne[:, 0:1]), r=[m_, gne], w=[m_])
                        c.op("dve", lambda e: e.reciprocal(out=m_[:, :, 3], in_=m_[:, :, 2]), r=[m_], w=[m_])
                        c.op("dve", lambda e: e.tensor_tensor(out=o_[:], in0=pt4[:], in1=m_[:, :, 0:1].to_broadcast([128, 4, 128]), op=ALU.subtract), r=[pt4, m_], w=[o_])
                        c.op("dve", lambda e: e.tensor_tensor(out=o_[:], in0=o_[:], in1=m_[:, :, 3:4].to_broadcast([128, 4, 128]), op=ALU.mult), r=[o_, m_], w=[o_])
                        c.op("pool", lambda e: e.tensor_tensor(out=o_[:], in0=o_[:], in1=s_[:], op=ALU.mult), r=[o_, s_], w=[o_])
                        c.dma("pool", T["ymix"].ap()[rows, 1280 + h * 128:1280 + (h + 1) * 128].rearrange("(q p) d -> p q d", p=128), o_[:], r=[o_], sbuf=o_)
                    if pend:
                        pend.pop(0)()
                    pend.append(epilogue)
            while pend:
                pend.pop(0)()

    def ret_bias(self, st, val):
        c = self.c
        key = (id(st), round(val, 9))
        if not hasattr(self, "_rb"):
            self._rb = {}
        if key not in self._rb:
            b = c.sb(st, "rb%d" % len(self._rb), [128, 1], F32)
            c.op("pool", lambda e: e.memset(b[:], float(val)), w=[b])
            self._rb[key] = b
        return self._rb[key]

    def stage_outproj(self, l, xsrc):
        c, T = self.c, self.T
        with ExitStack() as st:
            wbufs, pbanks = self.common_alloc(st)
            gain = self.load_bc(st, "gain", T["branch_norm"], l * D, D)
            ginvw = c.sb(st, "ginvw", [128, 3], F32)
            c.dma("sp", ginvw[:], T["ginvw"].ap(), w=[ginvw], sbuf=ginvw)
            xTs = [c.sb(st, "xT%d" % i, [128, 16, 1024], BF16) for i in range(2)]
            xts = [c.sb(st, "xt%d" % i, [128, D], F32) for i in range(2)]
            hbs = [c.sb(st, "hb%d" % i, [128, D], BF16) for i in range(2)]
            ptrs = [c.ps(st, "ptr%d" % i, [128, 4, 128], BF16) for i in range(2)]
            sm = [c.sb(st, "sm%d" % i, [128, 12], F32) for i in range(2)]
            xs = [c.sb(st, "xs%d" % i, [128, 512], F32) for i in range(4)]
            so = [c.sb(st, "so%d" % i, [128, 512], F32) for i in range(4)]
            cnt = [0]
            segs = [(0, 768), (768, 1280), (1280, 2048)]

            def A(half, tt):
                t0 = half * 1024
                xt, hb, s_ = xts[tt % 2], hbs[tt % 2], sm[tt % 2]
                c.dma("sp", xt[:], T["ymix"].ap()[t0 + tt * 128:t0 + (tt + 1) * 128, :], w=[xt], sbuf=xt)
                for g, (a, b) in enumerate(segs):
                    c.op("act", lambda e: e.activation(out=hb[:, a:b], in_=xt[:, a:b], func=AF.Square, accum_out=s_[:, g:g + 1]), r=[xt], w=[hb, s_])
                c.op("dve", lambda e: e.tensor_tensor(out=s_[:, 3:6], in0=s_[:, 0:3], in1=ginvw[:], op=ALU.mult), r=[s_, ginvw], w=[s_])
                c.op("act", lambda e: e.activation(out=s_[:, 6:9], in_=s_[:, 3:6], func=AF.Sqrt, bias=self.epsb[:, 0:1]), r=[s_, self.epsb], w=[s_])
                c.op("dve", lambda e: e.reciprocal(out=s_[:, 9:12], in_=s_[:, 6:9]), r=[s_], w=[s_])
                for g, (a, b) in enumerate(segs):
                    c.op("dve", lambda e: e.scalar_tensor_tensor(out=hb[:, a:b], in0=xt[:, a:b], scalar=s_[:, 9 + g:10 + g], in1=gain[:, a:b], op0=ALU.mult, op1=ALU.mult), r=[xt, s_, gain], w=[hb])

            def B(half, tt):
                self.transpose_into(hbs[tt % 2], xTs[half % 2], tt * 128, ptrs)

            def gemm_half(half, hook):
                self.gemm(xTs[half % 2], 16, 1024, T["w_out"].ap()[l], D, 512, "TM", self.mk_resid_evac(xsrc, half * 1024, xs, so, cnt), wbufs, pbanks, after_w=hook)
            self.run_halves(2, A, B, gemm_half)

    def mk_resid_evac(self, xsrc, t0, xs, so, cnt):
        c, T = self.c, self.T

        def evac(nb, tt, pb):
            x_ = xs[cnt[0] % len(xs)]
            o_ = so[cnt[0] % len(so)]
            cnt[0] += 1
            rows = slice(t0 + tt * 128, t0 + (tt + 1) * 128)
            cols = slice(nb * 512, (nb + 1) * 512)
            c.dma("act", x_[:], xsrc.ap()[rows, cols], r=[self.xres_buf] if xsrc is T["xres"] else [], w=[x_], sbuf=x_)
            c.op("dve", lambda e: e.tensor_tensor(out=o_[:], in0=pb[:], in1=x_[:], op=ALU.add), r=[pb, x_], w=[o_])
            c.dma("sp", T["xres"].ap()[rows, cols], o_[:], r=[o_], sbuf=o_)
        return evac

    def stage_ckv(self, l):
        c, T = self.c, self.T
        with ExitStack() as st:
            wbufs, pbanks = self.common_alloc(st)
            gain = self.load_bc(st, "gain", T["mem_norm"], 0, D)
            xT = c.sb(st, "xT", [128, 16, 256], BF16)
            xts = [c.sb(st, "xt%d" % i, [128, D], F32) for i in range(2)]
            hbs = [c.sb(st, "hb%d" % i, [128, D], BF16) for i in range(2)]
            ptrs = [c.ps(st, "ptr%d" % i, [128, 4, 128], BF16) for i in range(2)]
            smalls = [(c.sb(st, "ss%d" % i, [128, 2], F32), c.sb(st, "rs%d" % i, [128, 1], F32)) for i in range(2)]
            sg = [c.sb(st, "sg%d" % i, [128, 512], BF16) for i in range(4)]
            cnt = [0]
            for tt in range(2):
                xt = xts[tt]
                c.dma("sp", xt[:], T["mem"].ap()[tt * 128:(tt + 1) * 128, :], w=[xt], sbuf=xt)
                self.rms_to_T(xt, gain, hbs[tt], xT, tt * 128, ptrs, smalls[tt])

            def evac_k(fc, tb, pb):
                s_ = sg[cnt[0] % 4]
                cnt[0] += 1
                copy_on(c, c.evac_eng(), s_[:, 0:256], pb[:, 0:256], r=[pb], w=[s_])
                c.dma("sp", T["ckT"].ap()[fc * 128:(fc + 1) * 128, :], s_[:, 0:256], r=[s_], sbuf=s_)

            def evac_v(nb, tt, pb):
                s_ = sg[cnt[0] % 4]
                cnt[0] += 1
                copy_on(c, c.evac_eng(), s_[:], pb[:], r=[pb], w=[s_])
                c.dma("sp", T["cv"].ap()[tt * 128:(tt + 1) * 128, nb * 512:(nb + 1) * 512], s_[:], r=[s_], sbuf=s_)
            wk = T["w_ckv"].ap()[l]
            self.gemm(xT, 16, 256, wk[:, 0:D], D, 512, "FM", evac_k, wbufs, pbanks)
            self.gemm(xT, 16, 256, wk[:, D:2 * D], D, 512, "TM", evac_v, wbufs, pbanks)

    def stage_cq(self, l):
        c, T = self.c, self.T
        with ExitStack() as st:
            wbufs, pbanks = self.common_alloc(st)
            gain = self.load_bc(st, "gain", T["norm_cross"], l * D, D)
            xTs = [c.sb(st, "xT%d" % i, [128, 16, 1024], BF16) for i in range(2)]
            xts = [c.sb(st, "xt%d" % i, [128, D], F32) for i in range(2)]
            hbs = [c.sb(st, "hb%d" % i, [128, D], BF16) for i in range(2)]
            ptrs = [c.ps(st, "ptr%d" % i, [128, 4, 128], BF16) for i in range(2)]
            smalls = [(c.sb(st, "ss%d" % i, [128, 2], F32), c.sb(st, "rs%d" % i, [128, 1], F32)) for i in range(2)]
            sg = [c.sb(st, "sg%d" % i, [128, 512], BF16) for i in range(4)]
            cnt = [0]

            def A(half, tt):
                xt = xts[tt % 2]
                c.dma("sp", xt[:], T["xres"].ap()[half * 1024 + tt * 128:half * 1024 + (tt + 1) * 128, :], w=[xt], sbuf=xt)
                self.rms_to_T(xt, gain, hbs[tt % 2], None, 0, ptrs, smalls[tt % 2], no_T=True)

            def B(half, tt):
                self.transpose_into(hbs[tt % 2], xTs[half % 2], tt * 128, ptrs)

            def gemm_half(half, hook):
                t0 = half * 1024

                def evac(fc, tb, pb):
                    s_ = sg[cnt[0] % 4]
                    cnt[0] += 1
                    copy_on(c, c.evac_eng(), s_[:], pb[:], r=[pb], w=[s_], scale=512 ** -0.5)
                    c.dma("sp", T["cqT"].ap()[fc * 128:(fc + 1) * 128, t0 + tb * 512:t0 + (tb + 1) * 512], s_[:], r=[s_], sbuf=s_)
                self.gemm(xTs[half % 2], 16, 1024, T["w_cq"].ap()[l], D, 512, "FM", evac, wbufs, pbanks, after_w=hook)
            self.run_halves(2, A, B, gemm_half)

    def stage_cattn(self, l):
        c, T = self.c, self.T
        with ExitStack() as st:
            kT = c.sb(st, "kT", [128, 16, 256], BF16)
            v = c.sb(st, "v", [128, 2, D], BF16)
            c.dma("sp", kT[:], T["ckT"].ap().rearrange("(k p) m -> p k m", p=128), w=[kT], sbuf=kT)
            c.dma("sp", v[:], T["cv"].ap().rearrange("(j p) d -> p j d", p=128), w=[v], sbuf=v)
            qTs = [c.sb(st, "qT%d" % i, [128, 16, 512], BF16) for i in range(2)]
            oTs = [c.sb(st, "oT%d" % i, [128, 16, 512], BF16) for i in range(2)]
            pT = [c.sb(st, "pT%d" % i, [128, 2, 512], BF16) for i in range(2)]
            rd = [c.sb(st, "rd%d" % i, [128, 512], F32) for i in range(2)]
            pss = [c.ps(st, "pss%d" % i, [128, 512], F32) for i in range(3)]
            psd = c.ps(st, "psd", [128, 512], F32)
            pso = [c.ps(st, "pso%d" % i, [128, 512], F32) for i in range(3)]
            n = 0
            m = 0
            for tb in range(4):
                q_, o_ = qTs[tb % 2], oTs[tb % 2]
                c.dma("sp", q_[:], T["cqT"].ap()[:, tb * 512:(tb + 1) * 512].rearrange("(k p) t -> p k t", p=128), w=[q_], sbuf=q_)
                for hh in range(4):
                    p_ = pT[hh % 2]
                    for mh in range(2):
                        ps = pss[n % 3]
                        n += 1
                        for dc in range(4):
                            c.op("pe", lambda e: e.matmul(ps[:], kT[:, hh * 4 + dc, mh * 128:(mh + 1) * 128], q_[:, hh * 4 + dc, :], start=(dc == 0), stop=(dc == 3)), r=[kT, q_], w=[ps], signal=(dc == 3))
                        c.op("act", lambda e: e.activation(out=p_[:, mh, :], in_=ps[:], func=AF.Exp), r=[ps], w=[p_])
                    for mh in range(2):
                        c.op("pe", lambda e: e.matmul(psd[:], self.ones[:], p_[:, mh, :], start=(mh == 0), stop=(mh == 1)), r=[self.ones, p_], w=[psd], signal=(mh == 1))
                    r_ = rd[hh % 2]
                    c.op("dve", lambda e: e.reciprocal(out=r_[:], in_=psd[:]), r=[psd], w=[r_])
                    for dvc in range(4):
                        po = pso[m % 3]
                        m += 1
                        for mh in range(2):
                            c.op("pe", lambda e: e.matmul(po[:], v[:, mh, hh * 512 + dvc * 128:hh * 512 + (dvc + 1) * 128], p_[:, mh, :], start=(mh == 0), stop=(mh == 1)), r=[v, p_], w=[po], signal=(mh == 1))
                        c.op("dve", lambda e: e.tensor_tensor(out=o_[:, hh * 4 + dvc, :], in0=po[:], in1=r_[:], op=ALU.mult), r=[po, r_], w=[o_])
                c.dma("pool", T["coT"].ap()[:, tb * 512:(tb + 1) * 512].rearrange("(k p) t -> p k t", p=128), o_[:], r=[o_], sbuf=o_)

    def stage_co(self, l):
        c, T = self.c, self.T
        with ExitStack() as st:
            wbufs, pbanks = self.common_alloc(st)
            xT = c.sb(st, "xT", [128, 16, 1024], BF16)
            xs = [c.sb(st, "xs%d" % i, [128, 512], F32) for i in range(4)]
            so = [c.sb(st, "so%d" % i, [128, 512], F32) for i in range(4)]
            cnt = [0]
            for half in range(2):
                t0 = half * 1024
                c.dma("sp", xT[:], T["coT"].ap()[:, t0:t0 + 1024].rearrange("(k p) t -> p k t", p=128), w=[xT], sbuf=xT)
                self.gemm(xT, 16, 1024, T["w_co"].ap()[l], D, 512, "TM", self.mk_resid_evac(T["xres"], t0, xs, so, cnt), wbufs, pbanks)

    def stage_moe_h(self, l):
        c, T = self.c, self.T
        with ExitStack() as st:
            self.common_alloc(st, nw=0, npb=0)
            gain = self.load_bc(st, "gain", T["norm_moe"], l * D, D)
            wr = c.sb(st, "wr", [128, 16, NE], F32)
            wrh = c.sb(st, "wrh", [128, 16, NE], BF16)
            wrl = c.sb(st, "wrl", [128, 16, NE], BF16)
            wtmp = c.sb(st, "wtmp", [128, 16, NE], F32)
            c.dma("sp", wr[:], T["w_router"].ap()[l].rearrange("(k p) e -> p k e", p=128), w=[wr], sbuf=wr)
            c.op("dve", lambda e: e.tensor_copy(out=wrh[:], in_=wr[:]), r=[wr], w=[wrh])
            c.op("dve", lambda e: e.tensor_copy(out=wtmp[:], in_=wrh[:]), r=[wrh], w=[wtmp])
            c.op("dve", lambda e: e.tensor_tensor(out=wrl[:], in0=wr[:], in1=wtmp[:], op=ALU.subtract), r=[wr, wtmp], w=[wrl])
            xts = [c.sb(st, "xt%d" % i, [128, D], F32) for i in range(2)]
            hfs = [c.sb(st, "hf%d" % i, [128, D], F32) for i in range(2)]
            hbs = [c.sb(st, "hb%d" % i, [128, D], BF16) for i in range(2)]
            hls = [c.sb(st, "hl%d" % i, [128, D], BF16) for i in range(2)]
            hT = [c.sb(st, "hT%d" % i, [128, 16, 128], BF16) for i in range(2)]
            lT = [c.sb(st, "lT%d" % i, [128, 16, 128], BF16) for i in range(2)]
            ptrs = [c.ps(st, "ptr%d" % i, [128, 4, 128], BF16) for i in range(2)]
            pl = [c.ps(st, "pl%d" % i, [128, NE], F32) for i in range(2)]
            pa = c.ps(st, "pa", [NE, 128], F32)
            smalls = [(c.sb(st, "ss%d" % i, [128, 2], F32), c.sb(st, "rs%d" % i, [128, 1], F32)) for i in range(2)]
            ex = [c.sb(st, "ex%d" % i, [128, NE], F32) for i in range(2)]
            se = [c.sb(st, "se%d" % i, [128, 2], F32) for i in range(2)]
            af = [c.sb(st, "af%d" % i, [128, NE], F32) for i in range(2)]
            aT = c.sb(st, "aT", [NE, L], F32)
            for tt in range(16):
                xt, hf, hb, hl = xts[tt % 2], hfs[tt % 2], hbs[tt % 2], hls[tt % 2]
                ss, rstd = smalls[tt % 2]
                c.dma("sp", xt[:], T["xres"].ap()[tt * 128:(tt + 1) * 128, :], r=[self.xres_buf], w=[xt], sbuf=xt)
                c.op("act", lambda e: e.activation(out=hb[:], in_=xt[:], func=AF.Square, accum_out=ss[:, 0:1]), r=[xt], w=[hb, ss])
                c.op("act", lambda e: e.activation(out=ss[:, 1:2], in_=ss[:, 0:1], func=AF.Sqrt, scale=1.0 / D, bias=self.epsb[:, 0:1]), r=[ss, self.epsb], w=[ss])
                c.op("dve", lambda e: e.reciprocal(out=rstd[:, 0:1], in_=ss[:, 1:2]), r=[ss], w=[rstd])
                c.op("dve", lambda e: e.scalar_tensor_tensor(out=hf[:], in0=xt[:], scalar=rstd[:, 0:1], in1=gain[:], op0=ALU.mult, op1=ALU.mult), r=[xt, rstd, gain], w=[hf])
                c.op("act", lambda e: e.copy(out=hb[:], in_=hf[:]), r=[hf], w=[hb])
                c.op("pool", lambda e: e.tensor_tensor(out=hl[:], in0=hf[:], in1=hb[:], op=ALU.subtract), r=[hf, hb], w=[hl])
                c.dma("pool", T["hmoe"].ap()[tt * 128:(tt + 1) * 128, :], hb[:], r=[hb], w=[self.hmoe_buf], sbuf=hb)
                h_, l_ = hT[tt % 2], lT[tt % 2]
                self.transpose_into(hb, h_, 0, ptrs)
                self.transpose_into(hl, l_, 0, ptrs)
                p_ = pl[tt % 2]
                combos = [(h_, wrh), (l_, wrh), (h_, wrl)]
                for ci, (a_, w_) in enumerate(combos):
                    for k in range(16):
                        c.op("pe", lambda e: e.matmul(p_[:], a_[:, k, :], w_[:, k, :], start=(ci == 0 and k == 0), stop=(ci == 2 and k == 15)), r=[a_, w_], w=[p_], signal=(ci == 2 and k == 15))
                e_, s_, a2 = ex[tt % 2], se[tt % 2], af[tt % 2]
                c.op("act", lambda e: e.activation(out=e_[:], in_=p_[:], func=AF.Exp, accum_out=s_[:, 0:1]), r=[p_], w=[e_, s_])
                c.op("dve", lambda e: e.reciprocal(out=s_[:, 1:2], in_=s_[:, 0:1]), r=[s_], w=[s_])
                c.op("dve", lambda e: e.tensor_scalar(out=a2[:], in0=e_[:], scalar1=s_[:, 1:2], scalar2=None, op0=ALU.mult), r=[e_, s_], w=[a2])
                c.dma("pool", T["affc"].ap()[tt * 128:(tt + 1) * 128, :], a2[:], r=[a2], sbuf=a2)
                c.op("pe", lambda e: e.transpose(pa[:], a2[:], self.identf[:]), r=[a2, self.identf], w=[pa])
                c.op("act", lambda e: e.copy(out=aT[:, tt * 128:(tt + 1) * 128], in_=pa[:]), r=[pa], w=[aT])
            c.dma("sp", T["affT"].ap(), aT[:], r=[aT], w=[self.afft_buf], sbuf=aT)

    def stage_moe_x(self, l):
        c, T = self.c, self.T
        with ExitStack() as st:
            wbufs, pbanks = self.common_alloc(st, nw=4, npb=4)
            io = c.sb(st, "io", [128, 256], F32)
            pj = c.sb(st, "pj", [128, 16, 4], F32)
            pjb = [c.sb(st, "pjb%d" % i, [128, 16, 4], BF16) for i in range(2)]
            Sall = [c.sb(st, "Sall%d" % i, [128, 16, 256], BF16) for i in range(2)]
            c.dma("sp", io[:], T["iota256"].ap(), w=[io], sbuf=io)
            c.dma("sp", pj[:, :, 0:3], T["pj"].ap(), w=[pj], sbuf=pj)
            affc = c.sb(st, "affc", [128, 16, NE], F32)
            c.dma("sp", affc[:], T["affc"].ap().rearrange("(j p) e -> p j e", p=128), w=[affc], sbuf=affc)
            arow = [c.sb(st, "arow%d" % i, [128, L], F32) for i in range(2)]
            junk = c.sb(st, "junk", [128, L], BF16)
            rank = [c.sb(st, "rank%d" % i, [128, 16], F32) for i in range(2)]
            pidx = [c.ps(st, "pidx%d" % i, [128, 2, 4], F32) for i in range(2)]
            idf = [c.sb(st, "idf%d" % i, [128, 2, 6], F32) for i in range(2)]
            idxi = [[c.sb(st, "idx%d_%d" % (i, ch), [128, 1], I32) for ch in range(2)] for i in range(2)]
            xe = [c.sb(st, "xe%d" % i, [128, D], BF16) for i in range(2)]
            xeT = [c.sb(st, "xeT%d" % i, [128, 16, 256], BF16) for i in range(2)]
            hm = [c.sb(st, "hm%d" % i, [128, D], BF16) for i in range(2)]
            hmT = c.sb(st, "hmT", [128, 16, 256], BF16)
            sa = [c.sb(st, "sa%d" % i, [128, 512], F32) for i in range(2)]
            ysb = [c.sb(st, "ysb%d" % i, [128, D], F32) for i in range(2)]
            ptrs = [c.ps(st, "ptr%d" % i, [128, 4, 128], BF16) for i in range(2)]

            def rank_thunks(ex_):
                eb = ex_ % 2
                ar, rk = arow[eb], rank[eb]
                th = []

                def t0():
                    c.dma("sp", ar[:], row_bc(T["affT"], ex_ * L, L), r=[self.afft_buf], w=[ar], sbuf=ar)
                th.append(t0)
                for j in range(16):
                    def tj(j=j):
                        c.op("dve", lambda e: e.tensor_scalar(out=junk[:], in0=ar[:], scalar1=affc[:, j, ex_:ex_ + 1], scalar2=0.0, op0=ALU.is_gt, op1=ALU.add, accum_out=rk[:, j:j + 1]),
                             r=[ar, affc], w=[junk, rk])
                    th.append(tj)
                return th

            def slots_pre(ex_):
                eb = ex_ % 2
                rk, S_, pb_ = rank[eb], Sall[eb], pjb[eb]
                th = []
                th.append(lambda: c.op("dve", lambda e: e.tensor_copy(out=pj[:, :, 2], in_=affc[:, :, ex_]), r=[affc], w=[pj]))
                th.append(lambda: c.op("dve", lambda e: e.tensor_copy(out=pb_[:, :, 0:3], in_=pj[:, :, 0:3]), r=[pj], w=[pb_]))
                th.append(lambda: c.op("dve", lambda e: e.tensor_copy(out=pj[:, :, 3], in_=pb_[:, :, 2]), r=[pb_], w=[pj]))
                th.append(lambda: c.op("dve", lambda e: e.tensor_tensor(out=pb_[:, :, 3], in0=pj[:, :, 2], in1=pj[:, :, 3], op=ALU.subtract), r=[pj], w=[pb_]))
                for j in range(16):
                    th.append(lambda j=j: c.op("dve", lambda e: e.tensor_scalar(out=S_[:, j, :], in0=io[:], scalar1=rk[:, j:j + 1], scalar2=None, op0=ALU.is_equal), r=[io, rk], w=[S_]))
                return th

            def slots_idx(ex_):
                eb = ex_ % 2
                S_, pb_, pi_, f_ = Sall[eb], pjb[eb], pidx[eb], idf[eb]
                for ch in range(2):
                    for j in range(16):
                        c.op("pe", lambda e: e.matmul(pi_[:, ch, 0:4], S_[:, j, ch * 128:(ch + 1) * 128], pb_[:, j, :], start=(j == 0), stop=(j == 15)), r=[S_, pb_], w=[pi_], signal=(j == 15))
                c.op("dve", lambda e: e.tensor_copy(out=f_[:, :, 0:4], in_=pi_[:, :, 0:4]), r=[pi_], w=[f_])
                c.op("dve", lambda e: e.scalar_tensor_tensor(out=f_[:, :, 4], in0=f_[:, :, 1], scalar=128.0, in1=f_[:, :, 0], op0=ALU.mult, op1=ALU.add), r=[f_], w=[f_])
                c.op("dve", lambda e: e.tensor_tensor(out=f_[:, :, 5], in0=f_[:, :, 2], in1=f_[:, :, 3], op=ALU.add), r=[f_], w=[f_])
                for ch in range(2):
                    ix = idxi[eb][ch]
                    c.op("dve", lambda e: e.tensor_copy(out=ix[:], in_=f_[:, ch, 4:5]), r=[f_], w=[ix])

            def slots_gather(ex_):
                eb = ex_ % 2
                for ch in range(2):
                    ix = idxi[eb][ch]
                    x_ = xe[ch]
                    c.custom_dma("pool", lambda e: e.indirect_dma_start(out=x_[:], out_offset=None, in_=T["hmoe"].ap(), in_offset=bass.IndirectOffsetOnAxis(ap=ix[:, 0:1], axis=0)),
                                 r=[ix, self.hmoe_buf], w=[x_], sbuf=x_)

            def slots(ex_):
                for t in slots_pre(ex_):
                    t()
                slots_idx(ex_)
                slots_gather(ex_)

            def gather_T(ex_):
                for ch in range(2):
                    self.transpose_into(xe[ch], xeT[ex_ % 2], ch * 128, ptrs)

            def scatter(ex_):
                eb = ex_ % 2
                for ch in range(2):
                    ix = idxi[eb][ch]
                    y_ = ysb[ch]
                    c.custom_dma("pool", lambda e: e.indirect_dma_start(out=T["xres"].ap(), out_offset=bass.IndirectOffsetOnAxis(ap=ix[:, 0:1], axis=0), in_=y_[:], in_offset=None, compute_op=ALU.add),
                                 r=[ix, y_, self.xres_buf], w=[self.xres_buf], sbuf=y_)

            for t in rank_thunks(0):
                t()
            slots(0)
            gather_T(0)
            for ex_ in range(NE):
                eb = ex_ % 2
                xT_ = xeT[eb]
                f_ = idf[eb]
                wg = T["w_gate"].ap()[l, ex_]
                wu = T["w_up"].ap()[l, ex_]
                wd = T["w_down"].ap()[l, ex_]
                nxt = (rank_thunks(ex_ + 1) + slots_pre(ex_ + 1)) if ex_ + 1 < NE else []
                wcount = [0]

                def after_w(_nb):
                    wcount[0] += 1
                    if wcount[0] == 3 and ex_ > 0:
                        scatter(ex_ - 1)
                for nb in range(4):
                    def ev_gate(_nb, tt, pb, nb=nb):
                        s2 = sa[tt]
                        c.op("act", lambda e: e.activation(out=s2[:], in_=pb[:], func=AF.Silu), r=[pb], w=[s2])

                    def ev_up(_nb, tt, pb, nb=nb):
                        s2 = sa[tt]
                        c.op("dve", lambda e: e.tensor_tensor(out=hm[tt][:, nb * 512:(nb + 1) * 512], in0=pb[:], in1=s2[:], op=ALU.mult), r=[pb, s2], w=[hm[tt]])
                    self.gemm(xT_, 16, 256, wg[:, nb * 512:(nb + 1) * 512], 512, 512, "TM", ev_gate, wbufs, pbanks, after_w=after_w)
                    for _ in range(5):
                        if nxt:
                            nxt.pop(0)()
                    self.gemm(xT_, 16, 256, wu[:, nb * 512:(nb + 1) * 512], 512, 512, "TM", ev_up, wbufs, pbanks, after_w=after_w)
                    for _ in range(5):
                        if nxt:
                            nxt.pop(0)()
                while nxt:
                    nxt.pop(0)()
                if ex_ + 1 < NE:
                    slots_idx(ex_ + 1)
                for ch in range(2):
                    self.transpose_into(hm[ch], hmT, ch * 128, ptrs)

                def after_wd(nb):
                    if nb == 2 and ex_ + 1 < NE:
                        slots_gather(ex_ + 1)
                    if nb == 3 and ex_ + 1 < NE:
                        gather_T(ex_ + 1)

                def ev_down(nb, tt, pb):
                    y_ = ysb[tt]
                    if nb % 2 == 0:
                        c.op("act", lambda e: e.activation(out=y_[:, nb * 512:(nb + 1) * 512], in_=pb[:], func=AF.Copy, scale=f_[:, tt, 5:6]), r=[pb, f_], w=[y_])
                    else:
                        c.op("dve", lambda e: e.tensor_scalar(out=y_[:, nb * 512:(nb + 1) * 512], in0=pb[:], scalar1=f_[:, tt, 5:6], scalar2=None, op0=ALU.mult), r=[pb, f_], w=[y_])
                self.gemm(hmT, 16, 256, wd, D, 512, "TM", ev_down, wbufs, pbanks, after_w=after_wd)
            scatter(NE - 1)

    def stage_final(self):
        c, T = self.c, self.T
        with ExitStack() as st:
            self.common_alloc(st, nw=0, npb=0)
            gain = self.load_bc(st, "gain", T["final_norm"], 0, D)
            xts = [c.sb(st, "xt%d" % i, [128, D], F32) for i in range(3)]
            jk = [c.sb(st, "jk%d" % i, [128, D], BF16) for i in range(2)]
            os_ = [c.sb(st, "os%d" % i, [128, D], F32) for i in range(2)]
            sm = [c.sb(st, "sm%d" % i, [128, 3], F32) for i in range(2)]
            for tt in range(16):
                xt, j_, o_, s_ = xts[tt % 3], jk[tt % 2], os_[tt % 2], sm[tt % 2]
                c.dma("sp", xt[:], T["xres"].ap()[tt * 128:(tt + 1) * 128, :], r=[self.xres_buf], w=[xt], sbuf=xt)
                c.op("act", lambda e: e.activation(out=j_[:], in_=xt[:], func=AF.Square, accum_out=s_[:, 0:1]), r=[xt], w=[j_, s_])
                c.op("act", lambda e: e.activation(out=s_[:, 1:2], in_=s_[:, 0:1], func=AF.Sqrt, scale=1.0 / D, bias=self.epsb[:, 0:1]), r=[s_, self.epsb], w=[s_])
                c.op("dve", lambda e: e.reciprocal(out=s_[:, 2:3], in_=s_[:, 1:2]), r=[s_], w=[s_])
                c.op("dve", lambda e: e.scalar_tensor_tensor(out=o_[:], in0=xt[:], scalar=s_[:, 2:3], in1=gain[:], op0=ALU.mult, op1=ALU.mult), r=[xt, s_, gain], w=[o_])
                c.dma("pool", T["out"].ap()[tt * 128:(tt + 1) * 128, :], o_[:], r=[o_], sbuf=o_)


_CACHE = {}


def host_inputs(inputs, consts, b):
    m = {"x": np.ascontiguousarray(inputs["x"][b]), "mem": np.ascontiguousarray(inputs["mem"][b])}
    for k in WEIGHT_SHAPES:
        if k == "rpbY":
            continue
        m[k] = np.ascontiguousarray(np.asarray(inputs[k], np.float32))
    m["rpbY"] = rpb_layout(np.asarray(inputs["na_rpb"], np.float32))
    for k in CONST_DTYPES:
        m[k] = consts[k]
    return m


def kernel(**inputs):
    inputs = {k: np.asarray(v) for k, v in inputs.items()}
    if "prog" not in _CACHE:
        p = Prog()
        p.build()
        _CACHE["prog"] = p
    p = _CACHE["prog"]
    B = inputs["x"].shape[0]
    in_maps = [host_inputs(inputs, p.consts, b) for b in range(B)]
    res = run_bass_kernel_spmd(p.nc, in_maps, core_ids=[0, 2, 4, 6][:B])
    out = np.stack([np.asarray(r["out"], np.float32) for r in res.results], axis=0)
    return out
```

```python
import math
import numpy as np
import ml_dtypes
import concourse.bass as bass
import concourse.mybir as mybir
from concourse.bass_utils import run_bass_kernel_spmd
from contextlib import ExitStack

dt = mybir.dt
F32, BF16, I32 = dt.float32, dt.bfloat16, dt.int32
AF = mybir.ActivationFunctionType
ALU = mybir.AluOpType
AX = mybir.AxisListType

SAME_ENGINE_SYNC = True

D = 2048
L = 2048
DEPTH = 2
NMEM = 256
NA_H = 12
P_IN = 6912
NE = 16
CAP = 256
RMS_EPS = 1e-6
GN_EPS = 1e-5
PI = math.pi


class Sem:
    def __init__(self, h):
        self.h = h
        self.n = 0


class Buf:
    def __init__(self, t=None, name=""):
        self.t = t
        self.name = name
        self.w = None
        self.r = {}
        self.dsem = None

    def __getitem__(self, idx):
        return self.t[idx]


class Eng:
    def __init__(self, h, sem, name):
        self.h = h
        self.sem = sem
        self.name = name
        self.known = {}


class Ctx:
    def __init__(self, nc):
        self.nc = nc
        self.es = ExitStack()
        self.eng = {}
        for name, h in (("pe", nc.tensor), ("dve", nc.vector), ("act", nc.scalar),
                        ("pool", nc.gpsimd), ("sp", nc.sync)):
            s = Sem(self.es.enter_context(nc.semaphore("es_" + name)))
            self.eng[name] = Eng(h, s, name)
        self.free_sems = []
        self.all_dsems = []
        self.stage_bufs = []
        self.ninst = 0
        self.flip = 0

    def new_dsem(self):
        if self.free_sems:
            return self.free_sems.pop()
        s = Sem(self.es.enter_context(self.nc.semaphore("ds%d" % len(self.all_dsems))))
        self.all_dsems.append(s)
        return s

    def sb(self, st, name, shape, dtype):
        self.uid = getattr(self, "uid", 0) + 1
        name = "s%d_%s" % (self.uid, name)
        t = st.enter_context(self.nc.sbuf_tensor(name, list(shape), dtype))
        b = Buf(t, name)
        self.stage_bufs.append(b)
        return b

    def ps(self, st, name, shape, dtype=F32):
        self.uid = getattr(self, "uid", 0) + 1
        name = "p%d_%s" % (self.uid, name)
        t = st.enter_context(self.nc.psum_tensor(name, list(shape), dtype))
        b = Buf(t, name)
        self.stage_bufs.append(b)
        return b

    def _waits(self, E, r, w):
        waits = {}

        def need(s, v):
            if waits.get(s, 0) < v:
                waits[s] = v
        for b in r:
            if b.w is not None:
                need(*b.w)
        for b in w:
            if b.w is not None:
                need(*b.w)
            for s, v in b.r.items():
                need(s, v)
        for s, v in waits.items():
            if E.known.get(s, 0) >= v:
                continue
            if s is E.sem and (E.name == "pe" or not SAME_ENGINE_SYNC):
                continue
            E.h.wait_ge(s.h, v)
            self.ninst += 1
            E.known[s] = v

    def _record(self, dep, r, w):
        s, v = dep
        for b in r:
            if b.r.get(s, 0) < v:
                b.r[s] = v
        for b in w:
            b.w = dep
            b.r = {}

    def op(self, eng, fn, r=(), w=(), signal=True):
        E = self.eng[eng]
        self._waits(E, r, w)
        inst = fn(E.h)
        self.ninst += 1
        if signal:
            E.sem.n += 1
            inst.then_inc(E.sem.h, 1)
            dep = (E.sem, E.sem.n)
        else:
            dep = (E.sem, E.sem.n + 1)
        self._record(dep, r, w)
        return inst

    def dma(self, q, out, in_, r=(), w=(), sbuf=None, **kw):
        return self.custom_dma(q, lambda e: e.dma_start(out=out, in_=in_, **kw), r=r, w=w, sbuf=sbuf)

    def custom_dma(self, q, fn, r=(), w=(), sbuf=None):
        E = self.eng[q]
        self._waits(E, r, w)
        inst = fn(E.h)
        self.ninst += 1
        if sbuf.dsem is None:
            sbuf.dsem = self.new_dsem()
        s = sbuf.dsem
        s.n += 16
        inst.then_inc(s.h, 16)
        self._record((s, s.n), r, w)
        return inst

    def barrier(self):
        targets = [(e.sem, e.sem.n) for e in self.eng.values() if e.sem.n > 0]
        targets += [(s, s.n) for s in self.all_dsems if s.n > 0]
        for E in self.eng.values():
            for s, v in targets:
                if s is E.sem or E.known.get(s, 0) >= v:
                    continue
                E.h.wait_ge(s.h, v)
                self.ninst += 1
                E.known[s] = v

    def end_stage(self, dram_bufs=()):
        self.barrier()
        for b in self.stage_bufs:
            if b.dsem is not None:
                self.free_sems.append(b.dsem)
                b.dsem = None
        self.stage_bufs = []
        for b in dram_bufs:
            b.w = None
            b.r = {}

    def evac_eng(self):
        self.flip ^= 1
        return "act" if self.flip else "dve"


def copy_on(c, eng, out, in_, r, w, scale=None):
    if eng == "act":
        if scale is None:
            c.op("act", lambda e: e.copy(out=out, in_=in_), r=r, w=w)
        else:
            c.op("act", lambda e: e.mul(out, in_, float(scale)), r=r, w=w)
    else:
        if scale is None:
            c.op(eng, lambda e: e.tensor_copy(out=out, in_=in_), r=r, w=w)
        else:
            c.op(eng, lambda e: e.tensor_scalar(out=out, in0=in_, scalar1=float(scale), scalar2=None, op0=ALU.mult), r=r, w=w)


def bf(a):
    return np.ascontiguousarray(np.asarray(a, np.float32).astype(ml_dtypes.bfloat16))


def na_geometry():
    rows = 32

    def rs(r):
        return min(max(r - 4, 0), rows - 8)
    pairs = {}
    pats = {}
    c = np.arange(64)
    cs = np.clip(c - 8, 0, 48)
    colok = (c[:, None] >= cs[None, :]) & (c[:, None] < cs[None, :] + 16)
    tiles = []
    for i in range(16):
        lst = []
        for j in range(16):
            v = [[rs(2 * i + b) <= 2 * j + a < rs(2 * i + b) + 8 for b in range(2)] for a in range(2)]
            if not any(v[0] + v[1]):
                continue
            key = tuple(v[0] + v[1])
            if key not in pats:
                pats[key] = len(tiles)
                m = np.full((128, 128), -30000.0, np.float32)
                for a in range(2):
                    for b in range(2):
                        if v[a][b]:
                            blk = np.where(colok, 0.0, -30000.0)
                            m[a * 64:(a + 1) * 64, b * 64:(b + 1) * 64] = blk
                tiles.append(m)
            lst.append((j, 2 * j - 2 * i, pats[key]))
        pairs[i] = lst
    masks = np.stack(tiles, 0)
    return pairs, masks


def make_consts():
    cst = {}
    t = np.arange(L, dtype=np.float64)
    f = np.arange(L, dtype=np.float64)
    th = 2.0 * np.pi * (f[None, :] + 0.5) * t[:, None] / (2 * L)
    C = np.cos(th)
    S = np.sin(th)
    def fwd_layout(M):
        return M.reshape(16, 128, 16, 128).transpose(2, 1, 0, 3)
    cst["dftC"] = bf(fwd_layout(C))
    cst["dftS"] = bf(fwd_layout(S))
    def inv_layout(M):
        MT = M.T
        return MT.reshape(16, 128, 16, 128).transpose(2, 1, 0, 3)
    cst["dftCT"] = bf(inv_layout(C))
    cst["dftnST"] = bf(inv_layout(-S))
    tt = np.arange(L, dtype=np.float32)
    t01 = tt / (L - 1)
    bands = np.linspace(1e-4, 8 - 1, 8, dtype=np.float32)
    ang = (2.0 * np.float32(np.pi)) * (tt[:, None] / L) * bands[None, :]
    feats = np.concatenate([t01[:, None], np.cos(ang), -np.sin(ang)], axis=-1).astype(np.float32)
    cst["featsT"] = np.ascontiguousarray(feats.T)
    min_decay = math.log(1e-2) / 1.5
    max_decay = math.log(1e-2) / 0.3
    deltas = np.abs(np.linspace(min_decay, max_decay, 512, dtype=np.float32))
    cst["window"] = np.exp(-t01[:, None] * deltas[None, :]).astype(np.float32)
    inv = 1.0 / (10000.0 ** np.linspace(0.0, 1.0, 64, dtype=np.float32))
    angr = tt[:, None] * inv[None, :]
    cosT = np.cos(angr).T.astype(np.float32)
    sinT = np.sin(angr).T.astype(np.float32)
    cst["rotcos"] = np.ascontiguousarray(np.concatenate([cosT, cosT], 0))
    cst["rotsin"] = np.ascontiguousarray(np.concatenate([sinT, sinT], 0))
    R = np.zeros((128, 128), np.float32)
    for m in range(64):
        R[m + 64, m] = -1.0
    for m in range(64, 128):
        R[m - 64, m] = 1.0
    cst["rotR"] = bf(R)
    hidx = np.arange(6, dtype=np.float64)
    lgf = np.log1p(-np.exp2(-5.0 - hidx))
    lgb = np.log1p(-np.exp2(-5.5 - hidx))
    dd = np.zeros((6, 4, 128, 512), np.float32)
    jj = np.arange(128)[:, None]
    ii = np.arange(512)[None, :]
    for h in range(6):
        for pos in range(4):
            diff = ii - (jj + pos * 128)
            dd[h, pos] = np.where(diff >= 0, np.exp(lgf[h] * np.maximum(diff, 0)), np.exp(lgb[h] * np.maximum(-diff, 0)))
    cst["retD"] = bf(dd.transpose(0, 2, 1, 3))
    cst["retE"] = (ii - jj).astype(np.float32)
    rc_ = np.zeros((6, 128, 2, 512), np.float32)
    rt_ = np.zeros((128, 6, 24), np.float32)
    i5 = np.arange(512, dtype=np.float64)
    j1 = np.arange(128, dtype=np.float64)
    for h in range(6):
        rc_[h, :, 0, :] = np.exp(lgf[h] * i5)[None, :]
        rc_[h, :, 1, :] = np.exp(-lgb[h] * i5)[None, :]
        for m in range(1, 13):
            rt_[:, h, m - 1] = np.exp(lgf[h] * (128.0 * m - j1))
        for m in range(4, 16):
            rt_[:, h, 12 + m - 4] = np.exp(lgb[h] * (j1 + 128.0 * m))
    cst["retcol"] = rc_
    cst["rowtab"] = rt_
    cst["_lgf"] = lgf
    cst["_lgb"] = lgb
    pairs, masks = na_geometry()
    cst["namask"] = bf(masks.transpose(1, 0, 2))
    J = np.zeros((128, 128), np.float32)
    for a in range(2):
        for k in range(64):
            J[a * 64 + 63 - k, a * 64 + k] = 1.0
    cst["naJ"] = bf(J)
    cst["ident"] = bf(np.eye(128))
    cst["identf"] = np.eye(128, dtype=np.float32)
    cst["ones"] = bf(np.ones((128, 128)))
    cst["iota256"] = np.tile(np.arange(256, dtype=np.float32)[None, :], (128, 1))
    pj = np.zeros((128, 16, 3), np.float32)
    pj[:, :, 0] = np.arange(128)[:, None]
    pj[:, :, 1] = np.arange(16)[None, :]
    cst["pj"] = pj
    gw = np.zeros((128, 3), np.float32)
    gw[:, 0] = 1.0 / 768
    gw[:, 1] = 1.0 / 512
    gw[:, 2] = 1.0 / 768
    cst["ginvw"] = gw
    return cst, pairs


def rpb_layout(na_rpb):
    Y = np.zeros((DEPTH, NA_H, 15, 128), np.float32)
    for m in range(15):
        dri = 14 - m
        Y[:, :, m, 48:79] = na_rpb[:, :, dri, ::-1]
    return Y


CONST_DTYPES = {"dftC": BF16, "dftS": BF16, "dftCT": BF16, "dftnST": BF16, "featsT": F32, "window": F32,
                "rotcos": F32, "rotsin": F32, "rotR": BF16, "retD": BF16, "retE": F32, "namask": BF16,
                "naJ": BF16, "ident": BF16, "identf": F32, "ones": BF16, "iota256": F32, "pj": F32,
                "ginvw": F32, "retcol": F32, "rowtab": F32}

WEIGHT_SHAPES = {
    "norm_mix": (DEPTH, D), "w_in": (DEPTH, D, P_IN), "rpbY": (DEPTH, NA_H, 15, 128),
    "hy_conv_w": (DEPTH, 3, 1536), "hy_conv_b": (DEPTH, 1536), "hy_filt_w1": (DEPTH, 17, 64),
    "hy_filt_b1": (DEPTH, 64), "hy_filt_w2": (DEPTH, 64, 64), "hy_filt_b2": (DEPTH, 64),
    "hy_filt_w3": (DEPTH, 64, 2048), "hy_sin_freq": (DEPTH, 64), "hy_skip_d": (DEPTH, 2, 512),
    "branch_norm": (DEPTH, D), "w_out": (DEPTH, D, D), "norm_cross": (DEPTH, D), "mem_norm": (D,),
    "w_cq": (DEPTH, D, D), "w_ckv": (DEPTH, D, 2 * D), "w_co": (DEPTH, D, D), "norm_moe": (DEPTH, D),
    "w_router": (DEPTH, D, NE), "w_gate": (DEPTH, NE, D, D), "w_up": (DEPTH, NE, D, D),
    "w_down": (DEPTH, NE, D, D), "final_norm": (D,),
}


def row_bc(t, off, n, parts=128):
    return bass.AP(t, off, [[0, parts], [1, n]])


class Prog:
    def __init__(self, debug=False, stop_after=None, nlayers=DEPTH):
        self.debug = debug
        self.stop_after = stop_after
        self.nlayers = nlayers
        self.consts, self.na_pairs = make_consts()
        self.npat = self.consts["namask"].shape[1]
        nc = bass.Bass("TRN2", target_bir_lowering=False)
        self.nc = nc
        self.c = Ctx(nc)
        T = {}
        T["x"] = nc.dram_tensor("x", [L, D], F32, kind="ExternalInput")
        T["mem"] = nc.dram_tensor("mem", [NMEM, D], F32, kind="ExternalInput")
        for k, shp in WEIGHT_SHAPES.items():
            T[k] = nc.dram_tensor(k, list(shp), F32, kind="ExternalInput")
        for k, dty in CONST_DTYPES.items():
            T[k] = nc.dram_tensor(k, list(self.consts[k].shape), dty, kind="ExternalInput")
        T["out"] = nc.dram_tensor("out", [L, D], F32, kind="ExternalOutput")
        sk = "ExternalOutput" if debug else "Internal"
        for name, shp, dty in [
            ("xres", [L, D], F32), ("naqT", [768, L], BF16), ("nakT", [768, L], BF16), ("nav", [L, 768], BF16),
            ("hyp", [L, 1536], F32), ("hyx", [L, 1024], F32), ("rqT", [768, L], BF16), ("rkT", [768, L], BF16),
            ("rv", [L, 768], BF16), ("rg", [L, 768], F32), ("ymix", [L, D], F32), ("cqT", [D, L], BF16),
            ("coT", [D, L], BF16), ("ckT", [D, NMEM], BF16), ("cv", [NMEM, D], BF16), ("hmoe", [L, D], BF16),
            ("affT", [NE, L], F32), ("affc", [L, NE], F32), ("z0d", [L, 512], BF16),
        ]:
            T[name] = nc.dram_tensor(name, shp, dty, kind=sk)
        self.T = T
        self.xres_buf = Buf(None, "xres")
        self.hmoe_buf = Buf(None, "hmoe")
        self.afft_buf = Buf(None, "affT")

    def build(self):
        c = self.c
        with ExitStack() as gst:
            self.ident = c.sb(gst, "ident", [128, 128], BF16)
            self.identf = c.sb(gst, "identf", [128, 128], F32)
            self.ones = c.sb(gst, "ones", [128, 128], BF16)
            c.dma("sp", self.ident[:], self.T["ident"].ap(), w=[self.ident], sbuf=self.ident)
            c.dma("sp", self.identf[:], self.T["identf"].ap(), w=[self.identf], sbuf=self.identf)
            c.dma("sp", self.ones[:], self.T["ones"].ap(), w=[self.ones], sbuf=self.ones)
            c.stage_bufs = []
            stages = []
            for l in range(self.nlayers):
                xsrc = self.T["x"] if l == 0 else self.T["xres"]
                stages += [
                    ("inproj%d" % l, lambda l=l, xsrc=xsrc: self.stage_inproj(l, xsrc)),
                    ("na%d" % l, lambda l=l: self.stage_na(l)),
                    ("hy%d" % l, lambda l=l: self.stage_hyena(l)),
                    ("ret%d" % l, lambda l=l: self.stage_ret(l)),
                    ("outproj%d" % l, lambda l=l, xsrc=xsrc: self.stage_outproj(l, xsrc)),
                    ("ckv%d" % l, lambda l=l: self.stage_ckv(l)),
                    ("cq%d" % l, lambda l=l: self.stage_cq(l)),
                    ("cattn%d" % l, lambda l=l: self.stage_cattn(l)),
                    ("co%d" % l, lambda l=l: self.stage_co(l)),
                    ("moeh%d" % l, lambda l=l: self.stage_moe_h(l)),
                    ("moex%d" % l, lambda l=l: self.stage_moe_x(l)),
                ]
            stages.append(("final", self.stage_final))
            for name, fn in stages:
                fn()
                c.end_stage([self.xres_buf, self.hmoe_buf, self.afft_buf])
                if self.stop_after == name:
                    break
            c.barrier()
        return self.nc

    def load_bc(self, st, name, t, off, n, dtype=F32):
        b = self.c.sb(st, name, [128, n], dtype)
        self.c.dma("sp", b[:], row_bc(t, off, n), w=[b], sbuf=b)
        return b

    def rms_to_T(self, xt, gain, hb, xT, col0, ptrs, small, eps=RMS_EPS, width=D, no_T=False):
        c = self.c
        ss, rstd = small
        c.op("act", lambda e: e.activation(out=hb[:], in_=xt[:], func=AF.Square, accum_out=ss[:, 0:1]), r=[xt], w=[hb, ss])
        c.op("act", lambda e: e.activation(out=ss[:, 1:2], in_=ss[:, 0:1], func=AF.Sqrt, scale=1.0 / width, bias=self.epsb[:, 0:1]), r=[ss, self.epsb], w=[ss])
        c.op("dve", lambda e: e.reciprocal(out=rstd[:, 0:1], in_=ss[:, 1:2]), r=[ss], w=[rstd])
        c.op("dve", lambda e: e.scalar_tensor_tensor(out=hb[:], in0=xt[:], scalar=rstd[:, 0:1], in1=gain[:], op0=ALU.mult, op1=ALU.mult), r=[xt, rstd, gain], w=[hb])
        if not no_T:
            self.transpose_into(hb, xT, col0, ptrs)

    def transpose_into(self, hb, xT, col0, ptrs, nk=16):
        c = self.c
        for k4 in range(nk // 4):
            pt = ptrs[k4 % len(ptrs)]
            for q in range(4):
                k = k4 * 4 + q
                c.op("pe", lambda e: e.transpose(pt[:, q, :], hb[:, k * 128:(k + 1) * 128], self.ident[:]),
                     r=[hb, self.ident], w=[pt], signal=(q == 3))
            eng = c.evac_eng()
            copy_on(c, eng, xT[:, k4 * 4:(k4 + 1) * 4, col0:col0 + 128], pt[:, :, :], r=[pt], w=[xT])

    def gemm(self, xT, KC, Tn, w_ap, N, bw, mode, evac, wbufs, pbanks, after_w=None):
        c = self.c
        for nb in range(N // bw):
            wb = wbufs[self.wctr % len(wbufs)]
            self.wctr += 1
            c.dma("pool", wb[:, 0:KC, 0:bw], w_ap[:, nb * bw:(nb + 1) * bw].rearrange("(k p) n -> p k n", p=128), w=[wb], sbuf=wb)
            if after_w is not None:
                after_w(nb)
            if mode == "TM":
                for tt in range(Tn // 128):
                    pb = pbanks[self.pctr % len(pbanks)]
                    self.pctr += 1
                    for k in range(KC):
                        c.op("pe", lambda e: e.matmul(pb[:, 0:bw], xT[:, k, tt * 128:(tt + 1) * 128], wb[:, k, 0:bw], start=(k == 0), stop=(k == KC - 1)),
                             r=[xT, wb], w=[pb], signal=(k == KC - 1))
                    evac(nb, tt, pb)
            else:
                tbs = min(512, Tn)
                for sub in range(bw // 128):
                    for tb in range(Tn // tbs):
                        pb = pbanks[self.pctr % len(pbanks)]
                        self.pctr += 1
                        for k in range(KC):
                            c.op("pe", lambda e: e.matmul(pb[:, 0:tbs], wb[:, k, sub * 128:(sub + 1) * 128], xT[:, k, tb * tbs:(tb + 1) * tbs], start=(k == 0), stop=(k == KC - 1)),
                                 r=[xT, wb], w=[pb], signal=(k == KC - 1))
                        evac(nb * (bw // 128) + sub, tb, pb)


    def run_halves(self, nhalf, A, B, gemm_half):
        for tt in range(8):
            A(0, tt)
            B(0, tt)
        for half in range(nhalf):
            sched = []
            if half + 1 < nhalf:
                hn = half + 1
                sched = [[("A", 0), ("A", 1)], [("B", 0), ("B", 1), ("A", 2), ("A", 3)], [("B", 2), ("B", 3), ("A", 4), ("A", 5)],
                         [("B", 4), ("B", 5), ("A", 6), ("A", 7)], [("B", 6), ("B", 7)]]
            state = [0]

            def hook(_nb, half=half):
                if state[0] < len(sched):
                    for kind, tt in sched[state[0]]:
                        (A if kind == "A" else B)(half + 1, tt)
                    state[0] += 1
            gemm_half(half, hook)
            while state[0] < len(sched):
                hook(0)


    def conv_thunks(self, st, l):
        c, T = self.c, self.T
        cw = [self.load_bc(st, "cw%d" % k, T["hy_conv_w"], (l * 3 + k) * 1536, 1536) for k in range(3)]
        cb = self.load_bc(st, "cb", T["hy_conv_b"], l * 1536, 1536)
        pm = c.sb(st, "pm", [128, 1536], F32)
        p0 = c.sb(st, "p0", [128, 1536], F32)
        pp = c.sb(st, "pp", [128, 1536], F32)
        zt = c.sb(st, "zt", [128, 512], BF16)
        hyp = T["hyp"].ap()
        tiles = []
        for tt in range(16):
            a, b, d = pm, p0, pp
            r0 = tt * 128
            th = []
            if tt == 0:
                th.append(lambda a=a: c.op("dve", lambda e: e.memset(a[:], 0.0), w=[a]))
                th.append(lambda a=a: c.dma("sp", a[1:128, :], hyp[0:127, :], w=[a], sbuf=a))
            else:
                th.append(lambda a=a, r0=r0: c.dma("sp", a[:], hyp[r0 - 1:r0 + 127, :], w=[a], sbuf=a))
            th.append(lambda b=b, r0=r0: c.dma("sp", b[:], hyp[r0:r0 + 128, :], w=[b], sbuf=b))
            if tt == 15:
                th.append(lambda d=d: c.op("dve", lambda e: e.memset(d[:], 0.0), w=[d]))
                th.append(lambda d=d, r0=r0: c.dma("sp", d[0:127, :], hyp[r0 + 1:r0 + 128, :], w=[d], sbuf=d))
            else:
                th.append(lambda d=d, r0=r0: c.dma("sp", d[:], hyp[r0 + 1:r0 + 129, :], w=[d], sbuf=d))
            th.append(lambda a=a: c.op("pool", lambda e: e.tensor_tensor(out=a[:], in0=a[:], in1=cw[0][:], op=ALU.mult), r=[a, cw[0]], w=[a]))
            th.append(lambda b=b: c.op("dve", lambda e: e.tensor_tensor(out=b[:], in0=b[:], in1=cw[1][:], op=ALU.mult), r=[b, cw[1]], w=[b]))
            th.append(lambda d=d: c.op("pool", lambda e: e.tensor_tensor(out=d[:], in0=d[:], in1=cw[2][:], op=ALU.mult), r=[d, cw[2]], w=[d]))
            th.append(lambda b=b: c.op("dve", lambda e: e.tensor_tensor(out=b[:], in0=b[:], in1=cb[:], op=ALU.add), r=[b, cb], w=[b]))
            th.append(lambda a=a, d=d: c.op("dve", lambda e: e.tensor_tensor(out=a[:], in0=a[:], in1=d[:], op=ALU.add), r=[a, d], w=[a]))
            th.append(lambda a=a, b=b: c.op("dve", lambda e: e.tensor_tensor(out=b[:, 0:1024], in0=b[:, 0:1024], in1=a[:, 0:1024], op=ALU.add), r=[a, b], w=[b]))
            th.append(lambda a=a, b=b: c.op("dve", lambda e: e.tensor_tensor(out=zt[:], in0=b[:, 1024:1536], in1=a[:, 1024:1536], op=ALU.add), r=[a, b], w=[zt]))
            th.append(lambda b=b, r0=r0: c.dma("pool", T["hyx"].ap()[r0:r0 + 128, :], b[:, 0:1024], r=[b], sbuf=b))
            th.append(lambda r0=r0: c.dma("pool", T["z0d"].ap()[r0:r0 + 128, :], zt[:], r=[zt], sbuf=zt))
            tiles.append(th)
        return tiles

    def common_alloc(self, st, nw=3, npb=4, wk=16):
        c = self.c
        self.wctr = 0
        self.pctr = 0
        wbufs = [c.sb(st, "wb%d" % i, [128, wk, 512], BF16) for i in range(nw)]
        pbanks = [c.ps(st, "pb%d" % i, [128, 512], F32) for i in range(npb)]
        self.epsb = c.sb(st, "epsb", [128, 1], F32)
        c.op("dve", lambda e: e.memset(self.epsb[:], RMS_EPS), w=[self.epsb])
        return wbufs, pbanks

    def stage_inproj(self, l, xsrc):
        c, T = self.c, self.T
        with ExitStack() as st:
            wbufs, pbanks = self.common_alloc(st)
            gain = self.load_bc(st, "gain", T["norm_mix"], l * D, D)
            xTs = [c.sb(st, "xT%d" % i, [128, 16, 1024], BF16) for i in range(2)]
            xts = [c.sb(st, "xt%d" % i, [128, D], F32) for i in range(2)]
            hbs = [c.sb(st, "hb%d" % i, [128, D], BF16) for i in range(2)]
            ptrs = [c.ps(st, "ptr%d" % i, [128, 4, 128], BF16) for i in range(2)]
            smalls = [(c.sb(st, "ss%d" % i, [128, 2], F32), c.sb(st, "rs%d" % i, [128, 1], F32)) for i in range(2)]
            stg_bf = [c.sb(st, "sgb%d" % i, [128, 512], BF16) for i in range(4)]
            stg_f = [c.sb(st, "sgf%d" % i, [128, 512], F32) for i in range(3)]
            cnt = [0, 0]
            w_in = T["w_in"].ap()[l]

            def A(half, tt):
                xt = xts[tt % 2]
                c.dma("sp", xt[:], xsrc.ap()[half * 1024 + tt * 128:half * 1024 + (tt + 1) * 128, :], w=[xt], sbuf=xt)
                self.rms_to_T(xt, gain, hbs[tt % 2], None, 0, ptrs, smalls[tt % 2], no_T=True)

            def B(half, tt):
                self.transpose_into(hbs[tt % 2], xTs[half % 2], tt * 128, ptrs)

            def gemm_half(half, hook):
                t0 = half * 1024
                xT = xTs[half % 2]

                def mk_evac(dst, col_off, fm, dtype, scale, bw):
                    def evac(i0, i1, pb):
                        if dtype == BF16:
                            sg = stg_bf[cnt[0] % len(stg_bf)]
                            cnt[0] += 1
                        else:
                            sg = stg_f[cnt[1] % len(stg_f)]
                            cnt[1] += 1
                        eng = c.evac_eng()
                        if fm:
                            copy_on(c, eng, sg[:, 0:512], pb[:, 0:512], r=[pb], w=[sg], scale=scale)
                            c.dma("sp", dst.ap()[i0 * 128:(i0 + 1) * 128, t0 + i1 * 512:t0 + (i1 + 1) * 512], sg[:, 0:512], r=[sg], sbuf=sg)
                        else:
                            copy_on(c, eng, sg[:, 0:bw], pb[:, 0:bw], r=[pb], w=[sg], scale=scale)
                            c.dma("sp", dst.ap()[t0 + i1 * 128:t0 + (i1 + 1) * 128, col_off + i0 * bw:col_off + (i0 + 1) * bw], sg[:, 0:bw], r=[sg], sbuf=sg)
                    return evac
                groups = [
                    (0, 768, "FM", T["naqT"], BF16, 0.125, 384),
                    (768, 768, "FM", T["nakT"], BF16, None, 384),
                    (1536, 768, "TM", T["nav"], BF16, None, 384),
                    (2304, 1536, "TM", T["hyp"], F32, None, 512),
                    (3840, 768, "FM", T["rqT"], BF16, 128 ** -0.5, 384),
                    (4608, 768, "FM", T["rkT"], BF16, None, 384),
                    (5376, 768, "TM", T["rv"], BF16, None, 384),
                    (6144, 768, "TM", T["rg"], F32, None, 384),
                ]
                for (c0, n, mode, dst, dty, scale, bw) in groups:
                    self.gemm(xT, 16, 1024, w_in[:, c0:c0 + n], n, bw, mode, mk_evac(dst, 0, mode == "FM", dty, scale, bw), wbufs, pbanks, after_w=hook)
            self.run_halves(2, A, B, gemm_half)

    def stage_na(self, l):
        c, T = self.c, self.T
        DELTAS = [-6, -4, -2, 0, 2, 4, 6]
        with ExitStack() as st:
            qT = c.sb(st, "qT", [128, 6, L], BF16)
            kT = c.sb(st, "kT", [128, 6, L], BF16)
            vx = c.sb(st, "vx", [128, 16, 12, 65], BF16)
            COMBOS = sorted(set((dl, pat) for i in range(16) for (j, dl, pat) in self.na_pairs[i]))
            bt = c.sb(st, "bt", [128, NA_H * len(COMBOS), 128], BF16)
            bp = [c.sb(st, "bp%d" % i, [128, 7, 2, 64], BF16) for i in range(3)]
            mt = c.sb(st, "mt", [128, self.npat, 128], BF16)
            jm = c.sb(st, "jm", [128, 128], BF16)
            psc = [c.ps(st, "psc%d" % i, [128, 1024], F32) for i in range(2)]
            pso = [c.ps(st, "pso%d" % i, [128, 2, 512], F32) for i in range(2)]
            pT = [c.sb(st, "pT%d" % i, [128, 640], BF16) for i in range(3)]
            rden = [c.sb(st, "rden%d" % i, [128, 12], F32) for i in range(2)]
            yt = [c.sb(st, "yt%d" % i, [128, 768], F32) for i in range(2)]
            c.dma("sp", qT[:], T["naqT"].ap().rearrange("(k p) t -> p k t", p=128), w=[qT], sbuf=qT)
            c.dma("sp", kT[:], T["nakT"].ap().rearrange("(k p) t -> p k t", p=128), w=[kT], sbuf=kT)
            c.op("pool", lambda e: e.memset(vx[:], 1.0), w=[vx])
            for j in range(16):
                c.dma("sp", vx[:, j, :, 0:64], T["nav"].ap()[j * 128:(j + 1) * 128, :].rearrange("p (h d) -> p h d", d=64), w=[vx], sbuf=vx)
            c.dma("sp", mt[:], T["namask"].ap(), w=[mt], sbuf=mt)
            c.dma("sp", jm[:], T["naJ"].ap(), w=[jm], sbuf=jm)
            n = 0
            for h in range(NA_H):
                b = bp[h % 3]
                for a in range(2):
                    off = ((l * NA_H + h) * 15 + (1 - a)) * 128
                    src = bass.AP(T["rpbY"], off, [[1, 64], [256, 7], [128, 2], [1, 64]])
                    c.dma("pool", b[a * 64:(a + 1) * 64, :, :, :], src, w=[b], sbuf=b)
                for ci, (dl, pat) in enumerate(COMBOS):
                    slot = h * len(COMBOS) + ci
                    dd = (6 - dl) // 2
                    n += 1
                    pbb = psc[n % 2]
                    c.op("pe", lambda e: e.matmul(pbb[:, 0:128], jm[:], b[:, dd, :, :].rearrange("p b q -> p (b q)"), start=True, stop=False), r=[jm, b], w=[pbb], signal=False)
                    c.op("pe", lambda e: e.matmul(pbb[:, 0:128], self.ident[:], mt[:, pat, :], start=False, stop=True), r=[self.ident, mt], w=[pbb])
                    copy_on(c, c.evac_eng(), bt[:, slot, :], pbb[:, 0:128], r=[pbb], w=[bt])
            n = 0
            pend = []

            def epilogue(i, po):
                rd = rden[i % 2]
                y = yt[i % 2]
                for g in range(2):
                    c.op("dve", lambda e: e.reciprocal(out=rd[:, g * 6:(g + 1) * 6], in_=po[:, g, 0:390].rearrange("p (h d) -> p h d", d=65)[:, :, 64]), r=[po], w=[rd])
                for h in range(NA_H):
                    src = po[:, h // 6, (h % 6) * 65:(h % 6) * 65 + 64]
                    if h % 2 == 0:
                        c.op("dve", lambda e: e.tensor_scalar(out=y[:, h * 64:(h + 1) * 64], in0=src, scalar1=rd[:, h:h + 1], scalar2=None, op0=ALU.mult), r=[po, rd], w=[y])
                    else:
                        c.op("act", lambda e: e.activation(out=y[:, h * 64:(h + 1) * 64], in_=src, func=AF.Copy, scale=rd[:, h:h + 1]), r=[po, rd], w=[y])
                c.dma("sp", T["ymix"].ap()[i * 128:(i + 1) * 128, 0:768], y[:], r=[y], sbuf=y)

            conv = self.conv_thunks(st, l)
            for i in range(16):
                pairs = self.na_pairs[i]
                nk = len(pairs)
                po = pso[i % 2]
                cth = conv[i]
                for h in range(NA_H):
                    for _ in range(2):
                        if cth:
                            cth.pop(0)()
                    hp, off = h // 2, (h % 2) * 64
                    ps = psc[n % 2]
                    pt_ = pT[n % 3]
                    n += 1
                    for jj, (j, dl, pat) in enumerate(pairs):
                        reg = ps[:, jj * 128:(jj + 1) * 128]
                        slot = h * len(COMBOS) + COMBOS.index((dl, pat))
                        c.op("pe", lambda e: e.matmul(reg, kT[off:off + 64, hp, j * 128:(j + 1) * 128], qT[off:off + 64, hp, i * 128:(i + 1) * 128], start=True, stop=False),
                             r=[kT, qT], w=[ps], signal=False)
                        c.op("pe", lambda e: e.matmul(reg, self.ident[:], bt[:, slot, :], start=False, stop=True), r=[self.ident, bt], w=[ps], signal=(jj == nk - 1))
                    c.op("act", lambda e: e.activation(out=pt_[:, 0:nk * 128], in_=ps[:, 0:nk * 128], func=AF.Exp), r=[ps], w=[pt_])

                    def pv(i=i, h=h, pairs=pairs, nk=nk, pt_=pt_, po=po):
                        oreg = po[:, h // 6, (h % 6) * 65:(h % 6) * 65 + 65]
                        for jj, (j, dl, pat) in enumerate(pairs):
                            c.op("pe", lambda e: e.matmul(oreg, pt_[:, jj * 128:(jj + 1) * 128], vx[:, j, h, :], start=(jj == 0), stop=(jj == nk - 1)),
                                 r=[pt_, vx], w=[po], signal=(jj == nk - 1))
                        if h == NA_H - 1:
                            epilogue(i, po)
                    if pend:
                        pend.pop(0)()
                    pend.append(pv)
                while cth:
                    cth.pop(0)()
            while pend:
                pend.pop(0)()

    def stage_hyena(self, l):
        c, T = self.c, self.T
        with ExitStack() as st:
            z = [c.sb(st, "z%d" % i, [128, 16, 512], BF16) for i in range(2)]
            h2b = c.sb(st, "h2b", [64, L], BF16)
            w3b = c.sb(st, "w3b", [64, 2048], BF16)
            dsk = [self.load_bc(st, "dsk%d" % o, T["hy_skip_d"], (l * 2 + o) * 512, 512) for o in range(2)]
            c.dma("pool", w3b[:], T["hy_filt_w3"].ap()[l], w=[w3b], sbuf=w3b)
            c.dma("sp", z[0][:], T["z0d"].ap().rearrange("(j p) d -> p j d", p=128), w=[z[0]], sbuf=z[0])
            with ExitStack() as s1:
                fT = c.sb(s1, "fT", [17, L], F32)
                w1 = c.sb(s1, "w1", [17, 64], F32)
                w2 = c.sb(s1, "w2", [64, 64], F32)
                cols = c.sb(s1, "cols", [64, 6], F32)
                h1 = c.sb(s1, "h1", [64, L], F32)
                pre = [c.sb(s1, "pre%d" % i, [64, 512], F32) for i in range(2)]
                tmp = [c.sb(s1, "tmpm%d" % i, [64, 512], F32) for i in range(2)]
                pm_ = [c.ps(s1, "pmlp%d" % i, [64, 512], F32) for i in range(2)]
                c.dma("sp", fT[:], T["featsT"].ap(), w=[fT], sbuf=fT)
                c.dma("sp", w1[:], T["hy_filt_w1"].ap()[l], w=[w1], sbuf=w1)
                c.dma("sp", w2[:], T["hy_filt_w2"].ap()[l], w=[w2], sbuf=w2)
                c.dma("sp", cols[:, 0:1], T["hy_sin_freq"].ap()[l].rearrange("(p o) -> p o", o=1), w=[cols], sbuf=cols)
                c.dma("sp", cols[:, 1:2], T["hy_filt_b1"].ap()[l].rearrange("(p o) -> p o", o=1), w=[cols], sbuf=cols)
                c.dma("sp", cols[:, 2:3], T["hy_filt_b2"].ap()[l].rearrange("(p o) -> p o", o=1), w=[cols], sbuf=cols)
                c.op("dve", lambda e: e.tensor_tensor(out=cols[:, 3:4], in0=cols[:, 0:1], in1=cols[:, 1:2], op=ALU.mult), r=[cols], w=[cols])
                c.op("dve", lambda e: e.tensor_tensor(out=cols[:, 4:5], in0=cols[:, 0:1], in1=cols[:, 2:3], op=ALU.mult), r=[cols], w=[cols])

                def sin_layer(wt, kdim, src, dst, fbcol):
                    for tb in range(4):
                        pmm = pm_[tb % 2]
                        x_ = pre[tb % 2]
                        t_ = tmp[tb % 2]
                        c.op("pe", lambda e: e.matmul(pmm[:], wt[0:kdim, :], src[0:kdim, tb * 512:(tb + 1) * 512], start=True, stop=True), r=[wt, src], w=[pmm])
                        c.op("dve", lambda e: e.tensor_scalar(out=x_[:], in0=pmm[:], scalar1=cols[:, 0:1], scalar2=cols[:, fbcol:fbcol + 1], op0=ALU.mult, op1=ALU.add), r=[pmm, cols], w=[x_])
                        c.op("dve", lambda e: e.tensor_scalar(out=t_[:], in0=x_[:], scalar1=PI, scalar2=-2 * PI, op0=ALU.is_gt, op1=ALU.mult), r=[x_], w=[t_])
                        c.op("dve", lambda e: e.tensor_tensor(out=x_[:], in0=x_[:], in1=t_[:], op=ALU.add), r=[x_, t_], w=[x_])
                        c.op("dve", lambda e: e.tensor_scalar(out=t_[:], in0=x_[:], scalar1=-PI, scalar2=2 * PI, op0=ALU.is_lt, op1=ALU.mult), r=[x_], w=[t_])
                        c.op("dve", lambda e: e.tensor_tensor(out=x_[:], in0=x_[:], in1=t_[:], op=ALU.add), r=[x_, t_], w=[x_])
                        c.op("dve", lambda e: e.tensor_scalar(out=x_[:], in0=x_[:], scalar1=3.1415925, scalar2=-3.1415925, op0=ALU.min, op1=ALU.max), r=[x_], w=[x_])
                        c.op("act", lambda e: e.activation(out=dst[:, tb * 512:(tb + 1) * 512], in_=x_[:], func=AF.Sin), r=[x_], w=[dst])
                sin_layer(w1, 17, fT, h1, 3)
                sin_layer(w2, 64, h1, h2b, 4)
            c.barrier()
            with ExitStack() as s2:
                ksum = c.sb(s2, "ksum", [128, 16, 512], BF16)
                kdif = c.sb(s2, "kdif", [128, 16, 512], BF16)
                Pr = c.sb(s2, "Pr", [128, 16, 512], BF16)
                Pi = c.sb(s2, "Pi", [128, 16, 512], BF16)
                tabs = [(c.sb(s2, "tc%d" % i, [128, 16, 128], BF16), c.sb(s2, "ts%d" % i, [128, 16, 128], BF16)) for i in range(2)]
                pk = [c.ps(s2, "pk%d" % i, [128, 512], F32) for i in range(4)]
                pinv = [c.ps(s2, "pinv%d" % i, [128, 512], F32) for i in range(2)]
                pf = c.ps(s2, "pf", [128, 2, 512], F32)
                win = [c.sb(s2, "win%d" % i, [128, 512], F32) for i in range(2)]
                ff = [c.sb(s2, "ff%d" % i, [128, 512], F32) for i in range(2)]
                fb = [c.sb(s2, "fb%d" % i, [128, 512], F32) for i in range(2)]
                ksb = [c.sb(s2, "ksb%d" % i, [128, 2, 512], F32) for i in range(2)]
                ta = [c.sb(s2, "ta%d" % i, [128, 512], F32) for i in range(2)]
                tb_ = [c.sb(s2, "tbb%d" % i, [128, 512], F32) for i in range(2)]
                xg = [c.sb(s2, "xg%d" % i, [128, 512], F32) for i in range(2)]
                og = [c.sb(s2, "og%d" % i, [128, 512], F32) for i in range(2)]
                for o in range(2):
                    zin = z[o]
                    for tt in range(16):
                        w_ = win[tt % 2]
                        f_, b_ = ff[tt % 2], fb[tt % 2]
                        c.dma("sp", w_[:], T["window"].ap()[tt * 128:(tt + 1) * 128, :], w=[w_], sbuf=w_)
                        for dr in range(2):
                            c.op("pe", lambda e: e.matmul(pf[:, dr, :], h2b[:, tt * 128:(tt + 1) * 128], w3b[:, (o * 2 + dr) * 512:(o * 2 + dr + 1) * 512], start=True, stop=True),
                                 r=[h2b, w3b], w=[pf], signal=(dr == 1))
                        c.op("dve", lambda e: e.tensor_tensor(out=f_[:], in0=pf[:, 0, :], in1=w_[:], op=ALU.mult), r=[pf, w_], w=[f_])
                        c.op("dve", lambda e: e.tensor_tensor(out=b_[:], in0=pf[:, 1, :], in1=w_[:], op=ALU.mult), r=[pf, w_], w=[b_])
                        if tt == 0:
                            c.op("dve", lambda e: e.memset(b_[0:1, :], 0.0), w=[b_])
                        c.op("pool", lambda e: e.tensor_tensor(out=ksum[:, tt, :], in0=f_[:], in1=b_[:], op=ALU.add), r=[f_, b_], w=[ksum])
                        c.op("dve", lambda e: e.tensor_tensor(out=kdif[:, tt, :], in0=b_[:], in1=f_[:], op=ALU.subtract), r=[f_, b_], w=[kdif])
                    for fc in range(16):
                        tcb, tsb = tabs[fc % 2]
                        c.dma("sp", tcb[:], T["dftC"].ap()[fc], w=[tcb], sbuf=tcb)
                        c.dma("sp", tsb[:], T["dftS"].ap()[fc], w=[tsb], sbuf=tsb)
                        for gi, (tab, rhs) in enumerate([(tcb, ksum), (tsb, kdif), (tcb, zin), (tsb, zin)]):
                            for k in range(16):
                                c.op("pe", lambda e: e.matmul(pk[gi][:], tab[:, k, :], rhs[:, k, :], start=(k == 0), stop=(k == 15)), r=[tab, rhs], w=[pk[gi]], signal=(k == 15))
                        ks = ksb[fc % 2]
                        c.op("act", lambda e: e.copy(out=ks[:, 0, :], in_=pk[0][:]), r=[pk[0]], w=[ks])
                        c.op("act", lambda e: e.copy(out=ks[:, 1, :], in_=pk[1][:]), r=[pk[1]], w=[ks])
                        q0, q1, q2, q3 = ta[0], ta[1], tb_[0], tb_[1]
                        c.op("dve", lambda e: e.tensor_tensor(out=q0[:], in0=pk[2][:], in1=ks[:, 0, :], op=ALU.mult), r=[pk[2], ks], w=[q0])
                        c.op("dve", lambda e: e.tensor_tensor(out=q1[:], in0=pk[2][:], in1=ks[:, 1, :], op=ALU.mult), r=[pk[2], ks], w=[q1])
                        c.op("dve", lambda e: e.tensor_tensor(out=q2[:], in0=pk[3][:], in1=ks[:, 1, :], op=ALU.mult), r=[pk[3], ks], w=[q2])
                        c.op("dve", lambda e: e.tensor_tensor(out=q3[:], in0=pk[3][:], in1=ks[:, 0, :], op=ALU.mult), r=[pk[3], ks], w=[q3])
                        c.op("pool", lambda e: e.tensor_tensor(out=Pr[:, fc, :], in0=q0[:], in1=q2[:], op=ALU.add), r=[q0, q2], w=[Pr])
                        c.op("pool", lambda e: e.tensor_tensor(out=Pi[:, fc, :], in0=q1[:], in1=q3[:], op=ALU.subtract), r=[q1, q3], w=[Pi])
                    for tt in range(16):
                        tcb, tsb = tabs[tt % 2]
                        c.dma("sp", tcb[:], T["dftCT"].ap()[tt], w=[tcb], sbuf=tcb)
                        c.dma("sp", tsb[:], T["dftnST"].ap()[tt], w=[tsb], sbuf=tsb)
                        pv = pinv[tt % 2]
                        for k in range(16):
                            c.op("pe", lambda e: e.matmul(pv[:], tcb[:, k, :], Pr[:, k, :], start=(k == 0), stop=False), r=[tcb, Pr], w=[pv], signal=False)
                        for k in range(16):
                            c.op("pe", lambda e: e.matmul(pv[:], tsb[:, k, :], Pi[:, k, :], start=False, stop=(k == 15)), r=[tsb, Pi], w=[pv], signal=(k == 15))
                        x_ = xg[tt % 2]
                        a_ = ta[tt % 2]
                        c.dma("sp", x_[:], T["hyx"].ap()[tt * 128:(tt + 1) * 128, o * 512:(o + 1) * 512], w=[x_], sbuf=x_)
                        c.op("pool", lambda e: e.tensor_tensor(out=a_[:], in0=zin[:, tt, :], in1=dsk[o][:], op=ALU.mult), r=[zin, dsk[o]], w=[a_])
                        c.op("dve", lambda e: e.scalar_tensor_tensor(out=a_[:], in0=pv[:], scalar=1.0 / L, in1=a_[:], op0=ALU.mult, op1=ALU.add), r=[pv, a_], w=[a_])
                        if o == 0:
                            c.op("dve", lambda e: e.tensor_tensor(out=z[1][:, tt, :], in0=a_[:], in1=x_[:], op=ALU.mult), r=[a_, x_], w=[z[1]])
                        else:
                            o_ = og[tt % 2]
                            c.op("dve", lambda e: e.tensor_tensor(out=o_[:], in0=a_[:], in1=x_[:], op=ALU.mult), r=[a_, x_], w=[o_])
                            c.dma("act", T["ymix"].ap()[tt * 128:(tt + 1) * 128, 768:1280], o_[:], r=[o_], sbuf=o_)

    def stage_ret(self, l):
        c, T = self.c, self.T
        lgf, lgb = self.consts["_lgf"], self.consts["_lgb"]
        with ExitStack() as st:
            rc = c.sb(st, "rc", [128, L], F32)
            rs_ = c.sb(st, "rs", [128, L], F32)
            rR = c.sb(st, "rR", [128, 128], BF16)
            E = c.sb(st, "E", [128, 512], F32)
            gne = c.sb(st, "gne", [128, 1], F32)
            rowtab = c.sb(st, "rowtab", [128, 6, 24], F32)
            c.dma("sp", rowtab[:], T["rowtab"].ap(), w=[rowtab], sbuf=rowtab)
            colt = [c.sb(st, "colt%d" % i, [128, 2, 512], F32) for i in range(2)]
            qFB = [[c.sb(st, "qFB%d_%d" % (i, j), [128, L], BF16) for j in range(2)] for i in range(2)]
            c.op("dve", lambda e: e.memset(gne[:], GN_EPS), w=[gne])
            c.dma("sp", rc[:], T["rotcos"].ap(), w=[rc], sbuf=rc)
            c.dma("sp", rs_[:], T["rotsin"].ap(), w=[rs_], sbuf=rs_)
            c.dma("sp", rR[:], T["rotR"].ap(), w=[rR], sbuf=rR)
            c.dma("sp", E[:], T["retE"].ap(), w=[E], sbuf=E)
            raw = [c.sb(st, "raw%d" % i, [128, L], BF16) for i in range(2)]
            qk = [[c.sb(st, "qk%d_%d" % (i, j), [128, L], BF16) for j in range(2)] for i in range(2)]
            vh = [c.sb(st, "vh%d" % i, [128, 16, 128], BF16) for i in range(2)]
            dg = [c.sb(st, "dg%d" % i, [128, 4, 512], BF16) for i in range(2)]
            prot = [c.ps(st, "prot%d" % i, [128, 512], F32) for i in range(2)]
            pss = [c.ps(st, "pss%d" % i, [128, 512], F32) for i in range(2)]
            psy = [c.ps(st, "psy%d" % i, [128, 512], F32) for i in range(2)]
            ptt = [c.ps(st, "ptt%d" % i, [128, 4, 128], F32) for i in range(2)]
            t1 = [c.sb(st, "t1_%d" % i, [128, 512], F32) for i in range(2)]
            t2 = [c.sb(st, "t2_%d" % i, [128, 512], F32) for i in range(2)]
            dec = [c.sb(st, "dec%d" % i, [128, 512], BF16) for i in range(3)]
            pT = [c.sb(st, "pT%d" % i, [128, 512], BF16) for i in range(3)]
            yT = [c.sb(st, "yT%d" % i, [128, 512], F32) for i in range(2)]
            gt = [c.sb(st, "gt%d" % i, [128, 4, 128], F32) for i in range(2)]
            sg = [c.sb(st, "sg%d" % i, [128, 4, 128], F32) for i in range(2)]
            yo = [c.sb(st, "yo%d" % i, [128, 4, 128], F32) for i in range(2)]
            stt = [c.sb(st, "stt%d" % i, [128, 4, 6], F32) for i in range(2)]
            mv = [c.sb(st, "mv%d" % i, [128, 4, 4], F32) for i in range(2)]
            n = 0
            pend = []
            ypend = []
            for h in range(6):
                hb = h % 2
                for wi, src in enumerate([T["rqT"], T["rkT"]]):
                    rw = raw[wi]
                    dstb = qk[hb][wi]
                    c.dma("sp", rw[:], src.ap()[h * 128:(h + 1) * 128, :], w=[rw], sbuf=rw)
                    for tb in range(4):
                        sl = slice(tb * 512, (tb + 1) * 512)
                        pr = prot[tb % 2]
                        a_, b_ = t1[tb % 2], t2[tb % 2]
                        c.op("pe", lambda e: e.matmul(pr[:], rR[:], rw[:, sl], start=True, stop=True), r=[rR, rw], w=[pr])
                        c.op("dve", lambda e: e.tensor_tensor(out=a_[:], in0=pr[:], in1=rs_[:, sl], op=ALU.mult), r=[pr, rs_], w=[a_])
                        c.op("pool", lambda e: e.tensor_tensor(out=b_[:], in0=rw[:, sl], in1=rc[:, sl], op=ALU.mult), r=[rw, rc], w=[b_])
                        c.op("dve", lambda e: e.tensor_tensor(out=dstb[:, sl], in0=a_[:], in1=b_[:], op=ALU.add), r=[a_, b_], w=[dstb])
                qr, kr = qk[hb]
                v_ = vh[hb]
                d_ = dg[hb]
                c.dma("sp", v_[:], T["rv"].ap()[:, h * 128:(h + 1) * 128].rearrange("(j p) d -> p j d", p=128), w=[v_], sbuf=v_)
                c.dma("sp", d_[:], T["retD"].ap()[h], w=[d_], sbuf=d_)
                ct = colt[hb]
                c.dma("sp", ct[:], T["retcol"].ap()[h], w=[ct], sbuf=ct)
                qF, qB = qFB[hb]
                for tb in range(4):
                    sl = slice(tb * 512, (tb + 1) * 512)
                    c.op("pool", lambda e: e.tensor_tensor(out=qF[:, sl], in0=qr[:, sl], in1=ct[:, 0, :], op=ALU.mult), r=[qr, ct], w=[qF])
                    c.op("pool", lambda e: e.tensor_tensor(out=qB[:, sl], in0=qr[:, sl], in1=ct[:, 1, :], op=ALU.mult), r=[qr, ct], w=[qB])
                for ib in range(4):
                    py = psy[ib % 2]
                    for j in range(16):
                        ps = pss[n % 2]
                        p_ = pT[n % 3]
                        n += 1
                        offv = ib * 512 - j * 128
                        if 0 <= j - ib * 4 < 4:
                            c.op("pe", lambda e: e.matmul(ps[:], kr[:, j * 128:(j + 1) * 128], qr[:, ib * 512:(ib + 1) * 512], start=True, stop=True), r=[kr, qr], w=[ps])
                            decap = d_[:, j - ib * 4, :]
                            c.op("dve", lambda e: e.tensor_tensor(out=p_[:], in0=ps[:], in1=decap, op=ALU.mult), r=[ps, d_], w=[p_])
                        else:
                            if offv > 0:
                                qs, ti = qF, offv // 128 - 1
                            else:
                                qs, ti = qB, 12 + (-offv) // 128 - 4
                            c.op("pe", lambda e: e.matmul(ps[:], kr[:, j * 128:(j + 1) * 128], qs[:, ib * 512:(ib + 1) * 512], start=True, stop=True), r=[kr, qs], w=[ps])
                            c.op("act", lambda e: e.activation(out=p_[:], in_=ps[:], func=AF.Copy, scale=rowtab[:, h, ti:ti + 1]), r=[ps, rowtab], w=[p_])
                        def ymm(py=py, v_=v_, j=j, p_=p_):
                            c.op("pe", lambda e: e.matmul(py[:], v_[:, j, :], p_[:], start=(j == 0), stop=(j == 15)), r=[v_, p_], w=[py], signal=(j == 15))
                        if ypend:
                            ypend.pop(0)()
                        ypend.append(ymm)
                    while ypend:
                        ypend.pop(0)()
                    y_ = yT[ib % 2]
                    c.op("act", lambda e: e.copy(out=y_[:], in_=py[:]), r=[py], w=[y_])

                    def epilogue(h=h, ib=ib, y_=y_):
                        k_ = (ib + 4 * h) % 2
                        pt4 = ptt[k_]
                        for q in range(4):
                            c.op("pe", lambda e: e.transpose(pt4[:, q, :], y_[:, q * 128:(q + 1) * 128], self.identf[:]), r=[y_, self.identf], w=[pt4], signal=(q == 3))
                        g_, s_, o_, st_, m_ = gt[k_], sg[k_], yo[k_], stt[k_], mv[k_]
                        rows = slice(ib * 512, (ib + 1) * 512)
                        c.dma("sp", g_[:], T["rg"].ap()[rows, h * 128:(h + 1) * 128].rearrange("(q p) d -> p q d", p=128), w=[g_], sbuf=g_)
                        c.op("act", lambda e: e.activation(out=s_[:], in_=g_[:], func=AF.Silu), r=[g_], w=[s_])
                        for q in range(4):
                            c.op("dve", lambda e: e.bn_stats(out=st_[:, q, :], in_=pt4[:, q, :]), r=[pt4], w=[st_])
                        for q in range(4):
                            c.op("dve", lambda e: e.bn_aggr(out=m_[:, q, 0:2], in_=st_[:, q, :]), r=[st_], w=[m_])
                        c.op("act", lambda e: e.activation(out=m_[:, :, 2], in_=m_[:, :, 1], func=AF.Sqrt, bias=gne[:, 0:1]), r=[m_, gne], w=[m_])
                        c.op("dve", lambda e: e.reciprocal(out=m_[:, :, 3], in_=m_[:, :, 2]), r=[m_], w=[m_])
                        c.op("dve", lambda e: e.tensor_tensor(out=o_[:], in0=pt4[:], in1=m_[:, :, 0:1].to_broadcast([128, 4, 128]), op=ALU.subtract), r=[pt4, m_], w=[o_])
                        c.op("dve", lambda e: e.tensor_tensor(out=o_[:], in0=o_[:], in1=m_[:, :, 3:4].to_broadcast([128, 4, 128]), op=ALU.mult), r=[o_, m_], w=[o_])
                        c.op("pool", lambda e: e.tensor_tensor(out=o_[:], in0=o_[:], in1=s_[:], op=ALU.mult), r=[o_, s_], w=[o_])
                        c.dma("pool", T["ymix"].ap()[rows, 1280 + h * 128:1280 + (h + 1) * 128].rearrange("(q p) d -> p q d", p=128), o_[:], r=[o_], sbuf=o_)
                    if pend:
                        pend.pop(0)()
                    pend.append(epilogue)
            while pend:
                pend.pop(0)()

    def ret_bias(self, st, val):
        c = self.c
        key = (id(st), round(val, 9))
        if not hasattr(self, "_rb"):
            self._rb = {}
        if key not in self._rb:
            b = c.sb(st, "rb%d" % len(self._rb), [128, 1], F32)
            c.op("pool", lambda e: e.memset(b[:], float(val)), w=[b])
            self._rb[key] = b
        return self._rb[key]

    def stage_outproj(self, l, xsrc):
        c, T = self.c, self.T
        with ExitStack() as st:
            wbufs, pbanks = self.common_alloc(st)
            gain = self.load_bc(st, "gain", T["branch_norm"], l * D, D)
            ginvw = c.sb(st, "ginvw", [128, 3], F32)
            c.dma("sp", ginvw[:], T["ginvw"].ap(), w=[ginvw], sbuf=ginvw)
            xTs = [c.sb(st, "xT%d" % i, [128, 16, 1024], BF16) for i in range(2)]
            xts = [c.sb(st, "xt%d" % i, [128, D], F32) for i in range(2)]
            hbs = [c.sb(st, "hb%d" % i, [128, D], BF16) for i in range(2)]
            ptrs = [c.ps(st, "ptr%d" % i, [128, 4, 128], BF16) for i in range(2)]
            sm = [c.sb(st, "sm%d" % i, [128, 12], F32) for i in range(2)]
            xs = [c.sb(st, "xs%d" % i, [128, 512], F32) for i in range(4)]
            so = [c.sb(st, "so%d" % i, [128, 512], F32) for i in range(4)]
            cnt = [0]
            segs = [(0, 768), (768, 1280), (1280, 2048)]

            def A(half, tt):
                t0 = half * 1024
                xt, hb, s_ = xts[tt % 2], hbs[tt % 2], sm[tt % 2]
                c.dma("sp", xt[:], T["ymix"].ap()[t0 + tt * 128:t0 + (tt + 1) * 128, :], w=[xt], sbuf=xt)
                for g, (a, b) in enumerate(segs):
                    c.op("act", lambda e: e.activation(out=hb[:, a:b], in_=xt[:, a:b], func=AF.Square, accum_out=s_[:, g:g + 1]), r=[xt], w=[hb, s_])
                c.op("dve", lambda e: e.tensor_tensor(out=s_[:, 3:6], in0=s_[:, 0:3], in1=ginvw[:], op=ALU.mult), r=[s_, ginvw], w=[s_])
                c.op("act", lambda e: e.activation(out=s_[:, 6:9], in_=s_[:, 3:6], func=AF.Sqrt, bias=self.epsb[:, 0:1]), r=[s_, self.epsb], w=[s_])
                c.op("dve", lambda e: e.reciprocal(out=s_[:, 9:12], in_=s_[:, 6:9]), r=[s_], w=[s_])
                for g, (a, b) in enumerate(segs):
                    c.op("dve", lambda e: e.scalar_tensor_tensor(out=hb[:, a:b], in0=xt[:, a:b], scalar=s_[:, 9 + g:10 + g], in1=gain[:, a:b], op0=ALU.mult, op1=ALU.mult), r=[xt, s_, gain], w=[hb])

            def B(half, tt):
                self.transpose_into(hbs[tt % 2], xTs[half % 2], tt * 128, ptrs)

            def gemm_half(half, hook):
                self.gemm(xTs[half % 2], 16, 1024, T["w_out"].ap()[l], D, 512, "TM", self.mk_resid_evac(xsrc, half * 1024, xs, so, cnt), wbufs, pbanks, after_w=hook)
            self.run_halves(2, A, B, gemm_half)

    def mk_resid_evac(self, xsrc, t0, xs, so, cnt):
        c, T = self.c, self.T

        def evac(nb, tt, pb):
            x_ = xs[cnt[0] % len(xs)]
            o_ = so[cnt[0] % len(so)]
            cnt[0] += 1
            rows = slice(t0 + tt * 128, t0 + (tt + 1) * 128)
            cols = slice(nb * 512, (nb + 1) * 512)
            c.dma("act", x_[:], xsrc.ap()[rows, cols], r=[self.xres_buf] if xsrc is T["xres"] else [], w=[x_], sbuf=x_)
            c.op("dve", lambda e: e.tensor_tensor(out=o_[:], in0=pb[:], in1=x_[:], op=ALU.add), r=[pb, x_], w=[o_])
            c.dma("sp", T["xres"].ap()[rows, cols], o_[:], r=[o_], sbuf=o_)
        return evac

    def stage_ckv(self, l):
        c, T = self.c, self.T
        with ExitStack() as st:
            wbufs, pbanks = self.common_alloc(st)
            gain = self.load_bc(st, "gain", T["mem_norm"], 0, D)
            xT = c.sb(st, "xT", [128, 16, 256], BF16)
            xts = [c.sb(st, "xt%d" % i, [128, D], F32) for i in range(2)]
            hbs = [c.sb(st, "hb%d" % i, [128, D], BF16) for i in range(2)]
            ptrs = [c.ps(st, "ptr%d" % i, [128, 4, 128], BF16) for i in range(2)]
            smalls = [(c.sb(st, "ss%d" % i, [128, 2], F32), c.sb(st, "rs%d" % i, [128, 1], F32)) for i in range(2)]
            sg = [c.sb(st, "sg%d" % i, [128, 512], BF16) for i in range(4)]
            cnt = [0]
            for tt in range(2):
                xt = xts[tt]
                c.dma("sp", xt[:], T["mem"].ap()[tt * 128:(tt + 1) * 128, :], w=[xt], sbuf=xt)
                self.rms_to_T(xt, gain, hbs[tt], xT, tt * 128, ptrs, smalls[tt])

            def evac_k(fc, tb, pb):
                s_ = sg[cnt[0] % 4]
                cnt[0] += 1
                copy_on(c, c.evac_eng(), s_[:, 0:256], pb[:, 0:256], r=[pb], w=[s_])
                c.dma("sp", T["ckT"].ap()[fc * 128:(fc + 1) * 128, :], s_[:, 0:256], r=[s_], sbuf=s_)

            def evac_v(nb, tt, pb):
                s_ = sg[cnt[0] % 4]
                cnt[0] += 1
                copy_on(c, c.evac_eng(), s_[:], pb[:], r=[pb], w=[s_])
                c.dma("sp", T["cv"].ap()[tt * 128:(tt + 1) * 128, nb * 512:(nb + 1) * 512], s_[:], r=[s_], sbuf=s_)
            wk = T["w_ckv"].ap()[l]
            self.gemm(xT, 16, 256, wk[:, 0:D], D, 512, "FM", evac_k, wbufs, pbanks)
            self.gemm(xT, 16, 256, wk[:, D:2 * D], D, 512, "TM", evac_v, wbufs, pbanks)

    def stage_cq(self, l):
        c, T = self.c, self.T
        with ExitStack() as st:
            wbufs, pbanks = self.common_alloc(st)
            gain = self.load_bc(st, "gain", T["norm_cross"], l * D, D)
            xTs = [c.sb(st, "xT%d" % i, [128, 16, 1024], BF16) for i in range(2)]
            xts = [c.sb(st, "xt%d" % i, [128, D], F32) for i in range(2)]
            hbs = [c.sb(st, "hb%d" % i, [128, D], BF16) for i in range(2)]
            ptrs = [c.ps(st, "ptr%d" % i, [128, 4, 128], BF16) for i in range(2)]
            smalls = [(c.sb(st, "ss%d" % i, [128, 2], F32), c.sb(st, "rs%d" % i, [128, 1], F32)) for i in range(2)]
            sg = [c.sb(st, "sg%d" % i, [128, 512], BF16) for i in range(4)]
            cnt = [0]

            def A(half, tt):
                xt = xts[tt % 2]
                c.dma("sp", xt[:], T["xres"].ap()[half * 1024 + tt * 128:half * 1024 + (tt + 1) * 128, :], w=[xt], sbuf=xt)
                self.rms_to_T(xt, gain, hbs[tt % 2], None, 0, ptrs, smalls[tt % 2], no_T=True)

            def B(half, tt):
                self.transpose_into(hbs[tt % 2], xTs[half % 2], tt * 128, ptrs)

            def gemm_half(half, hook):
                t0 = half * 1024

                def evac(fc, tb, pb):
                    s_ = sg[cnt[0] % 4]
                    cnt[0] += 1
                    copy_on(c, c.evac_eng(), s_[:], pb[:], r=[pb], w=[s_], scale=512 ** -0.5)
                    c.dma("sp", T["cqT"].ap()[fc * 128:(fc + 1) * 128, t0 + tb * 512:t0 + (tb + 1) * 512], s_[:], r=[s_], sbuf=s_)
                self.gemm(xTs[half % 2], 16, 1024, T["w_cq"].ap()[l], D, 512, "FM", evac, wbufs, pbanks, after_w=hook)
            self.run_halves(2, A, B, gemm_half)

    def stage_cattn(self, l):
        c, T = self.c, self.T
        with ExitStack() as st:
            kT = c.sb(st, "kT", [128, 16, 256], BF16)
            v = c.sb(st, "v", [128, 2, D], BF16)
            c.dma("sp", kT[:], T["ckT"].ap().rearrange("(k p) m -> p k m", p=128), w=[kT], sbuf=kT)
            c.dma("sp", v[:], T["cv"].ap().rearrange("(j p) d -> p j d", p=128), w=[v], sbuf=v)
            qTs = [c.sb(st, "qT%d" % i, [128, 16, 512], BF16) for i in range(2)]
            oTs = [c.sb(st, "oT%d" % i, [128, 16, 512], BF16) for i in range(2)]
            pT = [c.sb(st, "pT%d" % i, [128, 2, 512], BF16) for i in range(2)]
            rd = [c.sb(st, "rd%d" % i, [128, 512], F32) for i in range(2)]
            pss = [c.ps(st, "pss%d" % i, [128, 512], F32) for i in range(3)]
            psd = c.ps(st, "psd", [128, 512], F32)
            pso = [c.ps(st, "pso%d" % i, [128, 512], F32) for i in range(3)]
            n = 0
            m = 0
            for tb in range(4):
                q_, o_ = qTs[tb % 2], oTs[tb % 2]
                c.dma("sp", q_[:], T["cqT"].ap()[:, tb * 512:(tb + 1) * 512].rearrange("(k p) t -> p k t", p=128), w=[q_], sbuf=q_)
                for hh in range(4):
                    p_ = pT[hh % 2]
                    for mh in range(2):
                        ps = pss[n % 3]
                        n += 1
                        for dc in range(4):
                            c.op("pe", lambda e: e.matmul(ps[:], kT[:, hh * 4 + dc, mh * 128:(mh + 1) * 128], q_[:, hh * 4 + dc, :], start=(dc == 0), stop=(dc == 3)), r=[kT, q_], w=[ps], signal=(dc == 3))
                        c.op("act", lambda e: e.activation(out=p_[:, mh, :], in_=ps[:], func=AF.Exp), r=[ps], w=[p_])
                    for mh in range(2):
                        c.op("pe", lambda e: e.matmul(psd[:], self.ones[:], p_[:, mh, :], start=(mh == 0), stop=(mh == 1)), r=[self.ones, p_], w=[psd], signal=(mh == 1))
                    r_ = rd[hh % 2]
                    c.op("dve", lambda e: e.reciprocal(out=r_[:], in_=psd[:]), r=[psd], w=[r_])
                    for dvc in range(4):
                        po = pso[m % 3]
                        m += 1
                        for mh in range(2):
                            c.op("pe", lambda e: e.matmul(po[:], v[:, mh, hh * 512 + dvc * 128:hh * 512 + (dvc + 1) * 128], p_[:, mh, :], start=(mh == 0), stop=(mh == 1)), r=[v, p_], w=[po], signal=(mh == 1))
                        c.op("dve", lambda e: e.tensor_tensor(out=o_[:, hh * 4 + dvc, :], in0=po[:], in1=r_[:], op=ALU.mult), r=[po, r_], w=[o_])
                c.dma("pool", T["coT"].ap()[:, tb * 512:(tb + 1) * 512].rearrange("(k p) t -> p k t", p=128), o_[:], r=[o_], sbuf=o_)

    def stage_co(self, l):
        c, T = self.c, self.T
        with ExitStack() as st:
            wbufs, pbanks = self.common_alloc(st)
            xT = c.sb(st, "xT", [128, 16, 1024], BF16)
            xs = [c.sb(st, "xs%d" % i, [128, 512], F32) for i in range(4)]
            so = [c.sb(st, "so%d" % i, [128, 512], F32) for i in range(4)]
            cnt = [0]
            for half in range(2):
                t0 = half * 1024
                c.dma("sp", xT[:], T["coT"].ap()[:, t0:t0 + 1024].rearrange("(k p) t -> p k t", p=128), w=[xT], sbuf=xT)
                self.gemm(xT, 16, 1024, T["w_co"].ap()[l], D, 512, "TM", self.mk_resid_evac(T["xres"], t0, xs, so, cnt), wbufs, pbanks)

    def stage_moe_h(self, l):
        c, T = self.c, self.T
        with ExitStack() as st:
            self.common_alloc(st, nw=0, npb=0)
            gain = self.load_bc(st, "gain", T["norm_moe"], l * D, D)
            wr = c.sb(st, "wr", [128, 16, NE], F32)
            wrh = c.sb(st, "wrh", [128, 16, NE], BF16)
            wrl = c.sb(st, "wrl", [128, 16, NE], BF16)
            wtmp = c.sb(st, "wtmp", [128, 16, NE], F32)
            c.dma("sp", wr[:], T["w_router"].ap()[l].rearrange("(k p) e -> p k e", p=128), w=[wr], sbuf=wr)
            c.op("dve", lambda e: e.tensor_copy(out=wrh[:], in_=wr[:]), r=[wr], w=[wrh])
            c.op("dve", lambda e: e.tensor_copy(out=wtmp[:], in_=wrh[:]), r=[wrh], w=[wtmp])
            c.op("dve", lambda e: e.tensor_tensor(out=wrl[:], in0=wr[:], in1=wtmp[:], op=ALU.subtract), r=[wr, wtmp], w=[wrl])
            xts = [c.sb(st, "xt%d" % i, [128, D], F32) for i in range(2)]
            hfs = [c.sb(st, "hf%d" % i, [128, D], F32) for i in range(2)]
            hbs = [c.sb(st, "hb%d" % i, [128, D], BF16) for i in range(2)]
            hls = [c.sb(st, "hl%d" % i, [128, D], BF16) for i in range(2)]
            hT = [c.sb(st, "hT%d" % i, [128, 16, 128], BF16) for i in range(2)]
            lT = [c.sb(st, "lT%d" % i, [128, 16, 128], BF16) for i in range(2)]
            ptrs = [c.ps(st, "ptr%d" % i, [128, 4, 128], BF16) for i in range(2)]
            pl = [c.ps(st, "pl%d" % i, [128, NE], F32) for i in range(2)]
            pa = c.ps(st, "pa", [NE, 128], F32)
            smalls = [(c.sb(st, "ss%d" % i, [128, 2], F32), c.sb(st, "rs%d" % i, [128, 1], F32)) for i in range(2)]
            ex = [c.sb(st, "ex%d" % i, [128, NE], F32) for i in range(2)]
            se = [c.sb(st, "se%d" % i, [128, 2], F32) for i in range(2)]
            af = [c.sb(st, "af%d" % i, [128, NE], F32) for i in range(2)]
            aT = c.sb(st, "aT", [NE, L], F32)
            for tt in range(16):
                xt, hf, hb, hl = xts[tt % 2], hfs[tt % 2], hbs[tt % 2], hls[tt % 2]
                ss, rstd = smalls[tt % 2]
                c.dma("sp", xt[:], T["xres"].ap()[tt * 128:(tt + 1) * 128, :], r=[self.xres_buf], w=[xt], sbuf=xt)
                c.op("act", lambda e: e.activation(out=hb[:], in_=xt[:], func=AF.Square, accum_out=ss[:, 0:1]), r=[xt], w=[hb, ss])
                c.op("act", lambda e: e.activation(out=ss[:, 1:2], in_=ss[:, 0:1], func=AF.Sqrt, scale=1.0 / D, bias=self.epsb[:, 0:1]), r=[ss, self.epsb], w=[ss])
                c.op("dve", lambda e: e.reciprocal(out=rstd[:, 0:1], in_=ss[:, 1:2]), r=[ss], w=[rstd])
                c.op("dve", lambda e: e.scalar_tensor_tensor(out=hf[:], in0=xt[:], scalar=rstd[:, 0:1], in1=gain[:], op0=ALU.mult, op1=ALU.mult), r=[xt, rstd, gain], w=[hf])
                c.op("act", lambda e: e.copy(out=hb[:], in_=hf[:]), r=[hf], w=[hb])
                c.op("pool", lambda e: e.tensor_tensor(out=hl[:], in0=hf[:], in1=hb[:], op=ALU.subtract), r=[hf, hb], w=[hl])
                c.dma("pool", T["hmoe"].ap()[tt * 128:(tt + 1) * 128, :], hb[:], r=[hb], w=[self.hmoe_buf], sbuf=hb)
                h_, l_ = hT[tt % 2], lT[tt % 2]
                self.transpose_into(hb, h_, 0, ptrs)
                self.transpose_into(hl, l_, 0, ptrs)
                p_ = pl[tt % 2]
                combos = [(h_, wrh), (l_, wrh), (h_, wrl)]
                for ci, (a_, w_) in enumerate(combos):
                    for k in range(16):
                        c.op("pe", lambda e: e.matmul(p_[:], a_[:, k, :], w_[:, k, :], start=(ci == 0 and k == 0), stop=(ci == 2 and k == 15)), r=[a_, w_], w=[p_], signal=(ci == 2 and k == 15))
                e_, s_, a2 = ex[tt % 2], se[tt % 2], af[tt % 2]
                c.op("act", lambda e: e.activation(out=e_[:], in_=p_[:], func=AF.Exp, accum_out=s_[:, 0:1]), r=[p_], w=[e_, s_])
                c.op("dve", lambda e: e.reciprocal(out=s_[:, 1:2], in_=s_[:, 0:1]), r=[s_], w=[s_])
                c.op("dve", lambda e: e.tensor_scalar(out=a2[:], in0=e_[:], scalar1=s_[:, 1:2], scalar2=None, op0=ALU.mult), r=[e_, s_], w=[a2])
                c.dma("pool", T["affc"].ap()[tt * 128:(tt + 1) * 128, :], a2[:], r=[a2], sbuf=a2)
                c.op("pe", lambda e: e.transpose(pa[:], a2[:], self.identf[:]), r=[a2, self.identf], w=[pa])
                c.op("act", lambda e: e.copy(out=aT[:, tt * 128:(tt + 1) * 128], in_=pa[:]), r=[pa], w=[aT])
            c.dma("sp", T["affT"].ap(), aT[:], r=[aT], w=[self.afft_buf], sbuf=aT)

    def stage_moe_x(self, l):
        c, T = self.c, self.T
        with ExitStack() as st:
            wbufs, pbanks = self.common_alloc(st, nw=4, npb=4)
            io = c.sb(st, "io", [128, 256], F32)
            pj = c.sb(st, "pj", [128, 16, 4], F32)
            pjb = [c.sb(st, "pjb%d" % i, [128, 16, 4], BF16) for i in range(2)]
            Sall = [c.sb(st, "Sall%d" % i, [128, 16, 256], BF16) for i in range(2)]
            c.dma("sp", io[:], T["iota256"].ap(), w=[io], sbuf=io)
            c.dma("sp", pj[:, :, 0:3], T["pj"].ap(), w=[pj], sbuf=pj)
            affc = c.sb(st, "affc", [128, 16, NE], F32)
            c.dma("sp", affc[:], T["affc"].ap().rearrange("(j p) e -> p j e", p=128), w=[affc], sbuf=affc)
            arow = [c.sb(st, "arow%d" % i, [128, L], F32) for i in range(2)]
            junk = c.sb(st, "junk", [128, L], BF16)
            rank = [c.sb(st, "rank%d" % i, [128, 16], F32) for i in range(2)]
            pidx = [c.ps(st, "pidx%d" % i, [128, 2, 4], F32) for i in range(2)]
            idf = [c.sb(st, "idf%d" % i, [128, 2, 6], F32) for i in range(2)]
            idxi = [[c.sb(st, "idx%d_%d" % (i, ch), [128, 1], I32) for ch in range(2)] for i in range(2)]
            xe = [c.sb(st, "xe%d" % i, [128, D], BF16) for i in range(2)]
            xeT = [c.sb(st, "xeT%d" % i, [128, 16, 256], BF16) for i in range(2)]
            hm = [c.sb(st, "hm%d" % i, [128, D], BF16) for i in range(2)]
            hmT = c.sb(st, "hmT", [128, 16, 256], BF16)
            sa = [c.sb(st, "sa%d" % i, [128, 512], F32) for i in range(2)]
            ysb = [c.sb(st, "ysb%d" % i, [128, D], F32) for i in range(2)]
            ptrs = [c.ps(st, "ptr%d" % i, [128, 4, 128], BF16) for i in range(2)]

            def rank_thunks(ex_):
                eb = ex_ % 2
                ar, rk = arow[eb], rank[eb]
                th = []

                def t0():
                    c.dma("sp", ar[:], row_bc(T["affT"], ex_ * L, L), r=[self.afft_buf], w=[ar], sbuf=ar)
                th.append(t0)
                for j in range(16):
                    def tj(j=j):
                        c.op("dve", lambda e: e.tensor_scalar(out=junk[:], in0=ar[:], scalar1=affc[:, j, ex_:ex_ + 1], scalar2=0.0, op0=ALU.is_gt, op1=ALU.add, accum_out=rk[:, j:j + 1]),
                             r=[ar, affc], w=[junk, rk])
                    th.append(tj)
                return th

            def slots_pre(ex_):
                eb = ex_ % 2
                rk, S_, pb_ = rank[eb], Sall[eb], pjb[eb]
                th = []
                th.append(lambda: c.op("dve", lambda e: e.tensor_copy(out=pj[:, :, 2], in_=affc[:, :, ex_]), r=[affc], w=[pj]))
                th.append(lambda: c.op("dve", lambda e: e.tensor_copy(out=pb_[:, :, 0:3], in_=pj[:, :, 0:3]), r=[pj], w=[pb_]))
                th.append(lambda: c.op("dve", lambda e: e.tensor_copy(out=pj[:, :, 3], in_=pb_[:, :, 2]), r=[pb_], w=[pj]))
                th.append(lambda: c.op("dve", lambda e: e.tensor_tensor(out=pb_[:, :, 3], in0=pj[:, :, 2], in1=pj[:, :, 3], op=ALU.subtract), r=[pj], w=[pb_]))
                for j in range(16):
                    th.append(lambda j=j: c.op("dve", lambda e: e.tensor_scalar(out=S_[:, j, :], in0=io[:], scalar1=rk[:, j:j + 1], scalar2=None, op0=ALU.is_equal), r=[io, rk], w=[S_]))
                return th

            def slots_idx(ex_):
                eb = ex_ % 2
                S_, pb_, pi_, f_ = Sall[eb], pjb[eb], pidx[eb], idf[eb]
                for ch in range(2):
                    for j in range(16):
                        c.op("pe", lambda e: e.matmul(pi_[:, ch, 0:4], S_[:, j, ch * 128:(ch + 1) * 128], pb_[:, j, :], start=(j == 0), stop=(j == 15)), r=[S_, pb_], w=[pi_], signal=(j == 15))
                c.op("dve", lambda e: e.tensor_copy(out=f_[:, :, 0:4], in_=pi_[:, :, 0:4]), r=[pi_], w=[f_])
                c.op("dve", lambda e: e.scalar_tensor_tensor(out=f_[:, :, 4], in0=f_[:, :, 1], scalar=128.0, in1=f_[:, :, 0], op0=ALU.mult, op1=ALU.add), r=[f_], w=[f_])
                c.op("dve", lambda e: e.tensor_tensor(out=f_[:, :, 5], in0=f_[:, :, 2], in1=f_[:, :, 3], op=ALU.add), r=[f_], w=[f_])
                for ch in range(2):
                    ix = idxi[eb][ch]
                    c.op("dve", lambda e: e.tensor_copy(out=ix[:], in_=f_[:, ch, 4:5]), r=[f_], w=[ix])

            def slots_gather(ex_):
                eb = ex_ % 2
                for ch in range(2):
                    ix = idxi[eb][ch]
                    x_ = xe[ch]
                    c.custom_dma("pool", lambda e: e.indirect_dma_start(out=x_[:], out_offset=None, in_=T["hmoe"].ap(), in_offset=bass.IndirectOffsetOnAxis(ap=ix[:, 0:1], axis=0)),
                                 r=[ix, self.hmoe_buf], w=[x_], sbuf=x_)

            def slots(ex_):
                for t in slots_pre(ex_):
                    t()
                slots_idx(ex_)
                slots_gather(ex_)

            def gather_T(ex_):
                for ch in range(2):
                    self.transpose_into(xe[ch], xeT[ex_ % 2], ch * 128, ptrs)

            def scatter(ex_):
                eb = ex_ % 2
                for ch in range(2):
                    ix = idxi[eb][ch]
                    y_ = ysb[ch]
                    c.custom_dma("pool", lambda e: e.indirect_dma_start(out=T["xres"].ap(), out_offset=bass.IndirectOffsetOnAxis(ap=ix[:, 0:1], axis=0), in_=y_[:], in_offset=None, compute_op=ALU.add),
                                 r=[ix, y_, self.xres_buf], w=[self.xres_buf], sbuf=y_)

            for t in rank_thunks(0):
                t()
            slots(0)
            gather_T(0)
            for ex_ in range(NE):
                eb = ex_ % 2
                xT_ = xeT[eb]
                f_ = idf[eb]
                wg = T["w_gate"].ap()[l, ex_]
                wu = T["w_up"].ap()[l, ex_]
                wd = T["w_down"].ap()[l, ex_]
                nxt = (rank_thunks(ex_ + 1) + slots_pre(ex_ + 1)) if ex_ + 1 < NE else []
                wcount = [0]

                def after_w(_nb):
                    wcount[0] += 1
                    if wcount[0] == 3 and ex_ > 0:
                        scatter(ex_ - 1)
                for nb in range(4):
                    def ev_gate(_nb, tt, pb, nb=nb):
                        s2 = sa[tt]
                        c.op("act", lambda e: e.activation(out=s2[:], in_=pb[:], func=AF.Silu), r=[pb], w=[s2])

                    def ev_up(_nb, tt, pb, nb=nb):
                        s2 = sa[tt]
                        c.op("dve", lambda e: e.tensor_tensor(out=hm[tt][:, nb * 512:(nb + 1) * 512], in0=pb[:], in1=s2[:], op=ALU.mult), r=[pb, s2], w=[hm[tt]])
                    self.gemm(xT_, 16, 256, wg[:, nb * 512:(nb + 1) * 512], 512, 512, "TM", ev_gate, wbufs, pbanks, after_w=after_w)
                    for _ in range(5):
                        if nxt:
                            nxt.pop(0)()
                    self.gemm(xT_, 16, 256, wu[:, nb * 512:(nb + 1) * 512], 512, 512, "TM", ev_up, wbufs, pbanks, after_w=after_w)
                    for _ in range(5):
                        if nxt:
                            nxt.pop(0)()
                while nxt:
                    nxt.pop(0)()
                if ex_ + 1 < NE:
                    slots_idx(ex_ + 1)
                for ch in range(2):
                    self.transpose_into(hm[ch], hmT, ch * 128, ptrs)

                def after_wd(nb):
                    if nb == 2 and ex_ + 1 < NE:
                        slots_gather(ex_ + 1)
                    if nb == 3 and ex_ + 1 < NE:
                        gather_T(ex_ + 1)

                def ev_down(nb, tt, pb):
                    y_ = ysb[tt]
                    if nb % 2 == 0:
                        c.op("act", lambda e: e.activation(out=y_[:, nb * 512:(nb + 1) * 512], in_=pb[:], func=AF.Copy, scale=f_[:, tt, 5:6]), r=[pb, f_], w=[y_])
                    else:
                        c.op("dve", lambda e: e.tensor_scalar(out=y_[:, nb * 512:(nb + 1) * 512], in0=pb[:], scalar1=f_[:, tt, 5:6], scalar2=None, op0=ALU.mult), r=[pb, f_], w=[y_])
                self.gemm(hmT, 16, 256, wd, D, 512, "TM", ev_down, wbufs, pbanks, after_w=after_wd)
            scatter(NE - 1)

    def stage_final(self):
        c, T = self.c, self.T
        with ExitStack() as st:
            self.common_alloc(st, nw=0, npb=0)
            gain = self.load_bc(st, "gain", T["final_norm"], 0, D)
            xts = [c.sb(st, "xt%d" % i, [128, D], F32) for i in range(3)]
            jk = [c.sb(st, "jk%d" % i, [128, D], BF16) for i in range(2)]
            os_ = [c.sb(st, "os%d" % i, [128, D], F32) for i in range(2)]
            sm = [c.sb(st, "sm%d" % i, [128, 3], F32) for i in range(2)]
            for tt in range(16):
                xt, j_, o_, s_ = xts[tt % 3], jk[tt % 2], os_[tt % 2], sm[tt % 2]
                c.dma("sp", xt[:], T["xres"].ap()[tt * 128:(tt + 1) * 128, :], r=[self.xres_buf], w=[xt], sbuf=xt)
                c.op("act", lambda e: e.activation(out=j_[:], in_=xt[:], func=AF.Square, accum_out=s_[:, 0:1]), r=[xt], w=[j_, s_])
                c.op("act", lambda e: e.activation(out=s_[:, 1:2], in_=s_[:, 0:1], func=AF.Sqrt, scale=1.0 / D, bias=self.epsb[:, 0:1]), r=[s_, self.epsb], w=[s_])
                c.op("dve", lambda e: e.reciprocal(out=s_[:, 2:3], in_=s_[:, 1:2]), r=[s_], w=[s_])
                c.op("dve", lambda e: e.scalar_tensor_tensor(out=o_[:], in0=xt[:], scalar=s_[:, 2:3], in1=gain[:], op0=ALU.mult, op1=ALU.mult), r=[xt, s_, gain], w=[o_])
                c.dma("pool", T["out"].ap()[tt * 128:(tt + 1) * 128, :], o_[:], r=[o_], sbuf=o_)


_CACHE = {}


def host_inputs(inputs, consts, b):
    m = {"x": np.ascontiguousarray(inputs["x"][b]), "mem": np.ascontiguousarray(inputs["mem"][b])}
    for k in WEIGHT_SHAPES:
        if k == "rpbY":
            continue
        m[k] = np.ascontiguousarray(np.asarray(inputs[k], np.float32))
    m["rpbY"] = rpb_layout(np.asarray(inputs["na_rpb"], np.float32))
    for k in CONST_DTYPES:
        m[k] = consts[k]
    return m


def kernel(**inputs):
    inputs = {k: np.asarray(v) for k, v in inputs.items()}
    if "prog" not in _CACHE:
        p = Prog()
        p.build()
        _CACHE["prog"] = p
    p = _CACHE["prog"]
    B = inputs["x"].shape[0]
    in_maps = [host_inputs(inputs, p.consts, b) for b in range(B)]
    res = run_bass_kernel_spmd(p.nc, in_maps, core_ids=[0, 2, 4, 6][:B])
    out = np.stack([np.asarray(r["out"], np.float32) for r in res.results], axis=0)
    return out
```
